# Optimizing a Trainium2 kernel written in Bass

```python
import jax, jax.numpy as jnp
from jax import lax
import numpy as np

D_MODEL = 1024
BATCH = 2
SEQ = 16384
DEPTH = 4

CHUNK = 64
N_A_LAYERS = DEPTH // 2
N_B_LAYERS = DEPTH - N_A_LAYERS
N_HEADS = 16
HEAD_DIM = D_MODEL // N_HEADS
Q_BLOCK = 128
CONV_WIDTH = 31
N_EXPERTS = 32
TOP_K = 4
D_EXPERT = D_MODEL
SWIGLU_ALPHA = 1.702
SWIGLU_LIMIT = 7.0
EXPERT_BLOCK = 256
EPS = 1e-6

kernel_name = "yoco_conformer_fox_moe_trunk"


def rmsnorm(x, g):
    xf = x.astype(jnp.float32)
    y = xf * lax.rsqrt(jnp.mean(xf * xf, axis=-1, keepdims=True) + EPS)
    return (y * g.astype(jnp.float32)).astype(x.dtype)


def layernorm(x, g, b):
    xf = x.astype(jnp.float32)
    mu = jnp.mean(xf, axis=-1, keepdims=True)
    var = jnp.mean(jnp.square(xf - mu), axis=-1, keepdims=True)
    y = (xf - mu) * lax.rsqrt(var + EPS)
    return (y * g.astype(jnp.float32) + b.astype(jnp.float32)).astype(x.dtype)


def modulate(h, shift, scale):
    return h * (1 + scale[:, None, :]) + shift[:, None, :]


def conformer_conv(h, w_pw1, b_pw1, w_dw, b_dw, ln_g, ln_b, w_pw2, b_pw2):
    d = h.shape[-1]
    u = h @ w_pw1 + b_pw1
    u = u[..., :d] * jax.nn.sigmoid(u[..., d:])
    u = lax.conv_general_dilated(
        u, w_dw[:, None, :], window_strides=(1,), padding=[(CONV_WIDTH - 1, 0)],
        dimension_numbers=('NWC', 'WIO', 'NWC'), feature_group_count=d) + b_dw
    u = jax.nn.silu(layernorm(u, ln_g, ln_b))
    return u @ w_pw2 + b_pw2


def shared_kv(x, shift, scale, kv_norm_g, w_kvf, b_f, k_norm_g):
    b, s, d = x.shape
    h = modulate(rmsnorm(x, kv_norm_g), shift, scale)
    kvf = h @ w_kvf
    k = rmsnorm(kvf[..., :d].reshape(b, s, N_HEADS, HEAD_DIM), k_norm_g).transpose(0, 2, 1, 3)
    v = kvf[..., d:2 * d].reshape(b, s, N_HEADS, HEAD_DIM).transpose(0, 2, 1, 3)
    fz = kvf[..., 2 * d:].astype(jnp.float32) + b_f.astype(jnp.float32)
    cum_logf = jnp.cumsum(jax.nn.log_sigmoid(fz), axis=1).transpose(0, 2, 1)
    return k, v, cum_logf


def fox_attention(q, k, v, cum_logf):
    b, h, s, dh = q.shape
    nq = s // Q_BLOCK
    qb = q.reshape(b, h, nq, Q_BLOCK, dh).transpose(2, 0, 1, 3, 4)
    fb = cum_logf.reshape(b, h, nq, Q_BLOCK).transpose(2, 0, 1, 3)
    kpos = jnp.arange(s)
    inv_sqrt = 1.0 / float(np.sqrt(dh))

    def one_block(args):
        i, qi, fi = args
        sc = jnp.einsum('bhqd,bhkd->bhqk', qi, k, preferred_element_type=jnp.float32) * inv_sqrt
        sc = sc + fi[..., None] - cum_logf[:, :, None, :]
        qpos = i * Q_BLOCK + jnp.arange(Q_BLOCK)
        sc = jnp.where(kpos[None, :] <= qpos[:, None], sc, -jnp.inf)
        p = jax.nn.softmax(sc, axis=-1)
        return jnp.einsum('bhqk,bhkd->bhqd', p.astype(v.dtype), v)

    out = lax.map(one_block, (jnp.arange(nq), qb, fb))
    return out.transpose(1, 2, 0, 3, 4).reshape(b, h, s, dh)


def fox_mixer(h, w_qg, q_norm_g, w_o, k, v, cum_logf):
    b, s, d = h.shape
    qg = h @ w_qg
    q = rmsnorm(qg[..., :d].reshape(b, s, N_HEADS, HEAD_DIM), q_norm_g).transpose(0, 2, 1, 3)
    o = fox_attention(q, k, v, cum_logf).transpose(0, 2, 1, 3).reshape(b, s, d)
    return (o * jax.nn.sigmoid(qg[..., d:])) @ w_o


def moe_ffn(h, router_w, router_b, w_gu, b_gu, w_down, b_down):
    b, s, d = h.shape
    n = b * s
    t = h.reshape(n, d)
    logits = (t @ router_w).astype(jnp.float32) + router_b.astype(jnp.float32)
    top_v, top_i = lax.top_k(logits, TOP_K)
    gate = jax.nn.softmax(top_v, axis=-1)
    nk = n * TOP_K
    flat_e = top_i.reshape(nk).astype(jnp.int32)
    flat_tok = jnp.arange(nk, dtype=jnp.int32) // TOP_K
    flat_gate = gate.reshape(nk)
    order = jnp.argsort(flat_e)
    sorted_e = flat_e[order]
    counts = jnp.bincount(flat_e, length=N_EXPERTS).astype(jnp.int32)
    padded = (counts + EXPERT_BLOCK - 1) // EXPERT_BLOCK * EXPERT_BLOCK
    start = jnp.cumsum(counts) - counts
    pend = jnp.cumsum(padded)
    pstart = pend - padded
    dest = pstart[sorted_e] + jnp.arange(nk, dtype=jnp.int32) - start[sorted_e]
    n_blocks = -(-nk // EXPERT_BLOCK) + N_EXPERTS
    p_rows = n_blocks * EXPERT_BLOCK
    row_tok = jnp.full((p_rows,), n, jnp.int32).at[dest].set(flat_tok[order])
    row_w = jnp.zeros((p_rows,), jnp.float32).at[dest].set(flat_gate[order])
    block_e = jnp.minimum(
        jnp.searchsorted(pend, jnp.arange(n_blocks, dtype=jnp.int32) * EXPERT_BLOCK, side='right'),
        N_EXPERTS - 1)
    t_pad = jnp.concatenate([t, jnp.zeros((1, d), t.dtype)], axis=0)

    def expert_block(args):
        rows, wts, e = args
        xb = t_pad[rows]
        gu = xb @ w_gu[e] + b_gu[e]
        x_glu = jnp.minimum(gu[:, :D_EXPERT], SWIGLU_LIMIT)
        x_lin = jnp.clip(gu[:, D_EXPERT:], -SWIGLU_LIMIT, SWIGLU_LIMIT)
        act = x_glu * jax.nn.sigmoid(SWIGLU_ALPHA * x_glu) * (x_lin + 1)
        yb = act @ w_down[e] + b_down[e]
        return yb * wts[:, None].astype(yb.dtype)

    out = lax.map(expert_block, (row_tok.reshape(n_blocks, EXPERT_BLOCK),
                                 row_w.reshape(n_blocks, EXPERT_BLOCK), block_e))
    y = jax.ops.segment_sum(out.reshape(p_rows, d), row_tok, num_segments=n + 1)[:n]
    return y.reshape(b, s, d)


def setup_inputs(seed: int = 0) -> dict:
    key = jax.random.key(seed)
    ks = iter(jax.random.split(key, 40))
    D = D_MODEL
    E = N_EXPERTS
    F = D_EXPERT

    def nrm(shape, s):
        return jax.random.normal(next(ks), shape, jnp.float32) * s

    inputs = {
        'x': nrm((BATCH, SEQ, D), 1.0),
        'c': nrm((BATCH, D), 1.0),
        'mod_w': nrm((DEPTH, D, 6 * D), 0.5 * D ** -0.5),
        'mod_b': nrm((DEPTH, 6 * D), 0.02),
        'norm1_g': 1.0 + nrm((DEPTH, D), 0.05),
        'norm2_g': 1.0 + nrm((DEPTH, D), 0.05),
        'conv_w_pw1': nrm((N_A_LAYERS, D, 2 * D), D ** -0.5),
        'conv_b_pw1': nrm((N_A_LAYERS, 2 * D), 0.02),
        'conv_w_dw': nrm((N_A_LAYERS, CONV_WIDTH, D), CONV_WIDTH ** -0.5),
        'conv_b_dw': nrm((N_A_LAYERS, D), 0.02),
        'conv_ln_g': 1.0 + nrm((N_A_LAYERS, D), 0.05),
        'conv_ln_b': nrm((N_A_LAYERS, D), 0.02),
        'conv_w_pw2': nrm((N_A_LAYERS, D, D), D ** -0.5),
        'conv_b_pw2': nrm((N_A_LAYERS, D), 0.02),
        'kv_mod_w': nrm((D, 2 * D), 0.5 * D ** -0.5),
        'kv_mod_b': nrm((2 * D,), 0.02),
        'kv_norm_g': 1.0 + nrm((D,), 0.05),
    }
    w_kv = nrm((D, 2 * D), D ** -0.5)
    w_f = nrm((D, N_HEADS), 0.1 * D ** -0.5)
    inputs['w_kvf'] = jnp.concatenate([w_kv, w_f], axis=1)
    inputs['b_f'] = jnp.linspace(1.0, 6.0, N_HEADS, dtype=jnp.float32) + nrm((N_HEADS,), 0.1)
    inputs['k_norm_g'] = 1.0 + nrm((HEAD_DIM,), 0.05)
    inputs['attn_w_qg'] = nrm((N_B_LAYERS, D, 2 * D), D ** -0.5)
    inputs['q_norm_g'] = 1.0 + nrm((N_B_LAYERS, HEAD_DIM), 0.05)
    inputs['attn_w_o'] = nrm((N_B_LAYERS, D, D), D ** -0.5)
    inputs['moe_router_w'] = nrm((DEPTH, D, E), D ** -0.5)
    inputs['moe_router_b'] = nrm((DEPTH, E), 0.01)
    inputs['moe_w_gu'] = nrm((DEPTH, E, D, 2 * F), D ** -0.5)
    inputs['moe_b_gu'] = nrm((DEPTH, E, 2 * F), 0.02)
    inputs['moe_w_down'] = nrm((DEPTH, E, F, D), F ** -0.5)
    inputs['moe_b_down'] = nrm((DEPTH, E, D), 0.02)
    inputs['final_norm_g'] = 1.0 + nrm((D,), 0.05)
    return inputs


def reference(x, c, mod_w, mod_b, norm1_g, norm2_g,
              conv_w_pw1, conv_b_pw1, conv_w_dw, conv_b_dw, conv_ln_g, conv_ln_b,
              conv_w_pw2, conv_b_pw2, kv_mod_w, kv_mod_b, kv_norm_g,
              w_kvf, b_f, k_norm_g, attn_w_qg, q_norm_g, attn_w_o,
              moe_router_w, moe_router_b, moe_w_gu, moe_b_gu, moe_w_down, moe_b_down,
              final_norm_g):
    d = x.shape[-1]
    c_act = jax.nn.silu(c)
    k = v = cum_logf = None
    for l in range(DEPTH):
        mods = c_act @ mod_w[l] + mod_b[l]
        sh1, sc1, g1, sh2, sc2, g2 = [mods[:, i * d:(i + 1) * d] for i in range(6)]
        h = modulate(rmsnorm(x, norm1_g[l]), sh1, sc1)
        if l < N_A_LAYERS:
            y = conformer_conv(h, conv_w_pw1[l], conv_b_pw1[l], conv_w_dw[l], conv_b_dw[l],
                               conv_ln_g[l], conv_ln_b[l], conv_w_pw2[l], conv_b_pw2[l])
        else:
            lb = l - N_A_LAYERS
            y = fox_mixer(h, attn_w_qg[lb], q_norm_g[lb], attn_w_o[lb], k, v, cum_logf)
        x = x + g1[:, None, :] * y
        h = modulate(rmsnorm(x, norm2_g[l]), sh2, sc2)
        x = x + g2[:, None, :] * moe_ffn(h, moe_router_w[l], moe_router_b[l], moe_w_gu[l],
                                         moe_b_gu[l], moe_w_down[l], moe_b_down[l])
        if l == N_A_LAYERS - 1:
            kv_mods = c_act @ kv_mod_w + kv_mod_b
            k, v, cum_logf = shared_kv(x, kv_mods[:, :d], kv_mods[:, d:], kv_norm_g,
                                       w_kvf, b_f, k_norm_g)
    return rmsnorm(x, final_norm_g)
```

```python
import os
import numpy as np
import ml_dtypes
import concourse.bass as bass
import concourse.mybir as mybir
from concourse.bass_utils import run_bass_kernel_spmd
from contextlib import ExitStack, contextmanager

F32 = mybir.dt.float32
BF16 = mybir.dt.bfloat16
I32 = mybir.dt.int32
ALU = mybir.AluOpType
AF = mybir.ActivationFunctionType
IOA = bass.IndirectOffsetOnAxis

P = 128
D = 1024
NE = 32
EPS = 1e-6
NCORES = 8
SEQ = 16384
TOWN = 4096
BLK = 512


class Buf:
    def __init__(self, t, name, dma_sem_key=None):
        self.t = t
        self.name = name
        self.last_w = None
        self.reads = []
        self.dma_key = dma_sem_key
        self.dma_cnt = 0

    def __getitem__(self, idx):
        return self.t[idx]

    def ap(self):
        return self.t.ap()


class Eng:
    def __init__(self, name, h):
        self.name = name
        self.h = h
        self.key = "e_" + name
        self.cnt = 0
        self.waited = {}


class Ctx:
    def __init__(self, nc):
        self.nc = nc
        self.root = ExitStack()
        self.stack = [self.root]
        self.sems = {}
        self.engs = {}
        self.free_dma_sems = []
        self.live_dma = {}
        for name, h in (("pe", nc.tensor), ("act", nc.scalar), ("dve", nc.vector),
                        ("pool", nc.gpsimd), ("sp", nc.sync)):
            e = Eng(name, h)
            self.sems[e.key] = self.root.enter_context(nc.semaphore(e.key))
            self.engs[name] = e
        self.nbuf = 0
        self.n_inst = 0
        self.sem_cnt = {}

    def _dma_key(self):
        if self.free_dma_sems:
            return self.free_dma_sems.pop()
        key = f"d{len(self.sems)}"
        self.sems[key] = self.root.enter_context(self.nc.semaphore(key))
        self.sem_cnt[key] = 0
        return key

    def sbuf(self, shape, dtype=F32, dma=False, name=None):
        self.nbuf += 1
        name = name or f"sb{self.nbuf}"
        t = self.stack[-1].enter_context(self.nc.sbuf_tensor(name, list(shape), dtype))
        b = Buf(t, name)
        if dma:
            b.dma_key = self._dma_key()
            b.dma_cnt = self.sem_cnt[b.dma_key]
            self.scope_keys[-1].append(b.dma_key) if self.scope_keys else None
        return b

    def psum(self, shape, dtype=F32, name=None):
        self.nbuf += 1
        name = name or f"ps{self.nbuf}"
        t = self.stack[-1].enter_context(self.nc.psum_tensor(name, list(shape), dtype))
        return Buf(t, name)

    def dram(self, name, shape, dtype, kind="Internal"):
        t = self.nc.dram_tensor(name, list(shape), dtype, kind=kind)
        return Buf(t, name)

    scope_keys = []

    @contextmanager
    def scope(self):
        es = ExitStack()
        self.stack.append(es)
        self.scope_keys.append([])
        try:
            yield
        finally:
            self.barrier()
            keys = self.scope_keys.pop()
            self.free_dma_sems.extend(keys)
            self.stack.pop()
            es.close()

    def barrier(self):
        for e in self.engs.values():
            for o in self.engs.values():
                if o is not e and o.cnt > 0:
                    self._wait(e, (o.key, o.cnt))
            for key, cnt in self.sem_cnt.items():
                if cnt > 0:
                    self._wait(e, (key, cnt))

    def _wait(self, eng, tok):
        if tok is None:
            return
        key, val = tok
        if key == eng.key and eng.name in ("pe", "sp"):
            return
        if eng.waited.get(key, 0) >= val:
            return
        eng.waited[key] = val
        eng.h.wait_ge(self.sems[key], val)

    def _deps(self, eng, reads, writes, nowaw=False):
        for r in reads:
            self._wait(eng, r.last_w)
        for w in writes:
            if not nowaw:
                self._wait(eng, w.last_w)
            for tok in w.reads:
                self._wait(eng, tok)

    def _commit(self, tok, reads, writes):
        for r in reads:
            r.reads.append(tok)
            if len(r.reads) > 48:
                best = {}
                for k, v in r.reads:
                    best[k] = max(best.get(k, 0), v)
                r.reads = list(best.items())
        for w in writes:
            w.last_w = tok
            w.reads = []

    def op(self, eng_name, fn, reads=(), writes=()):
        eng = self.engs[eng_name]
        self._deps(eng, reads, writes)
        inst = fn(eng.h)
        eng.cnt += 1
        inst.then_inc(self.sems[eng.key], 1)
        self._commit((eng.key, eng.cnt), reads, writes)
        self.n_inst += 1

    def dma(self, eng_name, fn, reads=(), writes=(), nowaw=False):
        eng = self.engs[eng_name]
        self._deps(eng, reads, writes, nowaw)
        sb = writes[0]
        if sb.dma_key is None:
            sb.dma_key = self._dma_key()
        inst = fn(eng.h)
        self.sem_cnt[sb.dma_key] += 16
        inst.then_inc(self.sems[sb.dma_key], 16)
        self._commit((sb.dma_key, self.sem_cnt[sb.dma_key]), reads, writes)
        self.n_inst += 1

    def finish(self, bufs):
        self.barrier()

    def close(self):
        self.root.close()


def rms_rstd(c, xt, junk, ss):
    c.op("act", lambda e: e.activation(out=junk[:, :], in_=xt[:, :], func=AF.Square, accum_out=ss[:, 0:1]),
         reads=[xt], writes=[junk, ss])
    c.op("dve", lambda e: e.tensor_scalar(out=ss[:, 1:2], in0=ss[:, 0:1], scalar1=1.0 / D, scalar2=EPS,
                                          op0=ALU.mult, op1=ALU.add), reads=[ss], writes=[ss])
    c.op("dve", lambda e: e.reciprocal(out=ss[:, 1:2], in_=ss[:, 1:2]), reads=[ss], writes=[ss])
    c.op("act", lambda e: e.activation(out=ss[:, 1:2], in_=ss[:, 1:2], func=AF.Sqrt), reads=[ss], writes=[ss])


def norm_mod(c, xt, h, ss, A, sh, modsb):
    rms_rstd(c, xt, h, ss)
    c.op("dve", lambda e: e.scalar_tensor_tensor(out=h[:, :], in0=xt[:, :], scalar=ss[:, 1:2],
                                                 in1=modsb[:, A:A + D], op0=ALU.mult, op1=ALU.mult),
         reads=[xt, ss, modsb], writes=[h])
    c.op("dve", lambda e: e.tensor_tensor(out=h[:, :], in0=h[:, :], in1=modsb[:, sh:sh + D], op=ALU.add),
         reads=[h, modsb], writes=[h])


class Consts:
    pass


def load_consts(c, d):
    k = Consts()
    k.identf = c.sbuf([P, P], F32, dma=True)
    k.identb = c.sbuf([P, P], BF16, dma=True)
    k.triu = c.sbuf([P, P], F32, dma=True)
    k.tril = c.sbuf([P, P], F32, dma=True)
    k.onesf = c.sbuf([P, P], F32)
    k.onesb = c.sbuf([P, P], BF16)
    c.dma("sp", lambda e: e.dma_start(out=k.identf[:, :], in_=d["identf"].ap()), writes=[k.identf])
    c.dma("sp", lambda e: e.dma_start(out=k.identb[:, :], in_=d["identb"].ap()), writes=[k.identb])
    c.dma("sp", lambda e: e.dma_start(out=k.triu[:, :], in_=d["triu"].ap()), writes=[k.triu])
    c.dma("sp", lambda e: e.dma_start(out=k.tril[:, :], in_=d["tril"].ap()), writes=[k.tril])
    c.op("dve", lambda e: e.memset(k.onesf[:, :], 1.0), writes=[k.onesf])
    c.op("dve", lambda e: e.memset(k.onesb[:, :], 1.0), writes=[k.onesb])
    return k


def host_consts():
    ii = np.arange(P)
    return dict(
        identf=np.eye(P, dtype=np.float32),
        identb=np.eye(P, dtype=np.float32).astype(ml_dtypes.bfloat16),
        triu=(ii[:, None] < ii[None, :]).astype(np.float32),
        tril=(ii[:, None] <= ii[None, :]).astype(np.float32),
    )


CONST_SPECS = dict(identf=([P, P], F32), identb=([P, P], BF16), triu=([P, P], F32), tril=([P, P], F32))


def stage_mods(c, k, cr_d, mw_d, mb_d, ng_d, modsb, ncols=6 * D, nnorm=2):
    with c.scope():
        cr = c.sbuf([P, 8], F32, dma=True)
        sg = c.sbuf([P, 8], F32)
        cb = c.sbuf([P, 8, P], F32)
        mbb = c.sbuf([P, ncols], F32, dma=True)
        nb = c.sbuf([P, nnorm, D], F32, dma=True)
        mwt = [c.sbuf([P, 8, 512], F32, dma=True) for _ in range(2)]
        ps = [c.psum([P, 512], F32) for _ in range(2)]
        c.dma("sp", lambda e: e.dma_start(out=cr[:, :], in_=cr_d.ap()), writes=[cr])
        c.dma("sp", lambda e: e.dma_start(out=mbb[:, :], in_=mb_d.ap().partition_broadcast(P)), writes=[mbb])
        for i in range(nnorm):
            c.dma("sp", lambda e: e.dma_start(out=nb[:, i, :], in_=ng_d.ap()[i:i + 1, :].partition_broadcast(P)),
                  writes=[nb])
        c.op("act", lambda e: e.activation(out=sg[:, :], in_=cr[:, :], func=AF.Sigmoid), reads=[cr], writes=[sg])
        c.op("dve", lambda e: e.tensor_tensor(out=sg[:, :], in0=sg[:, :], in1=cr[:, :], op=ALU.mult),
             reads=[sg, cr], writes=[sg])
        for kc in range(8):
            c.op("dve", lambda e: e.tensor_scalar(out=cb[:, kc, :], in0=k.onesf[:, :], scalar1=sg[:, kc:kc + 1],
                                                  scalar2=None, op0=ALU.mult), reads=[k.onesf, sg], writes=[cb])
        mwv = mw_d.ap().rearrange("(kc p) n -> p kc n", p=P)
        for j in range(ncols // 512):
            w = mwt[j % 2]
            pj = ps[j % 2]
            c.dma("sp", lambda e: e.dma_start(out=w[:, :, :], in_=mwv[:, :, j * 512:(j + 1) * 512]), writes=[w])
            for kc in range(8):
                c.op("pe", lambda e: e.matmul(pj[:, :], lhsT=cb[:, kc, :], rhs=w[:, kc, :], start=(kc == 0),
                                              stop=(kc == 7)), reads=[cb, w], writes=[pj])
            c.op("dve", lambda e: e.tensor_tensor(out=modsb[:, j * 512:(j + 1) * 512], in0=pj[:, :],
                                                  in1=mbb[:, j * 512:(j + 1) * 512], op=ALU.add),
                 reads=[pj, mbb], writes=[modsb])
        if nnorm == 2:
            slots = [(1, 0), (4, 1)]
        else:
            slots = [(1, 0)]
        for s, i in slots:
            c.op("dve", lambda e: e.scalar_tensor_tensor(out=modsb[:, s * D:(s + 1) * D], in0=modsb[:, s * D:(s + 1) * D],
                                                         scalar=1.0, in1=nb[:, i, :], op0=ALU.add, op1=ALU.mult),
                 reads=[modsb, nb], writes=[modsb])


def stage_conv(c, k, modsb, xin_d, flag_d, w, xmid_d, town=TOWN):
    CW = 31
    with c.scope():
        w1b = c.sbuf([P, 8, 2 * D], BF16, dma=True)
        w2b = c.sbuf([P, 8, D], BF16, dma=True)
        b1 = c.sbuf([P, 16], F32, dma=True)
        wdw = c.sbuf([P, 8, CW], F32, dma=True)
        bdw = c.sbuf([P, 8], F32, dma=True)
        lng = c.sbuf([P, 8], F32, dma=True)
        lnb = c.sbuf([P, 8], F32, dma=True)
        b2b = c.sbuf([1, D], BF16, dma=True)
        flag = c.sbuf([P, 1], F32, dma=True)
        w1v = w["pw1"].ap().rearrange("(kc p) n -> p kc n", p=P)
        w2v = w["pw2"].ap().rearrange("(kc p) n -> p kc n", p=P)
        for kc in range(8):
            c.dma("pool", lambda e: e.dma_start(out=w1b[:, kc, :], in_=w1v[:, kc, :]), writes=[w1b], nowaw=True)
            c.dma("pool", lambda e: e.dma_start(out=w2b[:, kc, :], in_=w2v[:, kc, :]), writes=[w2b], nowaw=True)
        c.dma("pool", lambda e: e.dma_start(out=b2b[:, :], in_=w["b_pw2"].ap()), writes=[b2b])
        for t, src in ((b1, "b_pw1"), (wdw, "w_dw"), (bdw, "b_dw"), (lng, "ln_g"), (lnb, "ln_b")):
            if t is wdw:
                c.dma("sp", lambda e: e.dma_start(out=t[:, :, :], in_=w[src].ap()), writes=[t])
            else:
                c.dma("sp", lambda e: e.dma_start(out=t[:, :], in_=w[src].ap()), writes=[t])
        c.dma("sp", lambda e: e.dma_start(out=flag[:, :], in_=flag_d.ap()), writes=[flag])

        xts = [c.sbuf([P, D], F32, dma=True) for _ in range(2)]
        xrs = [c.sbuf([P, D], F32, dma=True) for _ in range(2)]
        xos = [c.sbuf([P, D], F32) for _ in range(2)]
        h = c.sbuf([P, D], F32)
        ss = c.sbuf([P, 2], F32)
        hT = c.sbuf([P, 8, BLK], BF16)
        uT = c.sbuf([P, 8, 30 + BLK], BF16)
        acc = [c.sbuf([P, BLK], F32) for _ in range(8)]
        vb = c.sbuf([P, 8, BLK], BF16)
        v2 = c.sbuf([P, 8, BLK], BF16)
        sT = c.sbuf([P, 8, BLK], BF16)
        sgt = [c.sbuf([P, BLK], F32) for _ in range(2)]
        mean = c.sbuf([P, BLK], F32)
        msq = c.sbuf([P, BLK], F32)
        rstd = c.sbuf([P, BLK], F32)
        tmp = c.sbuf([P, 512], F32)
        pT = c.psum([P, D], F32)
        psA = [c.psum([P, BLK], F32) for _ in range(2)]
        psG = [c.psum([P, BLK], F32) for _ in range(2)]
        psO = [c.psum([P, 512], F32) for _ in range(2)]
        c.op("dve", lambda e: e.memset(uT[:, :, 0:30], 0.0), writes=[uT])

        blocks = [(0, P, True)] + [(P + i * BLK, BLK, False) for i in range(town // BLK)]
        nx = 0
        for (t0, n, is_halo) in blocks:
            nt = n // P
            for i in range(nt):
                xt = xts[nx % 2]
                nx += 1
                r0 = t0 + i * P
                c.dma("sp", lambda e: e.dma_start(out=xt[:, :], in_=xin_d.ap()[r0:r0 + P, :]), writes=[xt])
                norm_mod(c, xt, h, ss, 1 * D, 0 * D, modsb)
                for kc in range(8):
                    c.op("pe", lambda e: e.transpose(out=pT[:, kc * P:(kc + 1) * P], in_=h[:, kc * P:(kc + 1) * P],
                                                     identity=k.identf[:, :]), reads=[h, k.identf], writes=[pT])
                c.op("act", lambda e: e.activation(out=hT[:, :, i * P:(i + 1) * P],
                                                   in_=pT[:, :].rearrange("p (k t) -> p k t", k=8), func=AF.Copy),
                     reads=[pT], writes=[hT])
            for fc in range(8):
                pa, pg, sg = psA[fc % 2], psG[fc % 2], sgt[fc % 2]
                for kc in range(8):
                    c.op("pe", lambda e: e.matmul(pa[:, 0:n], lhsT=w1b[:, kc, fc * P:(fc + 1) * P], rhs=hT[:, kc, 0:n],
                                                  start=(kc == 0), stop=(kc == 7)), reads=[w1b, hT], writes=[pa])
                for kc in range(8):
                    c.op("pe", lambda e: e.matmul(pg[:, 0:n], lhsT=w1b[:, kc, D + fc * P:D + (fc + 1) * P],
                                                  rhs=hT[:, kc, 0:n], start=(kc == 0), stop=(kc == 7)),
                         reads=[w1b, hT], writes=[pg])
                c.op("act", lambda e: e.activation(out=sg[:, 0:n], in_=pg[:, 0:n], func=AF.Sigmoid,
                                                   bias=b1[:, 8 + fc:9 + fc]), reads=[pg, b1], writes=[sg])
                c.op("dve", lambda e: e.scalar_tensor_tensor(out=uT[:, fc, 30:30 + n], in0=pa[:, 0:n],
                                                             scalar=b1[:, fc:fc + 1], in1=sg[:, 0:n],
                                                             op0=ALU.add, op1=ALU.mult),
                     reads=[pa, b1, sg], writes=[uT])
            if is_halo:
                c.op("dve", lambda e: e.tensor_scalar(out=uT[:, :, 30:30 + n], in0=uT[:, :, 30:30 + n],
                                                      scalar1=flag[:, 0:1], scalar2=None, op0=ALU.mult),
                     reads=[uT, flag], writes=[uT])
            else:
                for cc in range(8):
                    en = "dve"
                    a = acc[cc]
                    c.op(en, lambda e: e.tensor_scalar(out=a[:, 0:n], in0=uT[:, cc, 0:n], scalar1=wdw[:, cc, 0:1],
                                                       scalar2=bdw[:, cc:cc + 1], op0=ALU.mult, op1=ALU.add),
                         reads=[uT, wdw, bdw], writes=[a])
                    for j in range(1, CW):
                        c.op(en, lambda e: e.scalar_tensor_tensor(out=a[:, 0:n], in0=uT[:, cc, j:j + n],
                                                                  scalar=wdw[:, cc, j:j + 1], in1=a[:, 0:n],
                                                                  op0=ALU.mult, op1=ALU.add),
                             reads=[uT, wdw, a], writes=[a])
                for cc in range(8):
                    a = acc[cc]
                    c.op("act", lambda e: e.activation(out=vb[:, cc, 0:n], in_=a[:, 0:n], func=AF.Copy),
                         reads=[a], writes=[vb])
                    c.op("act", lambda e: e.activation(out=v2[:, cc, 0:n], in_=a[:, 0:n], func=AF.Square),
                         reads=[a], writes=[v2])
                s1, s2 = psA[0], psG[0]
                for cc in range(8):
                    c.op("pe", lambda e: e.matmul(s1[:, 0:n], lhsT=k.onesb[:, :], rhs=vb[:, cc, 0:n], start=(cc == 0),
                                                  stop=(cc == 7)), reads=[k.onesb, vb], writes=[s1])
                for cc in range(8):
                    c.op("pe", lambda e: e.matmul(s2[:, 0:n], lhsT=k.onesb[:, :], rhs=v2[:, cc, 0:n], start=(cc == 0),
                                                  stop=(cc == 7)), reads=[k.onesb, v2], writes=[s2])
                c.op("dve", lambda e: e.tensor_scalar(out=mean[:, 0:n], in0=s1[:, 0:n], scalar1=1.0 / D, scalar2=None,
                                                      op0=ALU.mult), reads=[s1], writes=[mean])
                c.op("dve", lambda e: e.tensor_tensor(out=msq[:, 0:n], in0=mean[:, 0:n], in1=mean[:, 0:n], op=ALU.mult),
                     reads=[mean], writes=[msq])
                c.op("dve", lambda e: e.scalar_tensor_tensor(out=rstd[:, 0:n], in0=s2[:, 0:n], scalar=1.0 / D,
                                                             in1=msq[:, 0:n], op0=ALU.mult, op1=ALU.subtract),
                     reads=[s2, msq], writes=[rstd])
                c.op("dve", lambda e: e.tensor_scalar(out=rstd[:, 0:n], in0=rstd[:, 0:n], scalar1=EPS, scalar2=None,
                                                      op0=ALU.add), reads=[rstd], writes=[rstd])
                c.op("dve", lambda e: e.reciprocal(out=rstd[:, 0:n], in_=rstd[:, 0:n]), reads=[rstd], writes=[rstd])
                c.op("act", lambda e: e.activation(out=rstd[:, 0:n], in_=rstd[:, 0:n], func=AF.Sqrt),
                     reads=[rstd], writes=[rstd])
                for cc in range(8):
                    a = acc[cc]
                    en = "dve" if cc < 5 else "pool"
                    c.op(en, lambda e: e.tensor_tensor(out=a[:, 0:n], in0=a[:, 0:n], in1=mean[:, 0:n], op=ALU.subtract),
                         reads=[a, mean], writes=[a])
                    c.op(en, lambda e: e.tensor_tensor(out=a[:, 0:n], in0=a[:, 0:n], in1=rstd[:, 0:n], op=ALU.mult),
                         reads=[a, rstd], writes=[a])
                    c.op("act", lambda e: e.activation(out=sT[:, cc, 0:n], in_=a[:, 0:n], func=AF.Silu,
                                                       scale=lng[:, cc:cc + 1], bias=lnb[:, cc:cc + 1]),
                         reads=[a, lng, lnb], writes=[sT])
                for i in range(nt):
                    r0 = t0 + i * P
                    xr = xrs[i % 2]
                    xo = xos[i % 2]
                    c.dma("sp", lambda e: e.dma_start(out=xr[:, :], in_=xin_d.ap()[r0:r0 + P, :]), writes=[xr])
                    for hf in range(2):
                        po = psO[hf]
                        for cc in range(8):
                            c.op("pe", lambda e: e.matmul(po[:, :], lhsT=sT[:, cc, i * P:(i + 1) * P],
                                                          rhs=w2b[:, cc, hf * 512:(hf + 1) * 512], start=(cc == 0),
                                                          stop=False), reads=[sT, w2b], writes=[po])
                        c.op("pe", lambda e: e.matmul(po[:, :], lhsT=k.onesb[0:1, :], rhs=b2b[0:1, hf * 512:(hf + 1) * 512],
                                                      start=False, stop=True), reads=[k.onesb, b2b], writes=[po])
                        c.op("dve", lambda e: e.tensor_tensor(out=tmp[:, :], in0=po[:, :],
                                                              in1=modsb[:, 2 * D + hf * 512:2 * D + (hf + 1) * 512],
                                                              op=ALU.mult), reads=[po, modsb], writes=[tmp])
                        c.op("dve", lambda e: e.tensor_tensor(out=xo[:, hf * 512:(hf + 1) * 512], in0=tmp[:, :],
                                                              in1=xr[:, hf * 512:(hf + 1) * 512], op=ALU.add),
                             reads=[tmp, xr], writes=[xo])
                    c.dma("sp", lambda e: e.dma_start(out=xmid_d.ap()[r0 - P:r0, :], in_=xo[:, :]),
                          reads=[xo], writes=[xmid_d], nowaw=True)
            c.op("dve", lambda e: e.tensor_copy(out=uT[:, :, 0:30], in_=uT[:, :, n:n + 30]), reads=[uT], writes=[uT])


def stage_moe(c, k, modsb, xmid_d, w, scr, xout_d, T=TOWN, final_g=None):
    NT = T // P
    NB = (T * 4) // BLK + NE
    KMAX = T // BLK
    hbuf_d, table_d, ysl_d = scr["hbuf"], scr["table"], scr["ysl"]
    with c.scope():
        maskall = c.sbuf([P, NT, NE], F32)
        gall = c.sbuf([P, NT, NE], F32)
        s4i = c.sbuf([P, NT, 4], I32)
        ebi = c.sbuf([P, NB], I32)
        widx = c.sbuf([P, NB, 8], I32)
        bidx = c.sbuf([P, NB], I32)
        with c.scope():
            rw = c.sbuf([P, 8, NE], F32, dma=True)
            rbb = c.sbuf([P, NE], F32, dma=True)
            c.dma("sp", lambda e: e.dma_start(out=rw[:, :, :], in_=w["router_w"].ap().rearrange("(kc p) n -> p kc n", p=P)),
                  writes=[rw])
            c.dma("sp", lambda e: e.dma_start(out=rbb[:, :], in_=w["router_b"].ap().partition_broadcast(P)), writes=[rbb])
            xts = [c.sbuf([P, D], F32, dma=True) for _ in range(2)]
            hq = c.sbuf([P, D], F32)
            hbs = [c.sbuf([P, D], BF16) for _ in range(2)]
            hT32 = c.sbuf([P, 8, P], F32)
            ss = c.sbuf([P, 2], F32)
            lg = c.sbuf([P, NE], F32)
            ex = c.sbuf([P, NE], F32)
            m8 = c.sbuf([P, 8], F32)
            sm = c.sbuf([P, 4], F32)
            pT = c.psum([P, D], F32)
            pl = c.psum([P, NE], F32)
            for i in range(NT):
                xt = xts[i % 2]
                hb = hbs[i % 2]
                c.dma("sp", lambda e: e.dma_start(out=xt[:, :], in_=xmid_d.ap()[i * P:(i + 1) * P, :]),
                      reads=[xmid_d], writes=[xt])
                norm_mod(c, xt, hq, ss, 4 * D, 3 * D, modsb)
                c.op("pool", lambda e: e.tensor_copy(out=hb[:, :], in_=hq[:, :]), reads=[hq], writes=[hb])
                c.dma("sp", lambda e: e.dma_start(out=hbuf_d.ap()[i * P:(i + 1) * P, :], in_=hb[:, :]),
                      reads=[hb], writes=[hbuf_d], nowaw=True)
                for kc in range(8):
                    c.op("pe", lambda e: e.transpose(out=pT[:, kc * P:(kc + 1) * P], in_=hq[:, kc * P:(kc + 1) * P],
                                                     identity=k.identf[:, :]), reads=[hq, k.identf], writes=[pT])
                c.op("act", lambda e: e.activation(out=hT32[:, :, :], in_=pT[:, :].rearrange("p (k t) -> p k t", k=8),
                                                   func=AF.Copy), reads=[pT], writes=[hT32])
                for kc in range(8):
                    c.op("pe", lambda e: e.matmul(pl[:, :], lhsT=hT32[:, kc, :], rhs=rw[:, kc, :], start=(kc == 0),
                                                  stop=(kc == 7)), reads=[hT32, rw], writes=[pl])
                c.op("dve", lambda e: e.tensor_tensor(out=lg[:, :], in0=pl[:, :], in1=rbb[:, :], op=ALU.add),
                     reads=[pl, rbb], writes=[lg])
                c.op("dve", lambda e: e.max(out=m8[:, :], in_=lg[:, :]), reads=[lg], writes=[m8])
                c.op("dve", lambda e: e.tensor_scalar(out=maskall[:, i, :], in0=lg[:, :], scalar1=m8[:, 3:4], scalar2=None,
                                                      op0=ALU.is_ge), reads=[lg, m8], writes=[maskall])
                c.op("dve", lambda e: e.tensor_scalar(out=sm[:, 0:1], in0=m8[:, 0:1], scalar1=-1.0, scalar2=None,
                                                      op0=ALU.mult), reads=[m8], writes=[sm])
                c.op("act", lambda e: e.activation(out=ex[:, :], in_=lg[:, :], func=AF.Exp, bias=sm[:, 0:1]),
                     reads=[lg, sm], writes=[ex])
                c.op("dve", lambda e: e.scalar_tensor_tensor(out=ex[:, :], in0=ex[:, :], scalar=1.0, in1=maskall[:, i, :],
                                                             op0=ALU.mult, op1=ALU.mult, accum_out=sm[:, 1:2]),
                     reads=[ex, maskall], writes=[ex, sm])
                c.op("dve", lambda e: e.reciprocal(out=sm[:, 2:3], in_=sm[:, 1:2]), reads=[sm], writes=[sm])
                c.op("dve", lambda e: e.tensor_scalar(out=gall[:, i, :], in0=ex[:, :], scalar1=sm[:, 2:3], scalar2=None,
                                                      op0=ALU.mult), reads=[ex, sm], writes=[gall])
        with c.scope():
            W = NT * NE
            pos = c.sbuf([P, NT, NE], F32)
            cs = c.sbuf([P, NT, NE], F32)
            base = c.sbuf([P, NT + 1, NE], F32)
            key = c.sbuf([P, NT, NE], F32)
            top = c.sbuf([P, NT, 8], F32)
            g4 = c.sbuf([P, NT, 4], F32)
            src = c.sbuf([P, NT, 4, 2], I32)
            tok = c.sbuf([P, NT], I32)
            cnt = c.sbuf([P, NE], F32)
            nbk = c.sbuf([P, NE], F32)
            tmpe = c.sbuf([P, NE], F32)
            pend = c.sbuf([P, NE], F32)
            pstart = c.sbuf([P, NE], F32)
            thr_i = c.sbuf([P, NB], I32)
            thr = c.sbuf([P, NB], F32)
            eb = c.sbuf([P, NB], F32)
            pk_i = c.sbuf([P, 8], I32)
            pk = c.sbuf([P, 8], F32)
            wf = c.sbuf([P, NB, 8], F32)
            s4f = c.sbuf([P, NT, 4], F32)
            zt = c.sbuf([P, NB * BLK * 2 // P], I32)
            pp = [c.psum([P, 512], F32) for _ in range(2)]
            mflat = maskall[:, :, :].rearrange("p t e -> p (t e)")
            posf = pos[:, :, :].rearrange("p t e -> p (t e)")
            csf = cs[:, :, :].rearrange("p t e -> p (t e)")
            c.op("pool", lambda e: e.memset(zt[:, :], 0), writes=[zt])
            c.dma("sp", lambda e: e.dma_start(out=table_d.ap().rearrange("(p r) w -> p (r w)", p=P), in_=zt[:, :]),
                  reads=[zt], writes=[table_d])
            for j0 in range(0, W, 512):
                n = min(512, W - j0)
                c.op("pe", lambda e: e.matmul(pp[0][:, 0:n], lhsT=k.triu[:, :], rhs=mflat[:, j0:j0 + n], start=True, stop=True),
                     reads=[k.triu, maskall], writes=[pp[0]])
                c.op("pe", lambda e: e.matmul(pp[1][:, 0:n], lhsT=k.onesf[:, :], rhs=mflat[:, j0:j0 + n], start=True, stop=True),
                     reads=[k.onesf, maskall], writes=[pp[1]])
                c.op("dve", lambda e: e.tensor_copy(out=posf[:, j0:j0 + n], in_=pp[0][:, 0:n]), reads=[pp[0]], writes=[pos])
                c.op("act", lambda e: e.activation(out=csf[:, j0:j0 + n], in_=pp[1][:, 0:n], func=AF.Copy),
                     reads=[pp[1]], writes=[cs])
            c.op("dve", lambda e: e.memset(base[:, 0, :], 0.0), writes=[base])
            for i in range(NT):
                c.op("dve", lambda e: e.tensor_tensor(out=base[:, i + 1, :], in0=base[:, i, :], in1=cs[:, i, :], op=ALU.add),
                     reads=[base, cs], writes=[base])
            c.op("dve", lambda e: e.tensor_copy(out=cnt[:, :], in_=base[:, NT, :]), reads=[base], writes=[cnt])
            c.op("dve", lambda e: e.tensor_scalar(out=nbk[:, :], in0=cnt[:, :], scalar1=0.0, scalar2=None, op0=ALU.is_gt),
                 reads=[cnt], writes=[nbk])
            for kk in range(1, KMAX + 1):
                c.op("dve", lambda e: e.tensor_scalar(out=tmpe[:, :], in0=cnt[:, :], scalar1=float(BLK * kk), scalar2=None,
                                                      op0=ALU.is_gt), reads=[cnt], writes=[tmpe])
                c.op("dve", lambda e: e.tensor_tensor(out=nbk[:, :], in0=nbk[:, :], in1=tmpe[:, :], op=ALU.add),
                     reads=[nbk, tmpe], writes=[nbk])
            c.op("dve", lambda e: e.tensor_scalar(out=nbk[:, :], in0=nbk[:, :], scalar1=float(BLK), scalar2=None, op0=ALU.mult),
                 reads=[nbk], writes=[nbk])
            c.op("dve", lambda e: e.tensor_copy(out=pend[:, 0:1], in_=nbk[:, 0:1]), reads=[nbk], writes=[pend])
            for e_ in range(1, NE):
                c.op("dve", lambda e: e.tensor_tensor(out=pend[:, e_:e_ + 1], in0=pend[:, e_ - 1:e_], in1=nbk[:, e_:e_ + 1],
                                                      op=ALU.add), reads=[pend, nbk], writes=[pend])
            c.op("dve", lambda e: e.tensor_tensor(out=pstart[:, :], in0=pend[:, :], in1=nbk[:, :], op=ALU.subtract),
                 reads=[pend, nbk], writes=[pstart])
            c.op("dve", lambda e: e.tensor_tensor(out=pos[:, :, :], in0=pos[:, :, :], in1=base[:, 0:NT, :], op=ALU.add),
                 reads=[pos, base], writes=[pos])
            for i in range(NT):
                c.op("dve", lambda e: e.tensor_tensor(out=pos[:, i, :], in0=pos[:, i, :], in1=pstart[:, :], op=ALU.add),
                     reads=[pos, pstart], writes=[pos])
            c.op("dve", lambda e: e.scalar_tensor_tensor(out=key[:, :, :], in0=pos[:, :, :], scalar=1.0, in1=maskall[:, :, :],
                                                         op0=ALU.add, op1=ALU.mult), reads=[pos, maskall], writes=[key])
            for i in range(NT):
                c.op("dve", lambda e: e.max(out=top[:, i, :], in_=key[:, i, :]), reads=[key], writes=[top])
            c.op("dve", lambda e: e.tensor_scalar(out=s4f[:, :, :], in0=top[:, :, 0:4], scalar1=-1.0, scalar2=None, op0=ALU.add),
                 reads=[top], writes=[s4f])
            c.op("dve", lambda e: e.tensor_copy(out=s4i[:, :, :], in_=s4f[:, :, :]), reads=[s4f], writes=[s4i])
            for i in range(NT):
                for kk in range(4):
                    c.op("dve", lambda e: e.scalar_tensor_tensor(out=tmpe[:, :], in0=key[:, i, :], scalar=top[:, i, kk:kk + 1],
                                                                 in1=gall[:, i, :], op0=ALU.is_equal, op1=ALU.mult,
                                                                 accum_out=g4[:, i, kk:kk + 1]),
                         reads=[key, top, gall], writes=[tmpe, g4])
            c.op("pool", lambda e: e.iota(tok[:, :], pattern=[[P, NT]], base=0, channel_multiplier=1), writes=[tok])
            for kk in range(4):
                c.op("dve", lambda e: e.tensor_copy(out=src[:, :, kk, 0], in_=tok[:, :]), reads=[tok], writes=[src])
            c.op("dve", lambda e: e.tensor_copy(out=src[:, :, :, 1].bitcast(F32), in_=g4[:, :, :]), reads=[g4], writes=[src])
            for i in range(NT):
                for kk in range(4):
                    c.dma("pool", lambda e: e.indirect_dma_start(out=table_d.ap(), out_offset=IOA(ap=s4i[:, i, kk:kk + 1], axis=0),
                                                                 in_=src[:, i, kk, :], in_offset=None),
                          reads=[s4i, src], writes=[table_d], nowaw=(i + kk > 0))
            c.op("pool", lambda e: e.iota(thr_i[:, :], pattern=[[BLK, NB]], base=0, channel_multiplier=0), writes=[thr_i])
            c.op("dve", lambda e: e.tensor_copy(out=thr[:, :], in_=thr_i[:, :]), reads=[thr_i], writes=[thr])
            for b in range(NB):
                c.op("dve", lambda e: e.tensor_scalar(out=tmpe[:, :], in0=pend[:, :], scalar1=thr[:, b:b + 1], scalar2=0.0,
                                                      op0=ALU.is_le, op1=ALU.add, accum_out=eb[:, b:b + 1]),
                     reads=[pend, thr], writes=[tmpe, eb])
            c.op("dve", lambda e: e.tensor_scalar(out=eb[:, :], in0=eb[:, :], scalar1=float(NE - 1), scalar2=None, op0=ALU.min),
                 reads=[eb], writes=[eb])
            c.op("dve", lambda e: e.tensor_copy(out=ebi[:, :], in_=eb[:, :]), reads=[eb], writes=[ebi])
            c.op("pool", lambda e: e.iota(pk_i[:, :], pattern=[[P, 8]], base=0, channel_multiplier=1), writes=[pk_i])
            c.op("dve", lambda e: e.tensor_copy(out=pk[:, :], in_=pk_i[:, :]), reads=[pk_i], writes=[pk])
            for kc in range(8):
                c.op("dve", lambda e: e.tensor_scalar(out=wf[:, :, kc], in0=eb[:, :], scalar1=float(D), scalar2=pk[:, kc:kc + 1],
                                                      op0=ALU.mult, op1=ALU.add), reads=[eb, pk], writes=[wf])
            c.op("dve", lambda e: e.tensor_copy(out=widx[:, :, :], in_=wf[:, :, :]), reads=[wf], writes=[widx])
            c.op("dve", lambda e: e.tensor_scalar(out=eb[:, :], in0=eb[:, :], scalar1=float(P), scalar2=pk[:, 0:1],
                                                  op0=ALU.mult, op1=ALU.add), reads=[eb, pk], writes=[eb])
            c.op("dve", lambda e: e.tensor_copy(out=bidx[:, :], in_=eb[:, :]), reads=[eb], writes=[bidx])
        with c.scope():
            wg = [c.sbuf([P, 8, 2 * D], BF16, dma=True) for _ in range(2)]
            wd = [c.sbuf([P, 8, D], BF16, dma=True) for _ in range(2)]
            bg = [c.sbuf([P, 16], F32, dma=True) for _ in range(2)]
            bd = [c.sbuf([2, D], BF16, dma=True) for _ in range(2)]
            tk = [c.sbuf([P, 8], I32, dma=True) for _ in range(2)]
            xg = [c.sbuf([P, 4, D], BF16, dma=True) for _ in range(2)]
            xgT = c.sbuf([P, 8, BLK], BF16)
            actT = c.sbuf([P, 8, BLK], BF16)
            yo = [c.sbuf([P, 4, D], BF16) for _ in range(2)]
            t1 = [c.sbuf([P, BLK], F32) for _ in range(2)]
            sg = [c.sbuf([P, BLK], F32) for _ in range(2)]
            xl = [c.sbuf([P, BLK], F32) for _ in range(2)]
            bgp = [c.sbuf([P, 8], F32) for _ in range(2)]
            ptr = [c.psum([P, 2 * BLK], BF16) for _ in range(2)]
            psg = [c.psum([P, BLK], F32) for _ in range(2)]
            psl = [c.psum([P, BLK], F32) for _ in range(2)]
            psd = [c.psum([P, 512], F32) for _ in range(2)]
            wguv = w["w_gu"].ap().rearrange("e k n -> (e k) n")
            wdnv = w["w_down"].ap().rearrange("e k n -> (e k) n")
            bguv = w["b_gu_r"].ap().rearrange("e p n -> (e p) n")

            def prefetch(b):
                s = b % 2
                c.dma("sp", lambda e: e.dma_start(out=tk[s][:, :],
                                                  in_=table_d.ap()[b * BLK:(b + 1) * BLK, :].rearrange("(p j) w -> p (j w)", j=4)),
                      reads=[table_d], writes=[tk[s]])
                for j in range(4):
                    c.dma("pool", lambda e: e.indirect_dma_start(out=xg[s][:, j, :], out_offset=None, in_=hbuf_d.ap(),
                                                                 in_offset=IOA(ap=tk[s][:, 2 * j:2 * j + 1], axis=0)),
                          reads=[tk[s], hbuf_d], writes=[xg[s]], nowaw=(j > 0))
                for kc in range(8):
                    c.dma("pool", lambda e: e.indirect_dma_start(out=wg[s][:, kc, :], out_offset=None, in_=wguv,
                                                                 in_offset=IOA(ap=widx[:, b, kc:kc + 1], axis=0)),
                          reads=[widx], writes=[wg[s]], nowaw=(kc > 0))
                for kc in range(8):
                    c.dma("pool", lambda e: e.indirect_dma_start(out=wd[s][:, kc, :], out_offset=None, in_=wdnv,
                                                                 in_offset=IOA(ap=widx[:, b, kc:kc + 1], axis=0)),
                          reads=[widx], writes=[wd[s]], nowaw=(kc > 0))
                c.dma("pool", lambda e: e.indirect_dma_start(out=bg[s][:, :], out_offset=None, in_=bguv,
                                                             in_offset=IOA(ap=bidx[:, b:b + 1], axis=0)),
                      reads=[bidx], writes=[bg[s]])
                c.dma("pool", lambda e: e.indirect_dma_start(out=bd[s][0:2, :], out_offset=None, in_=w["b_down"].ap(),
                                                             in_offset=IOA(ap=ebi[0:2, b:b + 1], axis=0)),
                      reads=[ebi], writes=[bd[s]])

            prefetch(0)
            for b in range(NB):
                s = b % 2
                if b + 1 < NB:
                    prefetch(b + 1)
                for kc in range(8):
                    pt = ptr[(kc // 2) % 2]
                    o = (kc % 2) * BLK
                    for j in range(4):
                        c.op("pe", lambda e: e.transpose(out=pt[:, o + j * P:o + (j + 1) * P], in_=xg[s][:, j, kc * P:(kc + 1) * P],
                                                         identity=k.identb[:, :]), reads=[xg[s], k.identb], writes=[pt])
                    if kc % 2 == 1:
                        c.op("act", lambda e: e.activation(out=xgT[:, kc - 1:kc + 1, :],
                                                           in_=pt[:, :].rearrange("p (k t) -> p k t", k=2), func=AF.Copy),
                             reads=[pt], writes=[xgT])
                c.op("dve", lambda e: e.tensor_scalar(out=bgp[s][:, :], in0=bg[s][:, 8:16], scalar1=1.0, scalar2=None, op0=ALU.add),
                     reads=[bg[s]], writes=[bgp[s]])
                for fc in range(8):
                    q = fc % 2
                    for kc in range(8):
                        c.op("pe", lambda e: e.matmul(psg[q][:, :], lhsT=wg[s][:, kc, fc * P:(fc + 1) * P], rhs=xgT[:, kc, :],
                                                      start=(kc == 0), stop=(kc == 7)), reads=[wg[s], xgT], writes=[psg[q]])
                    for kc in range(8):
                        c.op("pe", lambda e: e.matmul(psl[q][:, :], lhsT=wg[s][:, kc, D + fc * P:D + (fc + 1) * P], rhs=xgT[:, kc, :],
                                                      start=(kc == 0), stop=(kc == 7)), reads=[wg[s], xgT], writes=[psl[q]])
                    c.op("dve", lambda e: e.tensor_scalar(out=t1[q][:, :], in0=psg[q][:, :], scalar1=bg[s][:, fc:fc + 1], scalar2=7.0,
                                                          op0=ALU.add, op1=ALU.min), reads=[psg[q], bg[s]], writes=[t1[q]])
                    c.op("act", lambda e: e.activation(out=sg[q][:, :], in_=t1[q][:, :], func=AF.Sigmoid, scale=1.702),
                         reads=[t1[q]], writes=[sg[q]])
                    c.op("dve", lambda e: e.tensor_scalar(out=xl[q][:, :], in0=psl[q][:, :], scalar1=bgp[s][:, fc:fc + 1], scalar2=-6.0,
                                                          op0=ALU.add, op1=ALU.max), reads=[psl[q], bgp[s]], writes=[xl[q]])
                    c.op("pool", lambda e: e.tensor_tensor(out=t1[q][:, :], in0=t1[q][:, :], in1=sg[q][:, :], op=ALU.mult),
                         reads=[t1[q], sg[q]], writes=[t1[q]])
                    c.op("dve", lambda e: e.scalar_tensor_tensor(out=actT[:, fc, :], in0=xl[q][:, :], scalar=8.0, in1=t1[q][:, :],
                                                                  op0=ALU.min, op1=ALU.mult), reads=[xl[q], t1[q]], writes=[actT])
                for j in range(4):
                    for hf in range(2):
                        pd = psd[hf]
                        for fc in range(8):
                            c.op("pe", lambda e: e.matmul(pd[:, :], lhsT=actT[:, fc, j * P:(j + 1) * P],
                                                          rhs=wd[s][:, fc, hf * 512:(hf + 1) * 512], start=(fc == 0), stop=False),
                                 reads=[actT, wd[s]], writes=[pd])
                        c.op("pe", lambda e: e.matmul(pd[:, :], lhsT=k.onesb[0:1, :], rhs=bd[s][0:1, hf * 512:(hf + 1) * 512],
                                                      start=False, stop=True), reads=[k.onesb, bd[s]], writes=[pd])
                        c.op("act", lambda e: e.activation(out=yo[s][:, j, hf * 512:(hf + 1) * 512], in_=pd[:, :], func=AF.Copy,
                                                           scale=tk[s][:, 2 * j + 1:2 * j + 2].bitcast(F32)),
                             reads=[pd, tk[s]], writes=[yo[s]])
                c.dma("sp", lambda e: e.dma_start(out=ysl_d.ap()[b * BLK:(b + 1) * BLK, :].rearrange("(p j) d -> p j d", j=4),
                                                  in_=yo[s][:, :, :]), reads=[yo[s]], writes=[ysl_d], nowaw=True)
        with c.scope():
            yg = [c.sbuf([P, 4, D], BF16, dma=True) for _ in range(2)]
            xts = [c.sbuf([P, D], F32, dma=True) for _ in range(2)]
            a0 = c.sbuf([P, D], F32)
            a1 = c.sbuf([P, D], F32)
            xo = [c.sbuf([P, D], F32) for _ in range(2)]
            ss = c.sbuf([P, 2], F32)
            if final_g is not None:
                fg = c.sbuf([P, D], F32, dma=True)
                c.dma("sp", lambda e: e.dma_start(out=fg[:, :], in_=final_g.ap().partition_broadcast(P)), writes=[fg])
            for i in range(NT):
                s = i % 2
                for kk in range(4):
                    c.dma("pool", lambda e: e.indirect_dma_start(out=yg[s][:, kk, :], out_offset=None, in_=ysl_d.ap(),
                                                                 in_offset=IOA(ap=s4i[:, i, kk:kk + 1], axis=0)),
                          reads=[s4i, ysl_d], writes=[yg[s]], nowaw=(kk > 0))
                c.dma("sp", lambda e: e.dma_start(out=xts[s][:, :], in_=xmid_d.ap()[i * P:(i + 1) * P, :]),
                      reads=[xmid_d], writes=[xts[s]])
                c.op("dve", lambda e: e.tensor_tensor(out=a0[:, :], in0=yg[s][:, 0, :], in1=yg[s][:, 1, :], op=ALU.add),
                     reads=[yg[s]], writes=[a0])
                c.op("pool", lambda e: e.tensor_tensor(out=a1[:, :], in0=yg[s][:, 2, :], in1=yg[s][:, 3, :], op=ALU.add),
                     reads=[yg[s]], writes=[a1])
                c.op("dve", lambda e: e.tensor_tensor(out=a0[:, :], in0=a0[:, :], in1=a1[:, :], op=ALU.add),
                     reads=[a0, a1], writes=[a0])
                c.op("dve", lambda e: e.tensor_tensor(out=a0[:, :], in0=a0[:, :], in1=modsb[:, 5 * D:6 * D], op=ALU.mult),
                     reads=[a0, modsb], writes=[a0])
                c.op("dve", lambda e: e.tensor_tensor(out=xo[s][:, :], in0=a0[:, :], in1=xts[s][:, :], op=ALU.add),
                     reads=[a0, xts[s]], writes=[xo[s]])
                if final_g is not None:
                    rms_rstd(c, xo[s], a1, ss)
                    c.op("dve", lambda e: e.scalar_tensor_tensor(out=xo[s][:, :], in0=xo[s][:, :], scalar=ss[:, 1:2], in1=fg[:, :],
                                                                 op0=ALU.mult, op1=ALU.mult), reads=[xo[s], ss, fg], writes=[xo[s]])
                c.dma("sp", lambda e: e.dma_start(out=xout_d.ap()[i * P:(i + 1) * P, :], in_=xo[s][:, :]),
                      reads=[xo[s]], writes=[xout_d], nowaw=True)


def moe_scratch(c, T, tag=""):
    NB = (T * 4) // BLK + NE
    return dict(hbuf=c.dram("hbuf" + tag, [T, D], BF16), table=c.dram("table" + tag, [NB * BLK, 2], I32),
                ysl=c.dram("ysl" + tag, [NB * BLK, D], BF16))


MOE_W = dict(router_w=([D, NE], F32), router_b=([1, NE], F32), w_gu=([NE, D, 2 * D], F32), b_gu_r=([NE, P, 16], F32),
             w_down=([NE, D, D], F32), b_down=([NE, D], F32))
CONV_W = dict(pw1=([D, 2 * D], F32), b_pw1=([P, 16], F32), w_dw=([P, 8, 31], F32), b_dw=([P, 8], F32), ln_g=([P, 8], F32),
              ln_b=([P, 8], F32), pw2=([D, D], F32), b_pw2=([1, D], F32))
MOD_W = dict(c_r=([P, 8], F32), mod_w=([D, 6 * D], F32), mod_b=([1, 6 * D], F32), norm_g=([2, D], F32))


def declare(c, specs, kind="ExternalInput", prefix=""):
    return {n: c.dram(prefix + n, shp, dt, kind=kind) for n, (shp, dt) in specs.items()}


def build_conv_layer():
    nc = bass.Bass("TRN2", target_bir_lowering=False)
    c = Ctx(nc)
    cd = declare(c, CONST_SPECS)
    mw = declare(c, MOD_W)
    cw = declare(c, CONV_W)
    ew = declare(c, MOE_W)
    xin = c.dram("xin", [P + TOWN, D], F32, kind="ExternalInput")
    flag = c.dram("flag", [P, 1], F32, kind="ExternalInput")
    xout = c.dram("xout", [TOWN, D], F32, kind="ExternalOutput")
    xmid = c.dram("xmid", [TOWN, D], F32)
    scr = moe_scratch(c, TOWN)
    k = load_consts(c, cd)
    modsb = c.sbuf([P, 6 * D], F32)
    stage_mods(c, k, mw["c_r"], mw["mod_w"], mw["mod_b"], mw["norm_g"], modsb)
    stage_conv(c, k, modsb, xin, flag, cw, xmid)
    stage_moe(c, k, modsb, xmid, ew, scr, xout)
    c.finish([xout])
    c.close()
    return nc


def pcol(v, n):
    return np.ascontiguousarray(np.asarray(v).reshape(n, P).T)


def mod_inputs(inp, l, b):
    return dict(c_r=pcol(inp["c"][b], 8), mod_w=inp["mod_w"][l], mod_b=inp["mod_b"][l][None, :],
                norm_g=np.stack([inp["norm1_g"][l], inp["norm2_g"][l]]))


def moe_inputs(inp, l):
    bgu = inp["moe_b_gu"][l]
    return dict(router_w=inp["moe_router_w"][l], router_b=inp["moe_router_b"][l][None, :], w_gu=inp["moe_w_gu"][l],
                b_gu_r=np.ascontiguousarray(bgu.reshape(NE, 16, P).transpose(0, 2, 1)), w_down=inp["moe_w_down"][l],
                b_down=inp["moe_b_down"][l])


def conv_inputs(inp, l):
    return dict(pw1=inp["conv_w_pw1"][l], b_pw1=pcol(inp["conv_b_pw1"][l], 16),
                w_dw=np.ascontiguousarray(inp["conv_w_dw"][l].reshape(31, 8, P).transpose(2, 1, 0)),
                b_dw=pcol(inp["conv_b_dw"][l], 8), ln_g=pcol(inp["conv_ln_g"][l], 8), ln_b=pcol(inp["conv_ln_b"][l], 8),
                pw2=inp["conv_w_pw2"][l], b_pw2=inp["conv_b_pw2"][l][None, :])


def head_rms(c, src, dst, sq, ssq, gbc, scale):
    for hh in range(16):
        c.op("dve", lambda e: e.scalar_tensor_tensor(out=sq[:, hh * 64:(hh + 1) * 64], in0=src[:, hh * 64:(hh + 1) * 64], scalar=1.0,
                                                     in1=src[:, hh * 64:(hh + 1) * 64], op0=ALU.mult, op1=ALU.mult,
                                                     accum_out=ssq[:, hh:hh + 1]), reads=[src], writes=[sq, ssq])
    c.op("dve", lambda e: e.tensor_scalar(out=ssq[:, 0:16], in0=ssq[:, 0:16], scalar1=1.0 / 64, scalar2=EPS, op0=ALU.mult,
                                          op1=ALU.add), reads=[ssq], writes=[ssq])
    c.op("dve", lambda e: e.reciprocal(out=ssq[:, 0:16], in_=ssq[:, 0:16]), reads=[ssq], writes=[ssq])
    c.op("act", lambda e: e.activation(out=ssq[:, 0:16], in_=ssq[:, 0:16], func=AF.Sqrt, scale=scale * scale),
         reads=[ssq], writes=[ssq])
    for hh in range(16):
        c.op("dve", lambda e: e.scalar_tensor_tensor(out=dst[:, hh * 64:(hh + 1) * 64], in0=src[:, hh * 64:(hh + 1) * 64],
                                                     scalar=ssq[:, hh:hh + 1], in1=gbc[:, 0:64], op0=ALU.mult, op1=ALU.mult),
             reads=[src, ssq, gbc], writes=[dst])


def pair_transpose_store(c, k, srcb, ptb, stg, dst_d, tok0, rows=64):
    for kc in range(8):
        c.op("pe", lambda e: e.transpose(out=ptb[:, kc * P:(kc + 1) * P], in_=srcb[:, kc * P:(kc + 1) * P], identity=k.identb[:, :]),
             reads=[srcb, k.identb], writes=[ptb])
    c.op("act", lambda e: e.activation(out=stg[:, :, :], in_=ptb[:, :].rearrange("p (k t) -> p k t", k=8), func=AF.Copy),
         reads=[ptb], writes=[stg])
    for kc in range(8):
        for half in range(2):
            c.dma("sp", lambda e: e.dma_start(out=dst_d.ap()[2 * kc + half, 0:64, tok0:tok0 + P], in_=stg[half * 64:(half + 1) * 64, kc, :]),
                  reads=[stg], writes=[dst_d], nowaw=True)


KV_W = dict(c_r=([P, 8], F32), mod_w=([D, 2 * D], F32), mod_b=([1, 2 * D], F32), norm_g=([1, D], F32),
            w_kvf=([D, 2 * D + 16], F32), b_f=([1, 16], F32), k_g=([1, 64], F32))


def build_kv():
    nc = bass.Bass("TRN2", target_bir_lowering=False)
    c = Ctx(nc)
    cd = declare(c, CONST_SPECS)
    w = declare(c, KV_W)
    xin = c.dram("xin", [TOWN, D], F32, kind="ExternalInput")
    kT_d = c.dram("kT", [16, 64, TOWN], BF16, kind="ExternalOutput")
    v_d = c.dram("v", [TOWN, 16, 72], BF16, kind="ExternalOutput")
    lf_d = c.dram("logf", [TOWN, 16], F32, kind="ExternalOutput")
    k = load_consts(c, cd)
    modsb = c.sbuf([P, 2 * D], F32)
    stage_mods(c, k, w["c_r"], w["mod_w"], w["mod_b"], w["norm_g"], modsb, ncols=2 * D, nnorm=1)
    with c.scope():
        wkv = c.sbuf([P, 8, 2 * D], BF16, dma=True)
        wf = c.sbuf([P, 8, 16], F32, dma=True)
        bfb = c.sbuf([P, 16], F32, dma=True)
        kgb = c.sbuf([P, 64], F32, dma=True)
        wv = w["w_kvf"].ap().rearrange("(kc p) n -> p kc n", p=P)
        for kc in range(8):
            c.dma("pool", lambda e: e.dma_start(out=wkv[:, kc, :], in_=wv[:, kc, 0:2 * D]), writes=[wkv], nowaw=True)
        c.dma("sp", lambda e: e.dma_start(out=wf[:, :, :], in_=wv[:, :, 2 * D:2 * D + 16]), writes=[wf])
        c.dma("sp", lambda e: e.dma_start(out=bfb[:, :], in_=w["b_f"].ap().partition_broadcast(P)), writes=[bfb])
        c.dma("sp", lambda e: e.dma_start(out=kgb[:, :], in_=w["k_g"].ap().partition_broadcast(P)), writes=[kgb])
        xts = [c.sbuf([P, D], F32, dma=True) for _ in range(2)]
        h = c.sbuf([P, D], F32)
        ss = c.sbuf([P, 2], F32)
        hT32 = c.sbuf([P, 8, P], F32)
        hTb = c.sbuf([P, 8, P], BF16)
        ksb = c.sbuf([P, D], F32)
        sq = c.sbuf([P, D], F32)
        ssq = c.sbuf([P, 16], F32)
        kn = c.sbuf([P, D], BF16)
        stg = [c.sbuf([P, 8, P], BF16) for _ in range(2)]
        vt = [c.sbuf([P, 16, 72], BF16) for _ in range(2)]
        fz = [c.sbuf([P, 16], F32) for _ in range(2)]
        pT = c.psum([P, D], F32)
        ptb = c.psum([P, D], BF16)
        pk = [c.psum([P, 512], F32) for _ in range(2)]
        pf = c.psum([P, 16], F32)
        for t in vt:
            c.op("dve", lambda e: e.memset(t[:, :, :], 1.0), writes=[t])
        LV = int(os.environ.get("KVSTOP", "9"))
        for i in range(int(os.environ.get("KVTILES", TOWN // P)) if LV > 2 else 0):
            xt = xts[i % 2]
            c.dma("sp", lambda e: e.dma_start(out=xt[:, :], in_=xin.ap()[i * P:(i + 1) * P, :]), writes=[xt])
            norm_mod(c, xt, h, ss, 1 * D, 0, modsb)
            if LV == 3:
                continue
            for kc in range(8):
                c.op("pe", lambda e: e.transpose(out=pT[:, kc * P:(kc + 1) * P], in_=h[:, kc * P:(kc + 1) * P], identity=k.identf[:, :]),
                     reads=[h, k.identf], writes=[pT])
            c.op("act", lambda e: e.activation(out=hT32[:, :, :], in_=pT[:, :].rearrange("p (k t) -> p k t", k=8), func=AF.Copy),
                 reads=[pT], writes=[hT32])
            c.op("dve", lambda e: e.tensor_copy(out=hTb[:, :, :], in_=hT32[:, :, :]), reads=[hT32], writes=[hTb])
            if LV == 4:
                continue
            for hf in range(2):
                for kc in range(8):
                    c.op("pe", lambda e: e.matmul(pk[hf][:, :], lhsT=hTb[:, kc, :], rhs=wkv[:, kc, hf * 512:(hf + 1) * 512],
                                                  start=(kc == 0), stop=(kc == 7)), reads=[hTb, wkv], writes=[pk[hf]])
                c.op("act", lambda e: e.activation(out=ksb[:, hf * 512:(hf + 1) * 512], in_=pk[hf][:, :], func=AF.Copy),
                     reads=[pk[hf]], writes=[ksb])
            if LV == 5:
                continue
            SK = os.environ.get("KVSKIP", "")
            if "k" not in SK:
                head_rms(c, ksb, kn, sq, ssq, kgb, 1.0)
            if "t" not in SK:
                pair_transpose_store(c, k, kn, ptb, stg[i % 2], kT_d, i * P)
            v = vt[i % 2]
            for hf in range(2 if "v" not in SK else 0):
                for kc in range(8):
                    c.op("pe", lambda e: e.matmul(pk[hf][:, :], lhsT=hTb[:, kc, :], rhs=wkv[:, kc, D + hf * 512:D + (hf + 1) * 512],
                                                  start=(kc == 0), stop=(kc == 7)), reads=[hTb, wkv], writes=[pk[hf]])
                c.op("act", lambda e: e.activation(out=v[:, hf * 8:(hf + 1) * 8, 0:64],
                                                   in_=pk[hf][:, :].rearrange("p (h d) -> p h d", d=64), func=AF.Copy),
                     reads=[pk[hf]], writes=[v])
            c.dma("sp", lambda e: e.dma_start(out=v_d.ap()[i * P:(i + 1) * P, :, :], in_=v[:, :, :]), reads=[v], writes=[v_d], nowaw=True)
            if "f" in SK:
                continue
            f = fz[i % 2]
            for kc in range(8):
                c.op("pe", lambda e: e.matmul(pf[:, :], lhsT=hT32[:, kc, :], rhs=wf[:, kc, :], start=(kc == 0), stop=(kc == 7)),
                     reads=[hT32, wf], writes=[pf])
            c.op("dve", lambda e: e.tensor_tensor(out=f[:, :], in0=pf[:, :], in1=bfb[:, :], op=ALU.add), reads=[pf, bfb], writes=[f])
            c.op("act", lambda e: e.activation(out=f[:, :], in_=f[:, :], func=AF.Exp, scale=-1.0), reads=[f], writes=[f])
            c.op("act", lambda e: e.activation(out=f[:, :], in_=f[:, :], func=AF.Ln, bias=1.0), reads=[f], writes=[f])
            c.op("dve", lambda e: e.tensor_scalar(out=f[:, :], in0=f[:, :], scalar1=-1.0, scalar2=None, op0=ALU.mult), reads=[f], writes=[f])
            c.dma("sp", lambda e: e.dma_start(out=lf_d.ap()[i * P:(i + 1) * P, :], in_=f[:, :]), reads=[f], writes=[lf_d], nowaw=True)
    c.finish([])
    c.close()
    return nc


def build_fcum():
    NTL = SEQ // P
    nc = bass.Bass("TRN2", target_bir_lowering=False)
    c = Ctx(nc)
    cd = declare(c, CONST_SPECS)
    lf_d = c.dram("logf", [SEQ, 16], F32, kind="ExternalInput")
    ft_d = c.dram("ft", [16, 3, SEQ], BF16, kind="ExternalOutput")
    fn_d = c.dram("ftn", [16, 3, SEQ], BF16, kind="ExternalOutput")
    k = load_consts(c, cd)
    with c.scope():
        lf = c.sbuf([P, NTL, 16], F32, dma=True)
        F = c.sbuf([16, SEQ], F32)
        R = c.sbuf([16, 4096], F32)
        base = c.sbuf([16, NTL + 1], F32)
        sp3s = [c.sbuf([16, 3, 4096], BF16) for _ in range(2)]
        ps = [c.psum([16, 512], F32) for _ in range(2)]
        c.dma("sp", lambda e: e.dma_start(out=lf[:, :, :], in_=lf_d.ap().rearrange("(n p) h -> p n h", p=P)), writes=[lf])
        for g in range(NTL // 4):
            pg = ps[g % 2]
            for q in range(4):
                n = g * 4 + q
                c.op("pe", lambda e: e.matmul(pg[:, q * P:(q + 1) * P], lhsT=lf[:, n, :], rhs=k.tril[:, :], start=True, stop=True),
                     reads=[lf, k.tril], writes=[pg])
            c.op("act", lambda e: e.activation(out=F[:, g * 512:(g + 1) * 512], in_=pg[:, :], func=AF.Copy), reads=[pg], writes=[F])
        c.op("dve", lambda e: e.memset(base[:, 0:1], 0.0), writes=[base])
        for n in range(NTL):
            c.op("dve", lambda e: e.tensor_tensor(out=base[:, n + 1:n + 2], in0=base[:, n:n + 1], in1=F[:, n * P + P - 1:n * P + P],
                                                  op=ALU.add), reads=[base, F], writes=[base])
        for n in range(1, NTL):
            c.op("dve", lambda e: e.tensor_scalar(out=F[:, n * P:(n + 1) * P], in0=F[:, n * P:(n + 1) * P], scalar1=base[:, n:n + 1],
                                                  scalar2=None, op0=ALU.add), reads=[F, base], writes=[F])
        CH = 4096
        for ci in range(SEQ // CH):
            Fc = F[:, ci * CH:(ci + 1) * CH]
            sp = sp3s[ci % 2]
            c.op("dve", lambda e: e.tensor_copy(out=sp[:, 0, :], in_=Fc), reads=[F], writes=[sp])
            c.op("dve", lambda e: e.tensor_tensor(out=R[:, :], in0=Fc, in1=sp[:, 0, :], op=ALU.subtract), reads=[F, sp], writes=[R])
            c.op("dve", lambda e: e.tensor_copy(out=sp[:, 1, :], in_=R[:, :]), reads=[R], writes=[sp])
            c.op("dve", lambda e: e.tensor_tensor(out=R[:, :], in0=R[:, :], in1=sp[:, 1, :], op=ALU.subtract), reads=[R, sp], writes=[R])
            c.op("dve", lambda e: e.tensor_copy(out=sp[:, 2, :], in_=R[:, :]), reads=[R], writes=[sp])
            c.dma("sp", lambda e: e.dma_start(out=ft_d.ap()[:, :, ci * CH:(ci + 1) * CH], in_=sp[:, :, :]), reads=[sp], writes=[ft_d], nowaw=True)
            for q in range(3):
                c.op("dve", lambda e: e.tensor_scalar(out=sp[:, q, :], in0=sp[:, q, :], scalar1=-1.0, scalar2=None, op0=ALU.mult),
                     reads=[sp], writes=[sp])
            c.dma("sp", lambda e: e.dma_start(out=fn_d.ap()[:, :, ci * CH:(ci + 1) * CH], in_=sp[:, :, :]), reads=[sp], writes=[fn_d], nowaw=True)
    c.finish([])
    c.close()
    return nc


ATT_W = dict(w_qg=([D, 2 * D], F32), q_g=([1, 64], F32), w_o=([D, D], F32))
NQB = TOWN // 512


def stage_attn(c, k, modsb, xin_d, w, kf_d, v_d, qf_d, mask_d, xmid_d):
    qT_d = c.dram("qT_s", [16, 70, TOWN], BF16)
    sg_d = c.dram("sgT_s", [16, 64, TOWN], BF16)
    og_d = c.dram("ogT_s", [16, 64, TOWN], BF16)
    c.dma("sp", lambda e: e.dma_start(out=qT_d.ap()[:, 64:70, :], in_=qf_d.ap()), writes=[qT_d])
    with c.scope():
        wq = c.sbuf([P, 8, 2 * D], BF16, dma=True)
        qgb = c.sbuf([P, 64], F32, dma=True)
        wv = w["w_qg"].ap().rearrange("(kc p) n -> p kc n", p=P)
        for kc in range(8):
            c.dma("pool", lambda e: e.dma_start(out=wq[:, kc, :], in_=wv[:, kc, :]), writes=[wq], nowaw=True)
        c.dma("sp", lambda e: e.dma_start(out=qgb[:, :], in_=w["q_g"].ap().partition_broadcast(P)), writes=[qgb])
        xts = [c.sbuf([P, D], F32, dma=True) for _ in range(2)]
        h = c.sbuf([P, D], F32)
        ss = c.sbuf([P, 2], F32)
        hTb = c.sbuf([P, 8, P], BF16)
        qsb = c.sbuf([P, D], F32)
        sq = c.sbuf([P, D], F32)
        ssq = c.sbuf([P, 16], F32)
        qn = c.sbuf([P, D], BF16)
        sgn = c.sbuf([P, D], BF16)
        stg = [c.sbuf([P, 8, P], BF16) for _ in range(4)]
        pT = c.psum([P, D], F32)
        ptb = c.psum([P, D], BF16)
        pk = [c.psum([P, 512], F32) for _ in range(2)]
        for i in range(TOWN // P):
            xt = xts[i % 2]
            c.dma("sp", lambda e: e.dma_start(out=xt[:, :], in_=xin_d.ap()[i * P:(i + 1) * P, :]), writes=[xt])
            norm_mod(c, xt, h, ss, 1 * D, 0, modsb)
            for kc in range(8):
                c.op("pe", lambda e: e.transpose(out=pT[:, kc * P:(kc + 1) * P], in_=h[:, kc * P:(kc + 1) * P], identity=k.identf[:, :]),
                     reads=[h, k.identf], writes=[pT])
            c.op("act", lambda e: e.activation(out=hTb[:, :, :], in_=pT[:, :].rearrange("p (k t) -> p k t", k=8), func=AF.Copy),
                 reads=[pT], writes=[hTb])
            for hf in range(2):
                for kc in range(8):
                    c.op("pe", lambda e: e.matmul(pk[hf][:, :], lhsT=hTb[:, kc, :], rhs=wq[:, kc, hf * 512:(hf + 1) * 512],
                                                  start=(kc == 0), stop=(kc == 7)), reads=[hTb, wq], writes=[pk[hf]])
                c.op("act", lambda e: e.activation(out=qsb[:, hf * 512:(hf + 1) * 512], in_=pk[hf][:, :], func=AF.Copy),
                     reads=[pk[hf]], writes=[qsb])
            head_rms(c, qsb, qn, sq, ssq, qgb, 0.125)
            pair_transpose_store(c, k, qn, ptb, stg[(2 * i) % 4], qT_d, i * P)
            for hf in range(2):
                for kc in range(8):
                    c.op("pe", lambda e: e.matmul(pk[hf][:, :], lhsT=hTb[:, kc, :], rhs=wq[:, kc, D + hf * 512:D + (hf + 1) * 512],
                                                  start=(kc == 0), stop=(kc == 7)), reads=[hTb, wq], writes=[pk[hf]])
                c.op("act", lambda e: e.activation(out=sgn[:, hf * 512:(hf + 1) * 512], in_=pk[hf][:, :], func=AF.Sigmoid),
                     reads=[pk[hf]], writes=[sgn])
            pair_transpose_store(c, k, sgn, ptb, stg[(2 * i + 1) % 4], sg_d, i * P)
    with c.scope():
        NKT = SEQ // P
        kf = [c.sbuf([70, SEQ], BF16, dma=True) for _ in range(2)]
        vv = [c.sbuf([P, NKT, 72], BF16, dma=True) for _ in range(2)]
        qq = [c.sbuf([70, TOWN], BF16, dma=True) for _ in range(2)]
        sgh = [c.sbuf([64, TOWN], BF16, dma=True) for _ in range(2)]
        mk = c.sbuf([P, 16, 512], BF16, dma=True)
        ssb = [c.sbuf([P, 512], F32) for _ in range(2)]
        pTb = [c.sbuf([P, 512], BF16) for _ in range(3)]
        rinv = c.sbuf([P, 512], F32)
        bcs = c.sbuf([64, 512], F32)
        on = c.sbuf([64, 512], F32)
        og = [c.sbuf([64, 512], BF16) for _ in range(2)]
        pS = [c.psum([P, 512], F32) for _ in range(3)]
        pO = [c.psum([P, 512], F32) for _ in range(2)]
        pB = c.psum([64, 512], F32)
        c.dma("sp", lambda e: e.dma_start(out=mk[:, :, :], in_=mask_d.ap()), writes=[mk])

        def load_head(hh):
            s = hh % 2
            c.dma("sp", lambda e: e.dma_start(out=kf[s][:, :], in_=kf_d.ap()[hh]), writes=[kf[s]])
            c.dma("sp", lambda e: e.dma_start(out=vv[s][:, :, :], in_=v_d.ap()[hh]), writes=[vv[s]])
            c.dma("sp", lambda e: e.dma_start(out=qq[s][:, :], in_=qT_d.ap()[hh]), reads=[qT_d], writes=[qq[s]])
            c.dma("sp", lambda e: e.dma_start(out=sgh[s][:, :], in_=sg_d.ap()[hh]), reads=[sg_d], writes=[sgh[s]])

        load_head(0)
        u = 0
        nq = 0
        NH_RUN = int(os.environ.get("ATT_HEADS", "16"))
        for hh in range(NH_RUN):
            s = hh % 2
            if hh + 1 < NH_RUN:
                load_head(hh + 1)
            for j in range(NQB):
                po = pO[nq % 2]
                nkt = 16 * (j + 1)
                for kt in range(nkt):
                    ps_ = pS[u % 3]
                    pt_ = pTb[u % 3]
                    c.op("pe", lambda e: e.matmul(ps_[:, :], lhsT=kf[s][:, kt * P:(kt + 1) * P], rhs=qq[s][:, j * 512:(j + 1) * 512],
                                                  start=True, stop=True), reads=[kf[s], qq[s]], writes=[ps_])
                    if kt >= 16 * j:
                        sb_ = ssb[u % 2]
                        c.op("dve", lambda e: e.tensor_tensor(out=sb_[:, :], in0=ps_[:, :], in1=mk[:, kt - 16 * j, :], op=ALU.add),
                             reads=[ps_, mk], writes=[sb_])
                        c.op("act", lambda e: e.activation(out=pt_[:, :], in_=sb_[:, :], func=AF.Exp), reads=[sb_], writes=[pt_])
                    else:
                        c.op("act", lambda e: e.activation(out=pt_[:, :], in_=ps_[:, :], func=AF.Exp), reads=[ps_], writes=[pt_])
                    c.op("pe", lambda e: e.matmul(po[0:65, :], lhsT=vv[s][:, kt, 0:65], rhs=pt_[:, :], start=(kt == 0), stop=(kt == nkt - 1)),
                         reads=[vv[s], pt_], writes=[po])
                    u += 1
                o_ = og[nq % 2]
                c.op("dve", lambda e: e.reciprocal(out=rinv[64:65, :], in_=po[64:65, :]), reads=[po], writes=[rinv])
                c.op("pe", lambda e: e.matmul(pB[:, :], lhsT=k.onesf[64:65, 0:64], rhs=rinv[64:65, :], start=True, stop=True),
                     reads=[k.onesf, rinv], writes=[pB])
                c.op("act", lambda e: e.activation(out=bcs[:, :], in_=pB[:, :], func=AF.Copy), reads=[pB], writes=[bcs])
                c.op("dve", lambda e: e.tensor_tensor(out=on[:, :], in0=po[0:64, :], in1=bcs[:, :], op=ALU.mult), reads=[po, bcs], writes=[on])
                c.op("pool", lambda e: e.tensor_tensor(out=o_[:, :], in0=on[:, :], in1=sgh[s][:, j * 512:(j + 1) * 512], op=ALU.mult),
                     reads=[on, sgh[s]], writes=[o_])
                c.dma("sp", lambda e: e.dma_start(out=og_d.ap()[hh, :, j * 512:(j + 1) * 512], in_=o_[:, :]), reads=[o_], writes=[og_d], nowaw=True)
                nq += 1
    with c.scope():
        wo = c.sbuf([64, 16, D], BF16, dma=True)
        c.dma("pool", lambda e: e.dma_start(out=wo[:, :, :], in_=w["w_o"].ap().rearrange("(h d) n -> d h n", d=64)), writes=[wo])
        ogt = [c.sbuf([64, 16, P], BF16, dma=True) for _ in range(2)]
        xrs = [c.sbuf([P, D], F32, dma=True) for _ in range(2)]
        xos = [c.sbuf([P, D], F32) for _ in range(2)]
        tmp = c.sbuf([P, 512], F32)
        py = [c.psum([P, 512], F32) for _ in range(2)]
        for i in range(TOWN // P):
            o_ = ogt[i % 2]
            xr = xrs[i % 2]
            xo = xos[i % 2]
            c.dma("sp", lambda e: e.dma_start(out=o_[:, :, :], in_=og_d.ap()[:, :, i * P:(i + 1) * P].rearrange("h d t -> d h t")),
                  reads=[og_d], writes=[o_])
            c.dma("sp", lambda e: e.dma_start(out=xr[:, :], in_=xin_d.ap()[i * P:(i + 1) * P, :]), writes=[xr])
            for hf in range(2):
                for hh in range(16):
                    c.op("pe", lambda e: e.matmul(py[hf][:, :], lhsT=o_[:, hh, :], rhs=wo[:, hh, hf * 512:(hf + 1) * 512],
                                                  start=(hh == 0), stop=(hh == 15)), reads=[o_, wo], writes=[py[hf]])
                c.op("dve", lambda e: e.tensor_tensor(out=tmp[:, :], in0=py[hf][:, :], in1=modsb[:, 2 * D + hf * 512:2 * D + (hf + 1) * 512],
                                                      op=ALU.mult), reads=[py[hf], modsb], writes=[tmp])
                c.op("dve", lambda e: e.tensor_tensor(out=xo[:, hf * 512:(hf + 1) * 512], in0=tmp[:, :], in1=xr[:, hf * 512:(hf + 1) * 512],
                                                      op=ALU.add), reads=[tmp, xr], writes=[xo])
            c.dma("sp", lambda e: e.dma_start(out=xmid_d.ap()[i * P:(i + 1) * P, :], in_=xo[:, :]), reads=[xo], writes=[xmid_d], nowaw=True)


def build_attn_layer(final):
    nc = bass.Bass("TRN2", target_bir_lowering=False)
    c = Ctx(nc)
    cd = declare(c, CONST_SPECS)
    mw = declare(c, MOD_W)
    aw = declare(c, ATT_W)
    ew = declare(c, MOE_W)
    xin = c.dram("xin", [TOWN, D], F32, kind="ExternalInput")
    kf_d = c.dram("kf", [16, 70, SEQ], BF16, kind="ExternalInput")
    v_d = c.dram("vh", [16, P, SEQ // P, 72], BF16, kind="ExternalInput")
    qf_d = c.dram("qf", [16, 6, TOWN], BF16, kind="ExternalInput")
    mask_d = c.dram("maskadd", [P, 16, 512], BF16, kind="ExternalInput")
    fg = c.dram("final_g", [1, D], F32, kind="ExternalInput") if final else None
    xout = c.dram("xout", [TOWN, D], F32, kind="ExternalOutput")
    xmid = c.dram("xmid", [TOWN, D], F32)
    scr = moe_scratch(c, TOWN)
    k = load_consts(c, cd)
    modsb = c.sbuf([P, 6 * D], F32)
    stage_mods(c, k, mw["c_r"], mw["mod_w"], mw["mod_b"], mw["norm_g"], modsb)
    stage_attn(c, k, modsb, xin, aw, kf_d, v_d, qf_d, mask_d, xmid)
    stage_moe(c, k, modsb, xmid, ew, scr, xout, final_g=fg)
    c.finish([xout])
    c.close()
    return nc


_PROGS = {}


def _prog(name, fn, *a):
    if name not in _PROGS:
        _PROGS[name] = fn(*a)
    return _PROGS[name]


def _run(nc, in_maps):
    res = run_bass_kernel_spmd(nc, in_maps, core_ids=list(range(NCORES)))
    return res.results


def kernel(**inp):
    inp = {k_: np.asarray(v_) for k_, v_ in inp.items()}
    hc = host_consts()
    x = inp["x"]
    xs = [x[b] for b in range(2)]
    for l in range(2):
        maps = []
        for core in range(NCORES):
            b, r = divmod(core, 4)
            t0 = r * TOWN
            xin = np.zeros((P + TOWN, D), np.float32)
            xin[P:] = xs[b][t0:t0 + TOWN]
            if r > 0:
                xin[:P] = xs[b][t0 - P:t0]
            m = dict(hc)
            m.update(mod_inputs(inp, l, b)); m.update(conv_inputs(inp, l)); m.update(moe_inputs(inp, l))
            m["xin"] = xin
            m["flag"] = np.full((P, 1), 1.0 if r > 0 else 0.0, np.float32)
            maps.append(m)
        res = _run(_prog("conv", build_conv_layer), maps)
        xs = [np.concatenate([res[b * 4 + r]["xout"] for r in range(4)], axis=0) for b in range(2)]
    maps = []
    for core in range(NCORES):
        b, r = divmod(core, 4)
        m = dict(hc)
        m.update(dict(c_r=pcol(inp["c"][b], 8), mod_w=inp["kv_mod_w"], mod_b=inp["kv_mod_b"][None, :],
                      norm_g=inp["kv_norm_g"][None, :], w_kvf=inp["w_kvf"], b_f=inp["b_f"][None, :], k_g=inp["k_norm_g"][None, :]))
        m["xin"] = np.ascontiguousarray(xs[b][r * TOWN:(r + 1) * TOWN])
        maps.append(m)
    res = _run(_prog("kv", build_kv), maps)
    kT = [np.concatenate([res[b * 4 + r]["kT"] for r in range(4)], axis=2) for b in range(2)]
    vv = [np.concatenate([res[b * 4 + r]["v"] for r in range(4)], axis=0) for b in range(2)]
    lf = [np.concatenate([res[b * 4 + r]["logf"] for r in range(4)], axis=0) for b in range(2)]
    maps = []
    for core in range(NCORES):
        m = dict(hc)
        m["logf"] = np.ascontiguousarray(lf[core % 2])
        maps.append(m)
    res = _run(_prog("fcum", build_fcum), maps)
    ft = [res[b]["ft"] for b in range(2)]
    ftn = [res[b]["ftn"] for b in range(2)]
    one = np.ones((), np.float32).astype(ml_dtypes.bfloat16)
    kf, vh = [], []
    for b in range(2):
        a = np.empty((16, 70, SEQ), ml_dtypes.bfloat16)
        a[:, 0:64] = kT[b]
        a[:, 64:67] = one
        a[:, 67:70] = ftn[b]
        kf.append(a)
        vh.append(np.ascontiguousarray(vv[b].reshape(SEQ // P, P, 16, 72).transpose(2, 1, 0, 3)))
    ii = np.arange(P)
    masks = []
    for r in range(4):
        mk = np.zeros((P, 16, 512), np.float32)
        for tz in range(16):
            spos = (tz // 4) * 512 + (tz % 4) * P + ii[:, None]
            tpos = r * 512 + np.arange(512)[None, :]
            mk[:, tz, :] = np.where(spos <= tpos, 0.0, -30000.0)
        masks.append(mk.astype(ml_dtypes.bfloat16))
    def own_rows(r):
        return np.concatenate([np.arange((4 * j + r) * 512, (4 * j + r + 1) * 512) for j in range(NQB)])
    for li in range(2):
        l = 2 + li
        final = (li == 1)
        maps = []
        for core in range(NCORES):
            b, r = divmod(core, 4)
            rows = own_rows(r)
            m = dict(hc)
            m.update(mod_inputs(inp, l, b)); m.update(moe_inputs(inp, l))
            m.update(dict(w_qg=inp["attn_w_qg"][li], q_g=inp["q_norm_g"][li][None, :], w_o=inp["attn_w_o"][li]))
            m["xin"] = np.ascontiguousarray(xs[b][rows])
            m["kf"] = kf[b]
            m["vh"] = vh[b]
            qf = np.empty((16, 6, TOWN), ml_dtypes.bfloat16)
            qf[:, 0:3] = ft[b][:, :, rows]
            qf[:, 3:6] = one
            m["qf"] = qf
            m["maskadd"] = masks[r]
            if final:
                m["final_g"] = inp["final_norm_g"][None, :]
            maps.append(m)
        res = _run(_prog("attn%d" % final, build_attn_layer, final), maps)
        nx = [np.empty((SEQ, D), np.float32) for _ in range(2)]
        for core in range(NCORES):
            b, r = divmod(core, 4)
            nx[b][own_rows(r)] = res[core]["xout"]
        xs = nx
    return np.stack(xs, axis=0).astype(np.float32)
```

```python
import os
import numpy as np
import ml_dtypes
import concourse.bass as bass
import concourse.mybir as mybir
from concourse.bass_utils import run_bass_kernel_spmd
from contextlib import ExitStack, contextmanager

F32 = mybir.dt.float32
BF16 = mybir.dt.bfloat16
I32 = mybir.dt.int32
ALU = mybir.AluOpType
AF = mybir.ActivationFunctionType
IOA = bass.IndirectOffsetOnAxis

P = 128
D = 1024
NE = 32
EPS = 1e-6
NCORES = 8
SEQ = 16384
TOWN = 4096
BLK = 512


class Buf:
    def __init__(self, t, name, dma_sem_key=None):
        self.t = t
        self.name = name
        self.last_w = None
        self.reads = []
        self.dma_key = dma_sem_key
        self.dma_cnt = 0

    def __getitem__(self, idx):
        return self.t[idx]

    def ap(self):
        return self.t.ap()


class Eng:
    def __init__(self, name, h):
        self.name = name
        self.h = h
        self.key = "e_" + name
        self.cnt = 0
        self.waited = {}


class Ctx:
    def __init__(self, nc):
        self.nc = nc
        self.root = ExitStack()
        self.stack = [self.root]
        self.sems = {}
        self.engs = {}
        self.free_dma_sems = []
        self.live_dma = {}
        for name, h in (("pe", nc.tensor), ("act", nc.scalar), ("dve", nc.vector),
                        ("pool", nc.gpsimd), ("sp", nc.sync)):
            e = Eng(name, h)
            self.sems[e.key] = self.root.enter_context(nc.semaphore(e.key))
            self.engs[name] = e
        self.nbuf = 0
        self.n_inst = 0
        self.sem_cnt = {}

    def _dma_key(self):
        if self.free_dma_sems:
            return self.free_dma_sems.pop()
        key = f"d{len(self.sems)}"
        self.sems[key] = self.root.enter_context(self.nc.semaphore(key))
        self.sem_cnt[key] = 0
        return key

    def sbuf(self, shape, dtype=F32, dma=False, name=None):
        self.nbuf += 1
        name = name or f"sb{self.nbuf}"
        t = self.stack[-1].enter_context(self.nc.sbuf_tensor(name, list(shape), dtype))
        b = Buf(t, name)
        if dma:
            b.dma_key = self._dma_key()
            b.dma_cnt = self.sem_cnt[b.dma_key]
            self.scope_keys[-1].append(b.dma_key) if self.scope_keys else None
        return b

    def psum(self, shape, dtype=F32, name=None):
        self.nbuf += 1
        name = name or f"ps{self.nbuf}"
        t = self.stack[-1].enter_context(self.nc.psum_tensor(name, list(shape), dtype))
        return Buf(t, name)

    def dram(self, name, shape, dtype, kind="Internal"):
        t = self.nc.dram_tensor(name, list(shape), dtype, kind=kind)
        return Buf(t, name)

    scope_keys = []

    @contextmanager
    def scope(self):
        es = ExitStack()
        self.stack.append(es)
        self.scope_keys.append([])
        try:
            yield
        finally:
            self.barrier()
            keys = self.scope_keys.pop()
            self.free_dma_sems.extend(keys)
            self.stack.pop()
            es.close()

    def barrier(self):
        for e in self.engs.values():
            for o in self.engs.values():
                if o is not e and o.cnt > 0:
                    self._wait(e, (o.key, o.cnt))
            for key, cnt in self.sem_cnt.items():
                if cnt > 0:
                    self._wait(e, (key, cnt))

    def _wait(self, eng, tok):
        if tok is None:
            return
        key, val = tok
        if key == eng.key and eng.name in ("pe", "sp"):
            return
        if eng.waited.get(key, 0) >= val:
            return
        eng.waited[key] = val
        eng.h.wait_ge(self.sems[key], val)

    def _deps(self, eng, reads, writes, nowaw=False):
        for r in reads:
            self._wait(eng, r.last_w)
        for w in writes:
            if not nowaw:
                self._wait(eng, w.last_w)
            for tok in w.reads:
                self._wait(eng, tok)

    def _commit(self, tok, reads, writes):
        for r in reads:
            r.reads.append(tok)
            if len(r.reads) > 48:
                best = {}
                for k, v in r.reads:
                    best[k] = max(best.get(k, 0), v)
                r.reads = list(best.items())
        for w in writes:
            w.last_w = tok
            w.reads = []

    def op(self, eng_name, fn, reads=(), writes=()):
        eng = self.engs[eng_name]
        self._deps(eng, reads, writes)
        inst = fn(eng.h)
        eng.cnt += 1
        inst.then_inc(self.sems[eng.key], 1)
        self._commit((eng.key, eng.cnt), reads, writes)
        self.n_inst += 1

    def dma(self, eng_name, fn, reads=(), writes=(), nowaw=False):
        eng = self.engs[eng_name]
        self._deps(eng, reads, writes, nowaw)
        sb = writes[0]
        if sb.dma_key is None:
            sb.dma_key = self._dma_key()
        inst = fn(eng.h)
        self.sem_cnt[sb.dma_key] += 16
        inst.then_inc(self.sems[sb.dma_key], 16)
        self._commit((sb.dma_key, self.sem_cnt[sb.dma_key]), reads, writes)
        self.n_inst += 1

    def finish(self, bufs):
        self.barrier()

    def close(self):
        self.root.close()


def rms_rstd(c, xt, junk, ss):
    c.op("act", lambda e: e.activation(out=junk[:, :], in_=xt[:, :], func=AF.Square, accum_out=ss[:, 0:1]),
         reads=[xt], writes=[junk, ss])
    c.op("dve", lambda e: e.tensor_scalar(out=ss[:, 1:2], in0=ss[:, 0:1], scalar1=1.0 / D, scalar2=EPS,
                                          op0=ALU.mult, op1=ALU.add), reads=[ss], writes=[ss])
    c.op("dve", lambda e: e.reciprocal(out=ss[:, 1:2], in_=ss[:, 1:2]), reads=[ss], writes=[ss])
    c.op("act", lambda e: e.activation(out=ss[:, 1:2], in_=ss[:, 1:2], func=AF.Sqrt), reads=[ss], writes=[ss])


def norm_mod(c, xt, h, ss, A, sh, modsb):
    rms_rstd(c, xt, h, ss)
    c.op("dve", lambda e: e.scalar_tensor_tensor(out=h[:, :], in0=xt[:, :], scalar=ss[:, 1:2],
                                                 in1=modsb[:, A:A + D], op0=ALU.mult, op1=ALU.mult),
         reads=[xt, ss, modsb], writes=[h])
    c.op("dve", lambda e: e.tensor_tensor(out=h[:, :], in0=h[:, :], in1=modsb[:, sh:sh + D], op=ALU.add),
         reads=[h, modsb], writes=[h])


class Consts:
    pass


def load_consts(c, d):
    k = Consts()
    k.identf = c.sbuf([P, P], F32, dma=True)
    k.identb = c.sbuf([P, P], BF16, dma=True)
    k.triu = c.sbuf([P, P], F32, dma=True)
    k.tril = c.sbuf([P, P], F32, dma=True)
    k.onesf = c.sbuf([P, P], F32)
    k.onesb = c.sbuf([P, P], BF16)
    c.dma("sp", lambda e: e.dma_start(out=k.identf[:, :], in_=d["identf"].ap()), writes=[k.identf])
    c.dma("sp", lambda e: e.dma_start(out=k.identb[:, :], in_=d["identb"].ap()), writes=[k.identb])
    c.dma("sp", lambda e: e.dma_start(out=k.triu[:, :], in_=d["triu"].ap()), writes=[k.triu])
    c.dma("sp", lambda e: e.dma_start(out=k.tril[:, :], in_=d["tril"].ap()), writes=[k.tril])
    c.op("dve", lambda e: e.memset(k.onesf[:, :], 1.0), writes=[k.onesf])
    c.op("dve", lambda e: e.memset(k.onesb[:, :], 1.0), writes=[k.onesb])
    return k


def host_consts():
    ii = np.arange(P)
    return dict(
        identf=np.eye(P, dtype=np.float32),
        identb=np.eye(P, dtype=np.float32).astype(ml_dtypes.bfloat16),
        triu=(ii[:, None] < ii[None, :]).astype(np.float32),
        tril=(ii[:, None] <= ii[None, :]).astype(np.float32),
    )


CONST_SPECS = dict(identf=([P, P], F32), identb=([P, P], BF16), triu=([P, P], F32), tril=([P, P], F32))


def stage_mods(c, k, cr_d, mw_d, mb_d, ng_d, modsb, ncols=6 * D, nnorm=2):
    with c.scope():
        cr = c.sbuf([P, 8], F32, dma=True)
        sg = c.sbuf([P, 8], F32)
        cb = c.sbuf([P, 8, P], F32)
        mbb = c.sbuf([P, ncols], F32, dma=True)
        nb = c.sbuf([P, nnorm, D], F32, dma=True)
        mwt = [c.sbuf([P, 8, 512], F32, dma=True) for _ in range(2)]
        ps = [c.psum([P, 512], F32) for _ in range(2)]
        c.dma("sp", lambda e: e.dma_start(out=cr[:, :], in_=cr_d.ap()), writes=[cr])
        c.dma("sp", lambda e: e.dma_start(out=mbb[:, :], in_=mb_d.ap().partition_broadcast(P)), writes=[mbb])
        for i in range(nnorm):
            c.dma("sp", lambda e: e.dma_start(out=nb[:, i, :], in_=ng_d.ap()[i:i + 1, :].partition_broadcast(P)),
                  writes=[nb])
        c.op("act", lambda e: e.activation(out=sg[:, :], in_=cr[:, :], func=AF.Sigmoid), reads=[cr], writes=[sg])
        c.op("dve", lambda e: e.tensor_tensor(out=sg[:, :], in0=sg[:, :], in1=cr[:, :], op=ALU.mult),
             reads=[sg, cr], writes=[sg])
        for kc in range(8):
            c.op("dve", lambda e: e.tensor_scalar(out=cb[:, kc, :], in0=k.onesf[:, :], scalar1=sg[:, kc:kc + 1],
                                                  scalar2=None, op0=ALU.mult), reads=[k.onesf, sg], writes=[cb])
        mwv = mw_d.ap().rearrange("(kc p) n -> p kc n", p=P)
        for j in range(ncols // 512):
            w = mwt[j % 2]
            pj = ps[j % 2]
            c.dma("sp", lambda e: e.dma_start(out=w[:, :, :], in_=mwv[:, :, j * 512:(j + 1) * 512]), writes=[w])
            for kc in range(8):
                c.op("pe", lambda e: e.matmul(pj[:, :], lhsT=cb[:, kc, :], rhs=w[:, kc, :], start=(kc == 0),
                                              stop=(kc == 7)), reads=[cb, w], writes=[pj])
            c.op("dve", lambda e: e.tensor_tensor(out=modsb[:, j * 512:(j + 1) * 512], in0=pj[:, :],
                                                  in1=mbb[:, j * 512:(j + 1) * 512], op=ALU.add),
                 reads=[pj, mbb], writes=[modsb])
        if nnorm == 2:
            slots = [(1, 0), (4, 1)]
        else:
            slots = [(1, 0)]
        for s, i in slots:
            c.op("dve", lambda e: e.scalar_tensor_tensor(out=modsb[:, s * D:(s + 1) * D], in0=modsb[:, s * D:(s + 1) * D],
                                                         scalar=1.0, in1=nb[:, i, :], op0=ALU.add, op1=ALU.mult),
                 reads=[modsb, nb], writes=[modsb])


def stage_conv(c, k, modsb, xin_d, flag_d, w, xmid_d, town=TOWN):
    CW = 31
    with c.scope():
        w1b = c.sbuf([P, 8, 2 * D], BF16, dma=True)
        w2b = c.sbuf([P, 8, D], BF16, dma=True)
        b1 = c.sbuf([P, 16], F32, dma=True)
        wdw = c.sbuf([P, 8, CW], F32, dma=True)
        bdw = c.sbuf([P, 8], F32, dma=True)
        lng = c.sbuf([P, 8], F32, dma=True)
        lnb = c.sbuf([P, 8], F32, dma=True)
        b2b = c.sbuf([1, D], BF16, dma=True)
        flag = c.sbuf([P, 1], F32, dma=True)
        w1v = w["pw1"].ap().rearrange("(kc p) n -> p kc n", p=P)
        w2v = w["pw2"].ap().rearrange("(kc p) n -> p kc n", p=P)
        for kc in range(8):
            c.dma("pool", lambda e: e.dma_start(out=w1b[:, kc, :], in_=w1v[:, kc, :]), writes=[w1b], nowaw=True)
            c.dma("pool", lambda e: e.dma_start(out=w2b[:, kc, :], in_=w2v[:, kc, :]), writes=[w2b], nowaw=True)
        c.dma("pool", lambda e: e.dma_start(out=b2b[:, :], in_=w["b_pw2"].ap()), writes=[b2b])
        for t, src in ((b1, "b_pw1"), (wdw, "w_dw"), (bdw, "b_dw"), (lng, "ln_g"), (lnb, "ln_b")):
            if t is wdw:
                c.dma("sp", lambda e: e.dma_start(out=t[:, :, :], in_=w[src].ap()), writes=[t])
            else:
                c.dma("sp", lambda e: e.dma_start(out=t[:, :], in_=w[src].ap()), writes=[t])
        c.dma("sp", lambda e: e.dma_start(out=flag[:, :], in_=flag_d.ap()), writes=[flag])

        xts = [c.sbuf([P, D], F32, dma=True) for _ in range(2)]
        xrs = [c.sbuf([P, D], F32, dma=True) for _ in range(2)]
        xos = [c.sbuf([P, D], F32) for _ in range(2)]
        h = c.sbuf([P, D], F32)
        ss = c.sbuf([P, 2], F32)
        hT = c.sbuf([P, 8, BLK], BF16)
        uT = c.sbuf([P, 8, 30 + BLK], BF16)
        acc = [c.sbuf([P, BLK], F32) for _ in range(8)]
        vb = c.sbuf([P, 8, BLK], BF16)
        v2 = c.sbuf([P, 8, BLK], BF16)
        sT = c.sbuf([P, 8, BLK], BF16)
        sgt = [c.sbuf([P, BLK], F32) for _ in range(2)]
        mean = c.sbuf([P, BLK], F32)
        msq = c.sbuf([P, BLK], F32)
        rstd = c.sbuf([P, BLK], F32)
        tmp = c.sbuf([P, 512], F32)
        pT = c.psum([P, D], F32)
        psA = [c.psum([P, BLK], F32) for _ in range(2)]
        psG = [c.psum([P, BLK], F32) for _ in range(2)]
        psO = [c.psum([P, 512], F32) for _ in range(2)]
        c.op("dve", lambda e: e.memset(uT[:, :, 0:30], 0.0), writes=[uT])

        blocks = [(0, P, True)] + [(P + i * BLK, BLK, False) for i in range(town // BLK)]
        nx = 0
        for (t0, n, is_halo) in blocks:
            nt = n // P
            for i in range(nt):
                xt = xts[nx % 2]
                nx += 1
                r0 = t0 + i * P
                c.dma("sp", lambda e: e.dma_start(out=xt[:, :], in_=xin_d.ap()[r0:r0 + P, :]), writes=[xt])
                norm_mod(c, xt, h, ss, 1 * D, 0 * D, modsb)
                for kc in range(8):
                    c.op("pe", lambda e: e.transpose(out=pT[:, kc * P:(kc + 1) * P], in_=h[:, kc * P:(kc + 1) * P],
                                                     identity=k.identf[:, :]), reads=[h, k.identf], writes=[pT])
                c.op("act", lambda e: e.activation(out=hT[:, :, i * P:(i + 1) * P],
                                                   in_=pT[:, :].rearrange("p (k t) -> p k t", k=8), func=AF.Copy),
                     reads=[pT], writes=[hT])
            for fc in range(8):
                pa, pg, sg = psA[fc % 2], psG[fc % 2], sgt[fc % 2]
                for kc in range(8):
                    c.op("pe", lambda e: e.matmul(pa[:, 0:n], lhsT=w1b[:, kc, fc * P:(fc + 1) * P], rhs=hT[:, kc, 0:n],
                                                  start=(kc == 0), stop=(kc == 7)), reads=[w1b, hT], writes=[pa])
                for kc in range(8):
                    c.op("pe", lambda e: e.matmul(pg[:, 0:n], lhsT=w1b[:, kc, D + fc * P:D + (fc + 1) * P],
                                                  rhs=hT[:, kc, 0:n], start=(kc == 0), stop=(kc == 7)),
                         reads=[w1b, hT], writes=[pg])
                c.op("act", lambda e: e.activation(out=sg[:, 0:n], in_=pg[:, 0:n], func=AF.Sigmoid,
                                                   bias=b1[:, 8 + fc:9 + fc]), reads=[pg, b1], writes=[sg])
                c.op("dve", lambda e: e.scalar_tensor_tensor(out=uT[:, fc, 30:30 + n], in0=pa[:, 0:n],
                                                             scalar=b1[:, fc:fc + 1], in1=sg[:, 0:n],
                                                             op0=ALU.add, op1=ALU.mult),
                     reads=[pa, b1, sg], writes=[uT])
            if is_halo:
                c.op("dve", lambda e: e.tensor_scalar(out=uT[:, :, 30:30 + n], in0=uT[:, :, 30:30 + n],
                                                      scalar1=flag[:, 0:1], scalar2=None, op0=ALU.mult),
                     reads=[uT, flag], writes=[uT])
            else:
                for cc in range(8):
                    en = "dve"
                    a = acc[cc]
                    c.op(en, lambda e: e.tensor_scalar(out=a[:, 0:n], in0=uT[:, cc, 0:n], scalar1=wdw[:, cc, 0:1],
                                                       scalar2=bdw[:, cc:cc + 1], op0=ALU.mult, op1=ALU.add),
                         reads=[uT, wdw, bdw], writes=[a])
                    for j in range(1, CW):
                        c.op(en, lambda e: e.scalar_tensor_tensor(out=a[:, 0:n], in0=uT[:, cc, j:j + n],
                                                                  scalar=wdw[:, cc, j:j + 1], in1=a[:, 0:n],
                                                                  op0=ALU.mult, op1=ALU.add),
                             reads=[uT, wdw, a], writes=[a])
                for cc in range(8):
                    a = acc[cc]
                    c.op("act", lambda e: e.activation(out=vb[:, cc, 0:n], in_=a[:, 0:n], func=AF.Copy),
                         reads=[a], writes=[vb])
                    c.op("act", lambda e: e.activation(out=v2[:, cc, 0:n], in_=a[:, 0:n], func=AF.Square),
                         reads=[a], writes=[v2])
                s1, s2 = psA[0], psG[0]
                for cc in range(8):
                    c.op("pe", lambda e: e.matmul(s1[:, 0:n], lhsT=k.onesb[:, :], rhs=vb[:, cc, 0:n], start=(cc == 0),
                                                  stop=(cc == 7)), reads=[k.onesb, vb], writes=[s1])
                for cc in range(8):
                    c.op("pe", lambda e: e.matmul(s2[:, 0:n], lhsT=k.onesb[:, :], rhs=v2[:, cc, 0:n], start=(cc == 0),
                                                  stop=(cc == 7)), reads=[k.onesb, v2], writes=[s2])
                c.op("dve", lambda e: e.tensor_scalar(out=mean[:, 0:n], in0=s1[:, 0:n], scalar1=1.0 / D, scalar2=None,
                                                      op0=ALU.mult), reads=[s1], writes=[mean])
                c.op("dve", lambda e: e.tensor_tensor(out=msq[:, 0:n], in0=mean[:, 0:n], in1=mean[:, 0:n], op=ALU.mult),
                     reads=[mean], writes=[msq])
                c.op("dve", lambda e: e.scalar_tensor_tensor(out=rstd[:, 0:n], in0=s2[:, 0:n], scalar=1.0 / D,
                                                             in1=msq[:, 0:n], op0=ALU.mult, op1=ALU.subtract),
                     reads=[s2, msq], writes=[rstd])
                c.op("dve", lambda e: e.tensor_scalar(out=rstd[:, 0:n], in0=rstd[:, 0:n], scalar1=EPS, scalar2=None,
                                                      op0=ALU.add), reads=[rstd], writes=[rstd])
                c.op("dve", lambda e: e.reciprocal(out=rstd[:, 0:n], in_=rstd[:, 0:n]), reads=[rstd], writes=[rstd])
                c.op("act", lambda e: e.activation(out=rstd[:, 0:n], in_=rstd[:, 0:n], func=AF.Sqrt),
                     reads=[rstd], writes=[rstd])
                for cc in range(8):
                    a = acc[cc]
                    en = "dve" if cc < 5 else "pool"
                    c.op(en, lambda e: e.tensor_tensor(out=a[:, 0:n], in0=a[:, 0:n], in1=mean[:, 0:n], op=ALU.subtract),
                         reads=[a, mean], writes=[a])
                    c.op(en, lambda e: e.tensor_tensor(out=a[:, 0:n], in0=a[:, 0:n], in1=rstd[:, 0:n], op=ALU.mult),
                         reads=[a, rstd], writes=[a])
                    c.op("act", lambda e: e.activation(out=sT[:, cc, 0:n], in_=a[:, 0:n], func=AF.Silu,
                                                       scale=lng[:, cc:cc + 1], bias=lnb[:, cc:cc + 1]),
                         reads=[a, lng, lnb], writes=[sT])
                for i in range(nt):
                    r0 = t0 + i * P
                    xr = xrs[i % 2]
                    xo = xos[i % 2]
                    c.dma("sp", lambda e: e.dma_start(out=xr[:, :], in_=xin_d.ap()[r0:r0 + P, :]), writes=[xr])
                    for hf in range(2):
                        po = psO[hf]
                        for cc in range(8):
                            c.op("pe", lambda e: e.matmul(po[:, :], lhsT=sT[:, cc, i * P:(i + 1) * P],
                                                          rhs=w2b[:, cc, hf * 512:(hf + 1) * 512], start=(cc == 0),
                                                          stop=False), reads=[sT, w2b], writes=[po])
                        c.op("pe", lambda e: e.matmul(po[:, :], lhsT=k.onesb[0:1, :], rhs=b2b[0:1, hf * 512:(hf + 1) * 512],
                                                      start=False, stop=True), reads=[k.onesb, b2b], writes=[po])
                        c.op("dve", lambda e: e.tensor_tensor(out=tmp[:, :], in0=po[:, :],
                                                              in1=modsb[:, 2 * D + hf * 512:2 * D + (hf + 1) * 512],
                                                              op=ALU.mult), reads=[po, modsb], writes=[tmp])
                        c.op("dve", lambda e: e.tensor_tensor(out=xo[:, hf * 512:(hf + 1) * 512], in0=tmp[:, :],
                                                              in1=xr[:, hf * 512:(hf + 1) * 512], op=ALU.add),
                             reads=[tmp, xr], writes=[xo])
                    c.dma("sp", lambda e: e.dma_start(out=xmid_d.ap()[r0 - P:r0, :], in_=xo[:, :]),
                          reads=[xo], writes=[xmid_d], nowaw=True)
            c.op("dve", lambda e: e.tensor_copy(out=uT[:, :, 0:30], in_=uT[:, :, n:n + 30]), reads=[uT], writes=[uT])


def stage_moe(c, k, modsb, xmid_d, w, scr, xout_d, T=TOWN, final_g=None):
    NT = T // P
    NB = (T * 4) // BLK + NE
    KMAX = T // BLK
    hbuf_d, table_d, ysl_d = scr["hbuf"], scr["table"], scr["ysl"]
    with c.scope():
        maskall = c.sbuf([P, NT, NE], F32)
        gall = c.sbuf([P, NT, NE], F32)
        s4i = c.sbuf([P, NT, 4], I32)
        ebi = c.sbuf([P, NB], I32)
        widx = c.sbuf([P, NB, 8], I32)
        bidx = c.sbuf([P, NB], I32)
        with c.scope():
            rw = c.sbuf([P, 8, NE], F32, dma=True)
            rbb = c.sbuf([P, NE], F32, dma=True)
            c.dma("sp", lambda e: e.dma_start(out=rw[:, :, :], in_=w["router_w"].ap().rearrange("(kc p) n -> p kc n", p=P)),
                  writes=[rw])
            c.dma("sp", lambda e: e.dma_start(out=rbb[:, :], in_=w["router_b"].ap().partition_broadcast(P)), writes=[rbb])
            xts = [c.sbuf([P, D], F32, dma=True) for _ in range(2)]
            hq = c.sbuf([P, D], F32)
            hbs = [c.sbuf([P, D], BF16) for _ in range(2)]
            hT32 = c.sbuf([P, 8, P], F32)
            ss = c.sbuf([P, 2], F32)
            lg = c.sbuf([P, NE], F32)
            ex = c.sbuf([P, NE], F32)
            m8 = c.sbuf([P, 8], F32)
            sm = c.sbuf([P, 4], F32)
            pT = c.psum([P, D], F32)
            pl = c.psum([P, NE], F32)
            for i in range(NT):
                xt = xts[i % 2]
                hb = hbs[i % 2]
                c.dma("sp", lambda e: e.dma_start(out=xt[:, :], in_=xmid_d.ap()[i * P:(i + 1) * P, :]),
                      reads=[xmid_d], writes=[xt])
                norm_mod(c, xt, hq, ss, 4 * D, 3 * D, modsb)
                c.op("pool", lambda e: e.tensor_copy(out=hb[:, :], in_=hq[:, :]), reads=[hq], writes=[hb])
                c.dma("sp", lambda e: e.dma_start(out=hbuf_d.ap()[i * P:(i + 1) * P, :], in_=hb[:, :]),
                      reads=[hb], writes=[hbuf_d], nowaw=True)
                for kc in range(8):
                    c.op("pe", lambda e: e.transpose(out=pT[:, kc * P:(kc + 1) * P], in_=hq[:, kc * P:(kc + 1) * P],
                                                     identity=k.identf[:, :]), reads=[hq, k.identf], writes=[pT])
                c.op("act", lambda e: e.activation(out=hT32[:, :, :], in_=pT[:, :].rearrange("p (k t) -> p k t", k=8),
                                                   func=AF.Copy), reads=[pT], writes=[hT32])
                for kc in range(8):
                    c.op("pe", lambda e: e.matmul(pl[:, :], lhsT=hT32[:, kc, :], rhs=rw[:, kc, :], start=(kc == 0),
                                                  stop=(kc == 7)), reads=[hT32, rw], writes=[pl])
                c.op("dve", lambda e: e.tensor_tensor(out=lg[:, :], in0=pl[:, :], in1=rbb[:, :], op=ALU.add),
                     reads=[pl, rbb], writes=[lg])
                c.op("dve", lambda e: e.max(out=m8[:, :], in_=lg[:, :]), reads=[lg], writes=[m8])
                c.op("dve", lambda e: e.tensor_scalar(out=maskall[:, i, :], in0=lg[:, :], scalar1=m8[:, 3:4], scalar2=None,
                                                      op0=ALU.is_ge), reads=[lg, m8], writes=[maskall])
                c.op("dve", lambda e: e.tensor_scalar(out=sm[:, 0:1], in0=m8[:, 0:1], scalar1=-1.0, scalar2=None,
                                                      op0=ALU.mult), reads=[m8], writes=[sm])
                c.op("act", lambda e: e.activation(out=ex[:, :], in_=lg[:, :], func=AF.Exp, bias=sm[:, 0:1]),
                     reads=[lg, sm], writes=[ex])
                c.op("dve", lambda e: e.scalar_tensor_tensor(out=ex[:, :], in0=ex[:, :], scalar=1.0, in1=maskall[:, i, :],
                                                             op0=ALU.mult, op1=ALU.mult, accum_out=sm[:, 1:2]),
                     reads=[ex, maskall], writes=[ex, sm])
                c.op("dve", lambda e: e.reciprocal(out=sm[:, 2:3], in_=sm[:, 1:2]), reads=[sm], writes=[sm])
                c.op("dve", lambda e: e.tensor_scalar(out=gall[:, i, :], in0=ex[:, :], scalar1=sm[:, 2:3], scalar2=None,
                                                      op0=ALU.mult), reads=[ex, sm], writes=[gall])
        with c.scope():
            W = NT * NE
            pos = c.sbuf([P, NT, NE], F32)
            cs = c.sbuf([P, NT, NE], F32)
            base = c.sbuf([P, NT + 1, NE], F32)
            key = c.sbuf([P, NT, NE], F32)
            top = c.sbuf([P, NT, 8], F32)
            g4 = c.sbuf([P, NT, 4], F32)
            src = c.sbuf([P, NT, 4, 2], I32)
            tok = c.sbuf([P, NT], I32)
            cnt = c.sbuf([P, NE], F32)
            nbk = c.sbuf([P, NE], F32)
            tmpe = c.sbuf([P, NE], F32)
            pend = c.sbuf([P, NE], F32)
            pstart = c.sbuf([P, NE], F32)
            thr_i = c.sbuf([P, NB], I32)
            thr = c.sbuf([P, NB], F32)
            eb = c.sbuf([P, NB], F32)
            same = c.sbuf([P, NB], F32)
            ebs = c.sbuf([P, NB], F32)
            pk_i = c.sbuf([P, 8], I32)
            pk = c.sbuf([P, 8], F32)
            wf = c.sbuf([P, NB, 8], F32)
            s4f = c.sbuf([P, NT, 4], F32)
            zt = c.sbuf([P, NB * BLK * 2 // P], I32)
            pp = [c.psum([P, 512], F32) for _ in range(2)]
            mflat = maskall[:, :, :].rearrange("p t e -> p (t e)")
            posf = pos[:, :, :].rearrange("p t e -> p (t e)")
            csf = cs[:, :, :].rearrange("p t e -> p (t e)")
            c.op("pool", lambda e: e.memset(zt[:, :], 0), writes=[zt])
            c.dma("sp", lambda e: e.dma_start(out=table_d.ap().rearrange("(p r) w -> p (r w)", p=P), in_=zt[:, :]),
                  reads=[zt], writes=[table_d])
            for j0 in range(0, W, 512):
                n = min(512, W - j0)
                c.op("pe", lambda e: e.matmul(pp[0][:, 0:n], lhsT=k.triu[:, :], rhs=mflat[:, j0:j0 + n], start=True, stop=True),
                     reads=[k.triu, maskall], writes=[pp[0]])
                c.op("pe", lambda e: e.matmul(pp[1][:, 0:n], lhsT=k.onesf[:, :], rhs=mflat[:, j0:j0 + n], start=True, stop=True),
                     reads=[k.onesf, maskall], writes=[pp[1]])
                c.op("dve", lambda e: e.tensor_copy(out=posf[:, j0:j0 + n], in_=pp[0][:, 0:n]), reads=[pp[0]], writes=[pos])
                c.op("act", lambda e: e.activation(out=csf[:, j0:j0 + n], in_=pp[1][:, 0:n], func=AF.Copy),
                     reads=[pp[1]], writes=[cs])
            c.op("dve", lambda e: e.memset(base[:, 0, :], 0.0), writes=[base])
            for i in range(NT):
                c.op("dve", lambda e: e.tensor_tensor(out=base[:, i + 1, :], in0=base[:, i, :], in1=cs[:, i, :], op=ALU.add),
                     reads=[base, cs], writes=[base])
            c.op("dve", lambda e: e.tensor_copy(out=cnt[:, :], in_=base[:, NT, :]), reads=[base], writes=[cnt])
            c.op("dve", lambda e: e.tensor_scalar(out=nbk[:, :], in0=cnt[:, :], scalar1=0.0, scalar2=None, op0=ALU.is_gt),
                 reads=[cnt], writes=[nbk])
            for kk in range(1, KMAX + 1):
                c.op("dve", lambda e: e.tensor_scalar(out=tmpe[:, :], in0=cnt[:, :], scalar1=float(BLK * kk), scalar2=None,
                                                      op0=ALU.is_gt), reads=[cnt], writes=[tmpe])
                c.op("dve", lambda e: e.tensor_tensor(out=nbk[:, :], in0=nbk[:, :], in1=tmpe[:, :], op=ALU.add),
                     reads=[nbk, tmpe], writes=[nbk])
            c.op("dve", lambda e: e.tensor_scalar(out=nbk[:, :], in0=nbk[:, :], scalar1=float(BLK), scalar2=None, op0=ALU.mult),
                 reads=[nbk], writes=[nbk])
            c.op("dve", lambda e: e.tensor_copy(out=pend[:, 0:1], in_=nbk[:, 0:1]), reads=[nbk], writes=[pend])
            for e_ in range(1, NE):
                c.op("dve", lambda e: e.tensor_tensor(out=pend[:, e_:e_ + 1], in0=pend[:, e_ - 1:e_], in1=nbk[:, e_:e_ + 1],
                                                      op=ALU.add), reads=[pend, nbk], writes=[pend])
            c.op("dve", lambda e: e.tensor_tensor(out=pstart[:, :], in0=pend[:, :], in1=nbk[:, :], op=ALU.subtract),
                 reads=[pend, nbk], writes=[pstart])
            c.op("dve", lambda e: e.tensor_tensor(out=pos[:, :, :], in0=pos[:, :, :], in1=base[:, 0:NT, :], op=ALU.add),
                 reads=[pos, base], writes=[pos])
            for i in range(NT):
                c.op("dve", lambda e: e.tensor_tensor(out=pos[:, i, :], in0=pos[:, i, :], in1=pstart[:, :], op=ALU.add),
                     reads=[pos, pstart], writes=[pos])
            c.op("dve", lambda e: e.scalar_tensor_tensor(out=key[:, :, :], in0=pos[:, :, :], scalar=1.0, in1=maskall[:, :, :],
                                                         op0=ALU.add, op1=ALU.mult), reads=[pos, maskall], writes=[key])
            for i in range(NT):
                c.op("dve", lambda e: e.max(out=top[:, i, :], in_=key[:, i, :]), reads=[key], writes=[top])
            c.op("dve", lambda e: e.tensor_scalar(out=s4f[:, :, :], in0=top[:, :, 0:4], scalar1=-1.0, scalar2=None, op0=ALU.add),
                 reads=[top], writes=[s4f])
            c.op("dve", lambda e: e.tensor_copy(out=s4i[:, :, :], in_=s4f[:, :, :]), reads=[s4f], writes=[s4i])
            for i in range(NT):
                for kk in range(4):
                    c.op("dve", lambda e: e.scalar_tensor_tensor(out=tmpe[:, :], in0=key[:, i, :], scalar=top[:, i, kk:kk + 1],
                                                                 in1=gall[:, i, :], op0=ALU.is_equal, op1=ALU.mult,
                                                                 accum_out=g4[:, i, kk:kk + 1]),
                         reads=[key, top, gall], writes=[tmpe, g4])
            c.op("pool", lambda e: e.iota(tok[:, :], pattern=[[P, NT]], base=0, channel_multiplier=1), writes=[tok])
            for kk in range(4):
                c.op("dve", lambda e: e.tensor_copy(out=src[:, :, kk, 0], in_=tok[:, :]), reads=[tok], writes=[src])
            c.op("dve", lambda e: e.tensor_copy(out=src[:, :, :, 1].bitcast(F32), in_=g4[:, :, :]), reads=[g4], writes=[src])
            for i in range(NT):
                for kk in range(4):
                    c.dma("pool", lambda e: e.indirect_dma_start(out=table_d.ap(), out_offset=IOA(ap=s4i[:, i, kk:kk + 1], axis=0),
                                                                 in_=src[:, i, kk, :], in_offset=None),
                          reads=[s4i, src], writes=[table_d], nowaw=(i + kk > 0))
            c.op("pool", lambda e: e.iota(thr_i[:, :], pattern=[[BLK, NB]], base=0, channel_multiplier=0), writes=[thr_i])
            c.op("dve", lambda e: e.tensor_copy(out=thr[:, :], in_=thr_i[:, :]), reads=[thr_i], writes=[thr])
            for b in range(NB):
                c.op("dve", lambda e: e.tensor_scalar(out=tmpe[:, :], in0=pend[:, :], scalar1=thr[:, b:b + 1], scalar2=0.0,
                                                      op0=ALU.is_le, op1=ALU.add, accum_out=eb[:, b:b + 1]),
                     reads=[pend, thr], writes=[tmpe, eb])
            c.op("dve", lambda e: e.tensor_scalar(out=eb[:, :], in0=eb[:, :], scalar1=float(NE - 1), scalar2=None, op0=ALU.min),
                 reads=[eb], writes=[eb])
            c.op("dve", lambda e: e.memset(same[:, :], 0.0), writes=[same])
            c.op("dve", lambda e: e.tensor_tensor(out=same[:, 1:NB], in0=eb[:, 1:NB], in1=eb[:, 0:NB - 1], op=ALU.is_equal),
                 reads=[eb], writes=[same])
            c.op("dve", lambda e: e.memset(same[:, NB // 2:NB // 2 + 1], 0.0), writes=[same])
            c.op("dve", lambda e: e.scalar_tensor_tensor(out=ebs[:, :], in0=same[:, :], scalar=1.0e9, in1=eb[:, :], op0=ALU.mult, op1=ALU.add),
                 reads=[same, eb], writes=[ebs])
            c.op("dve", lambda e: e.tensor_copy(out=ebi[:, :], in_=ebs[:, :]), reads=[ebs], writes=[ebi])
            c.op("pool", lambda e: e.iota(pk_i[:, :], pattern=[[P, 8]], base=0, channel_multiplier=1), writes=[pk_i])
            c.op("dve", lambda e: e.tensor_copy(out=pk[:, :], in_=pk_i[:, :]), reads=[pk_i], writes=[pk])
            for kc in range(8):
                c.op("dve", lambda e: e.tensor_scalar(out=wf[:, :, kc], in0=eb[:, :], scalar1=float(D), scalar2=pk[:, kc:kc + 1],
                                                      op0=ALU.mult, op1=ALU.add), reads=[eb, pk], writes=[wf])
            for kc in range(8):
                c.op("dve", lambda e: e.scalar_tensor_tensor(out=wf[:, :, kc], in0=same[:, :], scalar=1.0e9, in1=wf[:, :, kc],
                                                             op0=ALU.mult, op1=ALU.add), reads=[same, wf], writes=[wf])
            c.op("dve", lambda e: e.tensor_copy(out=widx[:, :, :], in_=wf[:, :, :]), reads=[wf], writes=[widx])
            c.op("dve", lambda e: e.tensor_scalar(out=eb[:, :], in0=eb[:, :], scalar1=float(P), scalar2=pk[:, 0:1],
                                                  op0=ALU.mult, op1=ALU.add), reads=[eb, pk], writes=[eb])
            c.op("dve", lambda e: e.scalar_tensor_tensor(out=eb[:, :], in0=same[:, :], scalar=1.0e9, in1=eb[:, :], op0=ALU.mult, op1=ALU.add),
                 reads=[same, eb], writes=[eb])
            c.op("dve", lambda e: e.tensor_copy(out=bidx[:, :], in_=eb[:, :]), reads=[eb], writes=[bidx])
        with c.scope():
            wg = [c.sbuf([P, 8, 2 * D], BF16, dma=True) for _ in range(2)]
            wd = [c.sbuf([P, 8, D], BF16, dma=True) for _ in range(2)]
            bg = [c.sbuf([P, 16], F32, dma=True) for _ in range(2)]
            bd = [c.sbuf([2, D], BF16, dma=True) for _ in range(2)]
            tk = [c.sbuf([P, 8], I32, dma=True) for _ in range(2)]
            xg = [c.sbuf([P, 4, D], BF16, dma=True) for _ in range(2)]
            xgT = c.sbuf([P, 8, BLK], BF16)
            actT = c.sbuf([P, 8, BLK], BF16)
            yo = [c.sbuf([P, 4, D], BF16) for _ in range(2)]
            t1 = [c.sbuf([P, BLK], F32) for _ in range(2)]
            sg = [c.sbuf([P, BLK], F32) for _ in range(2)]
            xl = [c.sbuf([P, BLK], F32) for _ in range(2)]
            bgp = [c.sbuf([P, 8], F32) for _ in range(2)]
            ptr = [c.psum([P, 2 * BLK], BF16) for _ in range(2)]
            psg = [c.psum([P, BLK], F32) for _ in range(2)]
            psl = [c.psum([P, BLK], F32) for _ in range(2)]
            psd = [c.psum([P, 512], F32) for _ in range(2)]
            wguv = w["w_gu"].ap().rearrange("e k n -> (e k) n")
            wdnv = w["w_down"].ap().rearrange("e k n -> (e k) n")
            bguv = w["b_gu_r"].ap().rearrange("e p n -> (e p) n")

            rg_w = c.nc.gpsimd.to_reg(NE * D - 1)
            rg_b = c.nc.gpsimd.to_reg(NE * P - 1)
            rg_e = c.nc.gpsimd.to_reg(NE - 1)

            def blk(kpos):
                return (kpos // 2) + (NB // 2) * (kpos % 2)

            def prefetch(kpos):
                s = kpos % 2
                b = blk(kpos)
                c.dma("sp", lambda e: e.dma_start(out=tk[s][:, :],
                                                  in_=table_d.ap()[b * BLK:(b + 1) * BLK, :].rearrange("(p j) w -> p (j w)", j=4)),
                      reads=[table_d], writes=[tk[s]])
                for j in range(4):
                    c.dma("pool", lambda e: e.indirect_dma_start(out=xg[s][:, j, :], out_offset=None, in_=hbuf_d.ap(),
                                                                 in_offset=IOA(ap=tk[s][:, 2 * j:2 * j + 1], axis=0)),
                          reads=[tk[s], hbuf_d], writes=[xg[s]], nowaw=(j > 0))
                for kc in range(8):
                    c.dma("pool", lambda e: e.indirect_dma_start(out=wg[s][:, kc, :], out_offset=None, in_=wguv,
                                                                 in_offset=IOA(ap=widx[:, b, kc:kc + 1], axis=0),
                                                                 bounds_check=rg_w, oob_is_err=False),
                          reads=[widx], writes=[wg[s]], nowaw=(kc > 0))
                for kc in range(8):
                    c.dma("pool", lambda e: e.indirect_dma_start(out=wd[s][:, kc, :], out_offset=None, in_=wdnv,
                                                                 in_offset=IOA(ap=widx[:, b, kc:kc + 1], axis=0),
                                                                 bounds_check=rg_w, oob_is_err=False),
                          reads=[widx], writes=[wd[s]], nowaw=(kc > 0))
                c.dma("pool", lambda e: e.indirect_dma_start(out=bg[s][:, :], out_offset=None, in_=bguv,
                                                             in_offset=IOA(ap=bidx[:, b:b + 1], axis=0),
                                                             bounds_check=rg_b, oob_is_err=False),
                      reads=[bidx], writes=[bg[s]])
                c.dma("pool", lambda e: e.indirect_dma_start(out=bd[s][0:2, :], out_offset=None, in_=w["b_down"].ap(),
                                                             in_offset=IOA(ap=ebi[0:2, b:b + 1], axis=0),
                                                             bounds_check=rg_e, oob_is_err=False),
                      reads=[ebi], writes=[bd[s]])

            prefetch(0)
            for kpos in range(NB):
                s = kpos % 2
                b = blk(kpos)
                if kpos + 1 < NB:
                    prefetch(kpos + 1)
                for kc in range(8):
                    pt = ptr[(kc // 2) % 2]
                    o = (kc % 2) * BLK
                    for j in range(4):
                        c.op("pe", lambda e: e.transpose(out=pt[:, o + j * P:o + (j + 1) * P], in_=xg[s][:, j, kc * P:(kc + 1) * P],
                                                         identity=k.identb[:, :]), reads=[xg[s], k.identb], writes=[pt])
                    if kc % 2 == 1:
                        c.op("act", lambda e: e.activation(out=xgT[:, kc - 1:kc + 1, :],
                                                           in_=pt[:, :].rearrange("p (k t) -> p k t", k=2), func=AF.Copy),
                             reads=[pt], writes=[xgT])
                c.op("dve", lambda e: e.tensor_scalar(out=bgp[s][:, :], in0=bg[s][:, 8:16], scalar1=1.0, scalar2=None, op0=ALU.add),
                     reads=[bg[s]], writes=[bgp[s]])
                for fc in range(8):
                    q = fc % 2
                    for kc in range(8):
                        c.op("pe", lambda e: e.matmul(psg[q][:, :], lhsT=wg[s][:, kc, fc * P:(fc + 1) * P], rhs=xgT[:, kc, :],
                                                      start=(kc == 0), stop=(kc == 7)), reads=[wg[s], xgT], writes=[psg[q]])
                    for kc in range(8):
                        c.op("pe", lambda e: e.matmul(psl[q][:, :], lhsT=wg[s][:, kc, D + fc * P:D + (fc + 1) * P], rhs=xgT[:, kc, :],
                                                      start=(kc == 0), stop=(kc == 7)), reads=[wg[s], xgT], writes=[psl[q]])
                    c.op("dve", lambda e: e.tensor_scalar(out=t1[q][:, :], in0=psg[q][:, :], scalar1=bg[s][:, fc:fc + 1], scalar2=7.0,
                                                          op0=ALU.add, op1=ALU.min), reads=[psg[q], bg[s]], writes=[t1[q]])
                    c.op("act", lambda e: e.activation(out=sg[q][:, :], in_=t1[q][:, :], func=AF.Sigmoid, scale=1.702),
                         reads=[t1[q]], writes=[sg[q]])
                    c.op("dve", lambda e: e.tensor_scalar(out=xl[q][:, :], in0=psl[q][:, :], scalar1=bgp[s][:, fc:fc + 1], scalar2=-6.0,
                                                          op0=ALU.add, op1=ALU.max), reads=[psl[q], bgp[s]], writes=[xl[q]])
                    c.op("pool", lambda e: e.tensor_tensor(out=t1[q][:, :], in0=t1[q][:, :], in1=sg[q][:, :], op=ALU.mult),
                         reads=[t1[q], sg[q]], writes=[t1[q]])
                    c.op("dve", lambda e: e.scalar_tensor_tensor(out=actT[:, fc, :], in0=xl[q][:, :], scalar=8.0, in1=t1[q][:, :],
                                                                  op0=ALU.min, op1=ALU.mult), reads=[xl[q], t1[q]], writes=[actT])
                for j in range(4):
                    for hf in range(2):
                        pd = psd[hf]
                        for fc in range(8):
                            c.op("pe", lambda e: e.matmul(pd[:, :], lhsT=actT[:, fc, j * P:(j + 1) * P],
                                                          rhs=wd[s][:, fc, hf * 512:(hf + 1) * 512], start=(fc == 0), stop=False),
                                 reads=[actT, wd[s]], writes=[pd])
                        c.op("pe", lambda e: e.matmul(pd[:, :], lhsT=k.onesb[0:1, :], rhs=bd[s][0:1, hf * 512:(hf + 1) * 512],
                                                      start=False, stop=True), reads=[k.onesb, bd[s]], writes=[pd])
                        c.op("act", lambda e: e.activation(out=yo[s][:, j, hf * 512:(hf + 1) * 512], in_=pd[:, :], func=AF.Copy,
                                                           scale=tk[s][:, 2 * j + 1:2 * j + 2].bitcast(F32)),
                             reads=[pd, tk[s]], writes=[yo[s]])
                c.dma("sp", lambda e: e.dma_start(out=ysl_d.ap()[b * BLK:(b + 1) * BLK, :].rearrange("(p j) d -> p j d", j=4),
                                                  in_=yo[s][:, :, :]), reads=[yo[s]], writes=[ysl_d], nowaw=True)
        with c.scope():
            yg = [c.sbuf([P, 4, D], BF16, dma=True) for _ in range(2)]
            xts = [c.sbuf([P, D], F32, dma=True) for _ in range(2)]
            a0 = c.sbuf([P, D], F32)
            a1 = c.sbuf([P, D], F32)
            xo = [c.sbuf([P, D], F32) for _ in range(2)]
            ss = c.sbuf([P, 2], F32)
            if final_g is not None:
                fg = c.sbuf([P, D], F32, dma=True)
                c.dma("sp", lambda e: e.dma_start(out=fg[:, :], in_=final_g.ap().partition_broadcast(P)), writes=[fg])
            for i in range(NT):
                s = i % 2
                for kk in range(4):
                    c.dma("pool", lambda e: e.indirect_dma_start(out=yg[s][:, kk, :], out_offset=None, in_=ysl_d.ap(),
                                                                 in_offset=IOA(ap=s4i[:, i, kk:kk + 1], axis=0)),
                          reads=[s4i, ysl_d], writes=[yg[s]], nowaw=(kk > 0))
                c.dma("sp", lambda e: e.dma_start(out=xts[s][:, :], in_=xmid_d.ap()[i * P:(i + 1) * P, :]),
                      reads=[xmid_d], writes=[xts[s]])
                c.op("dve", lambda e: e.tensor_tensor(out=a0[:, :], in0=yg[s][:, 0, :], in1=yg[s][:, 1, :], op=ALU.add),
                     reads=[yg[s]], writes=[a0])
                c.op("pool", lambda e: e.tensor_tensor(out=a1[:, :], in0=yg[s][:, 2, :], in1=yg[s][:, 3, :], op=ALU.add),
                     reads=[yg[s]], writes=[a1])
                c.op("dve", lambda e: e.tensor_tensor(out=a0[:, :], in0=a0[:, :], in1=a1[:, :], op=ALU.add),
                     reads=[a0, a1], writes=[a0])
                c.op("dve", lambda e: e.tensor_tensor(out=a0[:, :], in0=a0[:, :], in1=modsb[:, 5 * D:6 * D], op=ALU.mult),
                     reads=[a0, modsb], writes=[a0])
                c.op("dve", lambda e: e.tensor_tensor(out=xo[s][:, :], in0=a0[:, :], in1=xts[s][:, :], op=ALU.add),
                     reads=[a0, xts[s]], writes=[xo[s]])
                if final_g is not None:
                    rms_rstd(c, xo[s], a1, ss)
                    c.op("dve", lambda e: e.scalar_tensor_tensor(out=xo[s][:, :], in0=xo[s][:, :], scalar=ss[:, 1:2], in1=fg[:, :],
                                                                 op0=ALU.mult, op1=ALU.mult), reads=[xo[s], ss, fg], writes=[xo[s]])
                c.dma("sp", lambda e: e.dma_start(out=xout_d.ap()[i * P:(i + 1) * P, :], in_=xo[s][:, :]),
                      reads=[xo[s]], writes=[xout_d], nowaw=True)


def moe_scratch(c, T, tag=""):
    NB = (T * 4) // BLK + NE
    return dict(hbuf=c.dram("hbuf" + tag, [T, D], BF16), table=c.dram("table" + tag, [NB * BLK, 2], I32),
                ysl=c.dram("ysl" + tag, [NB * BLK, D], BF16))


MOE_W = dict(router_w=([D, NE], F32), router_b=([1, NE], F32), w_gu=([NE, D, 2 * D], F32), b_gu_r=([NE, P, 16], F32),
             w_down=([NE, D, D], F32), b_down=([NE, D], F32))
CONV_W = dict(pw1=([D, 2 * D], F32), b_pw1=([P, 16], F32), w_dw=([P, 8, 31], F32), b_dw=([P, 8], F32), ln_g=([P, 8], F32),
              ln_b=([P, 8], F32), pw2=([D, D], F32), b_pw2=([1, D], F32))
MOD_W = dict(c_r=([P, 8], F32), mod_w=([D, 6 * D], F32), mod_b=([1, 6 * D], F32), norm_g=([2, D], F32))


def declare(c, specs, kind="ExternalInput", prefix=""):
    return {n: c.dram(prefix + n, shp, dt, kind=kind) for n, (shp, dt) in specs.items()}


def build_conv_layer():
    nc = bass.Bass("TRN2", target_bir_lowering=False)
    c = Ctx(nc)
    cd = declare(c, CONST_SPECS)
    mw = declare(c, MOD_W)
    cw = declare(c, CONV_W)
    ew = declare(c, MOE_W)
    xin = c.dram("xin", [P + TOWN, D], F32, kind="ExternalInput")
    flag = c.dram("flag", [P, 1], F32, kind="ExternalInput")
    xout = c.dram("xout", [TOWN, D], F32, kind="ExternalOutput")
    xmid = c.dram("xmid", [TOWN, D], F32)
    scr = moe_scratch(c, TOWN)
    k = load_consts(c, cd)
    modsb = c.sbuf([P, 6 * D], F32)
    stage_mods(c, k, mw["c_r"], mw["mod_w"], mw["mod_b"], mw["norm_g"], modsb)
    stage_conv(c, k, modsb, xin, flag, cw, xmid)
    stage_moe(c, k, modsb, xmid, ew, scr, xout)
    c.finish([xout])
    c.close()
    return nc


def pcol(v, n):
    return np.ascontiguousarray(np.asarray(v).reshape(n, P).T)


def mod_inputs(inp, l, b):
    return dict(c_r=pcol(inp["c"][b], 8), mod_w=inp["mod_w"][l], mod_b=inp["mod_b"][l][None, :],
                norm_g=np.stack([inp["norm1_g"][l], inp["norm2_g"][l]]))


def moe_inputs(inp, l):
    bgu = inp["moe_b_gu"][l]
    return dict(router_w=inp["moe_router_w"][l], router_b=inp["moe_router_b"][l][None, :], w_gu=inp["moe_w_gu"][l],
                b_gu_r=np.ascontiguousarray(bgu.reshape(NE, 16, P).transpose(0, 2, 1)), w_down=inp["moe_w_down"][l],
                b_down=inp["moe_b_down"][l])


def conv_inputs(inp, l):
    return dict(pw1=inp["conv_w_pw1"][l], b_pw1=pcol(inp["conv_b_pw1"][l], 16),
                w_dw=np.ascontiguousarray(inp["conv_w_dw"][l].reshape(31, 8, P).transpose(2, 1, 0)),
                b_dw=pcol(inp["conv_b_dw"][l], 8), ln_g=pcol(inp["conv_ln_g"][l], 8), ln_b=pcol(inp["conv_ln_b"][l], 8),
                pw2=inp["conv_w_pw2"][l], b_pw2=inp["conv_b_pw2"][l][None, :])


def head_rms(c, src, dst, sq, ssq, gbc, scale):
    for hh in range(16):
        c.op("dve", lambda e: e.scalar_tensor_tensor(out=sq[:, hh * 64:(hh + 1) * 64], in0=src[:, hh * 64:(hh + 1) * 64], scalar=1.0,
                                                     in1=src[:, hh * 64:(hh + 1) * 64], op0=ALU.mult, op1=ALU.mult,
                                                     accum_out=ssq[:, hh:hh + 1]), reads=[src], writes=[sq, ssq])
    c.op("dve", lambda e: e.tensor_scalar(out=ssq[:, 0:16], in0=ssq[:, 0:16], scalar1=1.0 / 64, scalar2=EPS, op0=ALU.mult,
                                          op1=ALU.add), reads=[ssq], writes=[ssq])
    c.op("dve", lambda e: e.reciprocal(out=ssq[:, 0:16], in_=ssq[:, 0:16]), reads=[ssq], writes=[ssq])
    c.op("act", lambda e: e.activation(out=ssq[:, 0:16], in_=ssq[:, 0:16], func=AF.Sqrt, scale=scale * scale),
         reads=[ssq], writes=[ssq])
    for hh in range(16):
        c.op("dve", lambda e: e.scalar_tensor_tensor(out=dst[:, hh * 64:(hh + 1) * 64], in0=src[:, hh * 64:(hh + 1) * 64],
                                                     scalar=ssq[:, hh:hh + 1], in1=gbc[:, 0:64], op0=ALU.mult, op1=ALU.mult),
             reads=[src, ssq, gbc], writes=[dst])


def pair_transpose_store(c, k, srcb, ptb, stg, dst_d, tok0, rows=64):
    for kc in range(8):
        c.op("pe", lambda e: e.transpose(out=ptb[:, kc * P:(kc + 1) * P], in_=srcb[:, kc * P:(kc + 1) * P], identity=k.identb[:, :]),
             reads=[srcb, k.identb], writes=[ptb])
    c.op("act", lambda e: e.activation(out=stg[:, :, :], in_=ptb[:, :].rearrange("p (k t) -> p k t", k=8), func=AF.Copy),
         reads=[ptb], writes=[stg])
    for kc in range(8):
        for half in range(2):
            c.dma("sp", lambda e: e.dma_start(out=dst_d.ap()[2 * kc + half, 0:64, tok0:tok0 + P], in_=stg[half * 64:(half + 1) * 64, kc, :]),
                  reads=[stg], writes=[dst_d], nowaw=True)


KV_W = dict(c_r=([P, 8], F32), mod_w=([D, 2 * D], F32), mod_b=([1, 2 * D], F32), norm_g=([1, D], F32),
            w_kvf=([D, 2 * D + 16], F32), b_f=([1, 16], F32), k_g=([1, 64], F32))


def build_kv():
    nc = bass.Bass("TRN2", target_bir_lowering=False)
    c = Ctx(nc)
    cd = declare(c, CONST_SPECS)
    w = declare(c, KV_W)
    xin = c.dram("xin", [TOWN, D], F32, kind="ExternalInput")
    kT_d = c.dram("kT", [16, 64, TOWN], BF16, kind="ExternalOutput")
    v_d = c.dram("v", [TOWN, 16, 72], BF16, kind="ExternalOutput")
    lf_d = c.dram("logf", [TOWN, 16], F32, kind="ExternalOutput")
    k = load_consts(c, cd)
    modsb = c.sbuf([P, 2 * D], F32)
    stage_mods(c, k, w["c_r"], w["mod_w"], w["mod_b"], w["norm_g"], modsb, ncols=2 * D, nnorm=1)
    with c.scope():
        wkv = c.sbuf([P, 8, 2 * D], BF16, dma=True)
        wf = c.sbuf([P, 8, 16], F32, dma=True)
        bfb = c.sbuf([P, 16], F32, dma=True)
        kgb = c.sbuf([P, 64], F32, dma=True)
        wv = w["w_kvf"].ap().rearrange("(kc p) n -> p kc n", p=P)
        for kc in range(8):
            c.dma("pool", lambda e: e.dma_start(out=wkv[:, kc, :], in_=wv[:, kc, 0:2 * D]), writes=[wkv], nowaw=True)
        c.dma("sp", lambda e: e.dma_start(out=wf[:, :, :], in_=wv[:, :, 2 * D:2 * D + 16]), writes=[wf])
        c.dma("sp", lambda e: e.dma_start(out=bfb[:, :], in_=w["b_f"].ap().partition_broadcast(P)), writes=[bfb])
        c.dma("sp", lambda e: e.dma_start(out=kgb[:, :], in_=w["k_g"].ap().partition_broadcast(P)), writes=[kgb])
        xts = [c.sbuf([P, D], F32, dma=True) for _ in range(2)]
        h = c.sbuf([P, D], F32)
        ss = c.sbuf([P, 2], F32)
        hT32 = c.sbuf([P, 8, P], F32)
        hTb = c.sbuf([P, 8, P], BF16)
        ksb = c.sbuf([P, D], F32)
        sq = c.sbuf([P, D], F32)
        ssq = c.sbuf([P, 16], F32)
        kn = c.sbuf([P, D], BF16)
        stg = [c.sbuf([P, 8, P], BF16) for _ in range(2)]
        vt = [c.sbuf([P, 16, 72], BF16) for _ in range(2)]
        fz = [c.sbuf([P, 16], F32) for _ in range(2)]
        pT = c.psum([P, D], F32)
        ptb = c.psum([P, D], BF16)
        pk = [c.psum([P, 512], F32) for _ in range(2)]
        pf = c.psum([P, 16], F32)
        for t in vt:
            c.op("dve", lambda e: e.memset(t[:, :, :], 1.0), writes=[t])
        LV = int(os.environ.get("KVSTOP", "9"))
        for i in range(int(os.environ.get("KVTILES", TOWN // P)) if LV > 2 else 0):
            xt = xts[i % 2]
            c.dma("sp", lambda e: e.dma_start(out=xt[:, :], in_=xin.ap()[i * P:(i + 1) * P, :]), writes=[xt])
            norm_mod(c, xt, h, ss, 1 * D, 0, modsb)
            if LV == 3:
                continue
            for kc in range(8):
                c.op("pe", lambda e: e.transpose(out=pT[:, kc * P:(kc + 1) * P], in_=h[:, kc * P:(kc + 1) * P], identity=k.identf[:, :]),
                     reads=[h, k.identf], writes=[pT])
            c.op("act", lambda e: e.activation(out=hT32[:, :, :], in_=pT[:, :].rearrange("p (k t) -> p k t", k=8), func=AF.Copy),
                 reads=[pT], writes=[hT32])
            c.op("dve", lambda e: e.tensor_copy(out=hTb[:, :, :], in_=hT32[:, :, :]), reads=[hT32], writes=[hTb])
            if LV == 4:
                continue
            for hf in range(2):
                for kc in range(8):
                    c.op("pe", lambda e: e.matmul(pk[hf][:, :], lhsT=hTb[:, kc, :], rhs=wkv[:, kc, hf * 512:(hf + 1) * 512],
                                                  start=(kc == 0), stop=(kc == 7)), reads=[hTb, wkv], writes=[pk[hf]])
                c.op("act", lambda e: e.activation(out=ksb[:, hf * 512:(hf + 1) * 512], in_=pk[hf][:, :], func=AF.Copy),
                     reads=[pk[hf]], writes=[ksb])
            if LV == 5:
                continue
            SK = os.environ.get("KVSKIP", "")
            if "k" not in SK:
                head_rms(c, ksb, kn, sq, ssq, kgb, 1.0)
            if "t" not in SK:
                pair_transpose_store(c, k, kn, ptb, stg[i % 2], kT_d, i * P)
            v = vt[i % 2]
            for hf in range(2 if "v" not in SK else 0):
                for kc in range(8):
                    c.op("pe", lambda e: e.matmul(pk[hf][:, :], lhsT=hTb[:, kc, :], rhs=wkv[:, kc, D + hf * 512:D + (hf + 1) * 512],
                                                  start=(kc == 0), stop=(kc == 7)), reads=[hTb, wkv], writes=[pk[hf]])
                c.op("act", lambda e: e.activation(out=v[:, hf * 8:(hf + 1) * 8, 0:64],
                                                   in_=pk[hf][:, :].rearrange("p (h d) -> p h d", d=64), func=AF.Copy),
                     reads=[pk[hf]], writes=[v])
            c.dma("sp", lambda e: e.dma_start(out=v_d.ap()[i * P:(i + 1) * P, :, :], in_=v[:, :, :]), reads=[v], writes=[v_d], nowaw=True)
            if "f" in SK:
                continue
            f = fz[i % 2]
            for kc in range(8):
                c.op("pe", lambda e: e.matmul(pf[:, :], lhsT=hT32[:, kc, :], rhs=wf[:, kc, :], start=(kc == 0), stop=(kc == 7)),
                     reads=[hT32, wf], writes=[pf])
            c.op("dve", lambda e: e.tensor_tensor(out=f[:, :], in0=pf[:, :], in1=bfb[:, :], op=ALU.add), reads=[pf, bfb], writes=[f])
            c.op("act", lambda e: e.activation(out=f[:, :], in_=f[:, :], func=AF.Exp, scale=-1.0), reads=[f], writes=[f])
            c.op("act", lambda e: e.activation(out=f[:, :], in_=f[:, :], func=AF.Ln, bias=1.0), reads=[f], writes=[f])
            c.op("dve", lambda e: e.tensor_scalar(out=f[:, :], in0=f[:, :], scalar1=-1.0, scalar2=None, op0=ALU.mult), reads=[f], writes=[f])
            c.dma("sp", lambda e: e.dma_start(out=lf_d.ap()[i * P:(i + 1) * P, :], in_=f[:, :]), reads=[f], writes=[lf_d], nowaw=True)
    c.finish([])
    c.close()
    return nc


def build_fcum():
    NTL = SEQ // P
    nc = bass.Bass("TRN2", target_bir_lowering=False)
    c = Ctx(nc)
    cd = declare(c, CONST_SPECS)
    lf_d = c.dram("logf", [SEQ, 16], F32, kind="ExternalInput")
    ft_d = c.dram("ft", [16, 3, SEQ], BF16, kind="ExternalOutput")
    fn_d = c.dram("ftn", [16, 3, SEQ], BF16, kind="ExternalOutput")
    k = load_consts(c, cd)
    with c.scope():
        lf = c.sbuf([P, NTL, 16], F32, dma=True)
        F = c.sbuf([16, SEQ], F32)
        R = c.sbuf([16, 4096], F32)
        base = c.sbuf([16, NTL + 1], F32)
        sp3s = [c.sbuf([16, 3, 4096], BF16) for _ in range(2)]
        ps = [c.psum([16, 512], F32) for _ in range(2)]
        c.dma("sp", lambda e: e.dma_start(out=lf[:, :, :], in_=lf_d.ap().rearrange("(n p) h -> p n h", p=P)), writes=[lf])
        for g in range(NTL // 4):
            pg = ps[g % 2]
            for q in range(4):
                n = g * 4 + q
                c.op("pe", lambda e: e.matmul(pg[:, q * P:(q + 1) * P], lhsT=lf[:, n, :], rhs=k.tril[:, :], start=True, stop=True),
                     reads=[lf, k.tril], writes=[pg])
            c.op("act", lambda e: e.activation(out=F[:, g * 512:(g + 1) * 512], in_=pg[:, :], func=AF.Copy), reads=[pg], writes=[F])
        c.op("dve", lambda e: e.memset(base[:, 0:1], 0.0), writes=[base])
        for n in range(NTL):
            c.op("dve", lambda e: e.tensor_tensor(out=base[:, n + 1:n + 2], in0=base[:, n:n + 1], in1=F[:, n * P + P - 1:n * P + P],
                                                  op=ALU.add), reads=[base, F], writes=[base])
        for n in range(1, NTL):
            c.op("dve", lambda e: e.tensor_scalar(out=F[:, n * P:(n + 1) * P], in0=F[:, n * P:(n + 1) * P], scalar1=base[:, n:n + 1],
                                                  scalar2=None, op0=ALU.add), reads=[F, base], writes=[F])
        CH = 4096
        for ci in range(SEQ // CH):
            Fc = F[:, ci * CH:(ci + 1) * CH]
            sp = sp3s[ci % 2]
            c.op("dve", lambda e: e.tensor_copy(out=sp[:, 0, :], in_=Fc), reads=[F], writes=[sp])
            c.op("dve", lambda e: e.tensor_tensor(out=R[:, :], in0=Fc, in1=sp[:, 0, :], op=ALU.subtract), reads=[F, sp], writes=[R])
            c.op("dve", lambda e: e.tensor_copy(out=sp[:, 1, :], in_=R[:, :]), reads=[R], writes=[sp])
            c.op("dve", lambda e: e.tensor_tensor(out=R[:, :], in0=R[:, :], in1=sp[:, 1, :], op=ALU.subtract), reads=[R, sp], writes=[R])
            c.op("dve", lambda e: e.tensor_copy(out=sp[:, 2, :], in_=R[:, :]), reads=[R], writes=[sp])
            c.dma("sp", lambda e: e.dma_start(out=ft_d.ap()[:, :, ci * CH:(ci + 1) * CH], in_=sp[:, :, :]), reads=[sp], writes=[ft_d], nowaw=True)
            for q in range(3):
                c.op("dve", lambda e: e.tensor_scalar(out=sp[:, q, :], in0=sp[:, q, :], scalar1=-1.0, scalar2=None, op0=ALU.mult),
                     reads=[sp], writes=[sp])
            c.dma("sp", lambda e: e.dma_start(out=fn_d.ap()[:, :, ci * CH:(ci + 1) * CH], in_=sp[:, :, :]), reads=[sp], writes=[fn_d], nowaw=True)
    c.finish([])
    c.close()
    return nc


ATT_W = dict(w_qg=([D, 2 * D], F32), q_g=([1, 64], F32), w_o=([D, D], F32))
NQB = TOWN // 512


def stage_attn(c, k, modsb, xin_d, w, kf_d, v_d, qf_d, mask_d, xmid_d):
    qT_d = c.dram("qT_s", [16, 70, TOWN], BF16)
    sg_d = c.dram("sgT_s", [16, 64, TOWN], BF16)
    og_d = c.dram("ogT_s", [16, 64, TOWN], BF16)
    c.dma("sp", lambda e: e.dma_start(out=qT_d.ap()[:, 64:70, :], in_=qf_d.ap()), writes=[qT_d])
    with c.scope():
        wq = c.sbuf([P, 8, 2 * D], BF16, dma=True)
        qgb = c.sbuf([P, 64], F32, dma=True)
        wv = w["w_qg"].ap().rearrange("(kc p) n -> p kc n", p=P)
        for kc in range(8):
            c.dma("pool", lambda e: e.dma_start(out=wq[:, kc, :], in_=wv[:, kc, :]), writes=[wq], nowaw=True)
        c.dma("sp", lambda e: e.dma_start(out=qgb[:, :], in_=w["q_g"].ap().partition_broadcast(P)), writes=[qgb])
        xts = [c.sbuf([P, D], F32, dma=True) for _ in range(2)]
        h = c.sbuf([P, D], F32)
        ss = c.sbuf([P, 2], F32)
        hTb = c.sbuf([P, 8, P], BF16)
        qsb = c.sbuf([P, D], F32)
        sq = c.sbuf([P, D], F32)
        ssq = c.sbuf([P, 16], F32)
        qn = c.sbuf([P, D], BF16)
        sgn = c.sbuf([P, D], BF16)
        stg = [c.sbuf([P, 8, P], BF16) for _ in range(4)]
        pT = c.psum([P, D], F32)
        ptb = c.psum([P, D], BF16)
        pk = [c.psum([P, 512], F32) for _ in range(2)]
        for i in range(TOWN // P):
            xt = xts[i % 2]
            c.dma("sp", lambda e: e.dma_start(out=xt[:, :], in_=xin_d.ap()[i * P:(i + 1) * P, :]), writes=[xt])
            norm_mod(c, xt, h, ss, 1 * D, 0, modsb)
            for kc in range(8):
                c.op("pe", lambda e: e.transpose(out=pT[:, kc * P:(kc + 1) * P], in_=h[:, kc * P:(kc + 1) * P], identity=k.identf[:, :]),
                     reads=[h, k.identf], writes=[pT])
            c.op("act", lambda e: e.activation(out=hTb[:, :, :], in_=pT[:, :].rearrange("p (k t) -> p k t", k=8), func=AF.Copy),
                 reads=[pT], writes=[hTb])
            for hf in range(2):
                for kc in range(8):
                    c.op("pe", lambda e: e.matmul(pk[hf][:, :], lhsT=hTb[:, kc, :], rhs=wq[:, kc, hf * 512:(hf + 1) * 512],
                                                  start=(kc == 0), stop=(kc == 7)), reads=[hTb, wq], writes=[pk[hf]])
                c.op("act", lambda e: e.activation(out=qsb[:, hf * 512:(hf + 1) * 512], in_=pk[hf][:, :], func=AF.Copy),
                     reads=[pk[hf]], writes=[qsb])
            head_rms(c, qsb, qn, sq, ssq, qgb, 0.125)
            pair_transpose_store(c, k, qn, ptb, stg[(2 * i) % 4], qT_d, i * P)
            for hf in range(2):
                for kc in range(8):
                    c.op("pe", lambda e: e.matmul(pk[hf][:, :], lhsT=hTb[:, kc, :], rhs=wq[:, kc, D + hf * 512:D + (hf + 1) * 512],
                                                  start=(kc == 0), stop=(kc == 7)), reads=[hTb, wq], writes=[pk[hf]])
                c.op("act", lambda e: e.activation(out=sgn[:, hf * 512:(hf + 1) * 512], in_=pk[hf][:, :], func=AF.Sigmoid),
                     reads=[pk[hf]], writes=[sgn])
            pair_transpose_store(c, k, sgn, ptb, stg[(2 * i + 1) % 4], sg_d, i * P)
    with c.scope():
        NKT = SEQ // P
        kf = [c.sbuf([70, SEQ], BF16, dma=True) for _ in range(2)]
        vv = [c.sbuf([P, NKT, 72], BF16, dma=True) for _ in range(2)]
        qq = [c.sbuf([70, TOWN], BF16, dma=True) for _ in range(2)]
        sgh = [c.sbuf([64, TOWN], BF16, dma=True) for _ in range(2)]
        mk = c.sbuf([P, 16, 512], BF16, dma=True)
        ssb = [c.sbuf([P, 512], F32) for _ in range(3)]
        pTb = [c.sbuf([P, 512], BF16) for _ in range(4)]
        rinv = c.sbuf([P, 512], F32)
        bcs = c.sbuf([64, 512], F32)
        on = c.sbuf([64, 512], F32)
        og = [c.sbuf([64, 512], BF16) for _ in range(2)]
        pS = [c.psum([P, 512], F32) for _ in range(4)]
        pO = [c.psum([P, 512], F32) for _ in range(2)]
        pB = c.psum([64, 512], F32)
        c.dma("sp", lambda e: e.dma_start(out=mk[:, :, :], in_=mask_d.ap()), writes=[mk])

        def load_head(hh):
            s = hh % 2
            c.dma("sp", lambda e: e.dma_start(out=kf[s][:, :], in_=kf_d.ap()[hh]), writes=[kf[s]])
            c.dma("sp", lambda e: e.dma_start(out=vv[s][:, :, :], in_=v_d.ap()[hh]), writes=[vv[s]])
            c.dma("sp", lambda e: e.dma_start(out=qq[s][:, :], in_=qT_d.ap()[hh]), reads=[qT_d], writes=[qq[s]])
            c.dma("sp", lambda e: e.dma_start(out=sgh[s][:, :], in_=sg_d.ap()[hh]), reads=[sg_d], writes=[sgh[s]])

        load_head(0)
        NH_RUN = int(os.environ.get("ATT_HEADS", "16"))
        NS, LA = 4, 2
        units = []
        nq = 0
        for hh in range(NH_RUN):
            for j in range(NQB):
                nkt = 16 * (j + 1)
                for kt in range(nkt):
                    units.append((hh, j, kt, nkt, nq))
                nq += 1
        epi = {}

        def emit_qk(i):
            hh, j, kt, nkt, nq_ = units[i]
            s = hh % 2
            ps_ = pS[i % NS]
            c.op("pe", lambda e: e.matmul(ps_[:, :], lhsT=kf[s][:, kt * P:(kt + 1) * P], rhs=qq[s][:, j * 512:(j + 1) * 512],
                                          start=True, stop=True), reads=[kf[s], qq[s]], writes=[ps_])
            if kt >= 16 * j:
                sb_ = ssb[i % 3]
                c.op("dve", lambda e: e.tensor_tensor(out=sb_[:, :], in0=ps_[:, :], in1=mk[:, kt - 16 * j, :], op=ALU.add),
                     reads=[ps_, mk], writes=[sb_])

        def emit_rest(i):
            hh, j, kt, nkt, nq_ = units[i]
            s = hh % 2
            if j == 0 and kt == 6 and hh + 1 < NH_RUN:
                load_head(hh + 1)
            ps_ = pS[i % NS]
            pt_ = pTb[i % NS]
            po = pO[nq_ % 2]
            if kt >= 16 * j:
                sb_ = ssb[i % 3]
                c.op("act", lambda e: e.activation(out=pt_[:, :], in_=sb_[:, :], func=AF.Exp), reads=[sb_], writes=[pt_])
            else:
                c.op("act", lambda e: e.activation(out=pt_[:, :], in_=ps_[:, :], func=AF.Exp), reads=[ps_], writes=[pt_])
            c.op("pe", lambda e: e.matmul(po[0:65, :], lhsT=vv[s][:, kt, 0:65], rhs=pt_[:, :], start=(kt == 0), stop=(kt == nkt - 1)),
                 reads=[vv[s], pt_], writes=[po])
            if kt == nkt - 1:
                c.op("dve", lambda e: e.reciprocal(out=rinv[64:65, :], in_=po[64:65, :]), reads=[po], writes=[rinv])
                epi[i + 2] = (hh, j, nq_)

        def emit_epi(hh, j, nq_):
            s = hh % 2
            po = pO[nq_ % 2]
            o_ = og[nq_ % 2]
            c.op("pe", lambda e: e.matmul(pB[:, :], lhsT=k.onesf[64:65, 0:64], rhs=rinv[64:65, :], start=True, stop=True),
                 reads=[k.onesf, rinv], writes=[pB])
            c.op("act", lambda e: e.activation(out=bcs[:, :], in_=pB[:, :], func=AF.Copy), reads=[pB], writes=[bcs])
            c.op("dve", lambda e: e.tensor_tensor(out=on[:, :], in0=po[0:64, :], in1=bcs[:, :], op=ALU.mult), reads=[po, bcs], writes=[on])
            c.op("pool", lambda e: e.tensor_tensor(out=o_[:, :], in0=on[:, :], in1=sgh[s][:, j * 512:(j + 1) * 512], op=ALU.mult),
                 reads=[on, sgh[s]], writes=[o_])
            c.dma("sp", lambda e: e.dma_start(out=og_d.ap()[hh, :, j * 512:(j + 1) * 512], in_=o_[:, :]), reads=[o_], writes=[og_d], nowaw=True)

        NU = len(units)
        for idx in range(NU + LA + 3):
            if idx < NU:
                emit_qk(idx)
            if 0 <= idx - LA < NU:
                emit_rest(idx - LA)
            if (idx - LA) in epi:
                emit_epi(*epi.pop(idx - LA))
        assert not epi
    with c.scope():
        wo = c.sbuf([64, 16, D], BF16, dma=True)
        c.dma("pool", lambda e: e.dma_start(out=wo[:, :, :], in_=w["w_o"].ap().rearrange("(h d) n -> d h n", d=64)), writes=[wo])
        ogt = [c.sbuf([64, 16, P], BF16, dma=True) for _ in range(2)]
        xrs = [c.sbuf([P, D], F32, dma=True) for _ in range(2)]
        xos = [c.sbuf([P, D], F32) for _ in range(2)]
        tmp = c.sbuf([P, 512], F32)
        py = [c.psum([P, 512], F32) for _ in range(2)]
        for i in range(TOWN // P):
            o_ = ogt[i % 2]
            xr = xrs[i % 2]
            xo = xos[i % 2]
            c.dma("sp", lambda e: e.dma_start(out=o_[:, :, :], in_=og_d.ap()[:, :, i * P:(i + 1) * P].rearrange("h d t -> d h t")),
                  reads=[og_d], writes=[o_])
            c.dma("sp", lambda e: e.dma_start(out=xr[:, :], in_=xin_d.ap()[i * P:(i + 1) * P, :]), writes=[xr])
            for hf in range(2):
                for hh in range(16):
                    c.op("pe", lambda e: e.matmul(py[hf][:, :], lhsT=o_[:, hh, :], rhs=wo[:, hh, hf * 512:(hf + 1) * 512],
                                                  start=(hh == 0), stop=(hh == 15)), reads=[o_, wo], writes=[py[hf]])
                c.op("dve", lambda e: e.tensor_tensor(out=tmp[:, :], in0=py[hf][:, :], in1=modsb[:, 2 * D + hf * 512:2 * D + (hf + 1) * 512],
                                                      op=ALU.mult), reads=[py[hf], modsb], writes=[tmp])
                c.op("dve", lambda e: e.tensor_tensor(out=xo[:, hf * 512:(hf + 1) * 512], in0=tmp[:, :], in1=xr[:, hf * 512:(hf + 1) * 512],
                                                      op=ALU.add), reads=[tmp, xr], writes=[xo])
            c.dma("sp", lambda e: e.dma_start(out=xmid_d.ap()[i * P:(i + 1) * P, :], in_=xo[:, :]), reads=[xo], writes=[xmid_d], nowaw=True)


def build_attn_layer(final):
    nc = bass.Bass("TRN2", target_bir_lowering=False)
    c = Ctx(nc)
    cd = declare(c, CONST_SPECS)
    mw = declare(c, MOD_W)
    aw = declare(c, ATT_W)
    ew = declare(c, MOE_W)
    xin = c.dram("xin", [TOWN, D], F32, kind="ExternalInput")
    kf_d = c.dram("kf", [16, 70, SEQ], BF16, kind="ExternalInput")
    v_d = c.dram("vh", [16, P, SEQ // P, 72], BF16, kind="ExternalInput")
    qf_d = c.dram("qf", [16, 6, TOWN], BF16, kind="ExternalInput")
    mask_d = c.dram("maskadd", [P, 16, 512], BF16, kind="ExternalInput")
    fg = c.dram("final_g", [1, D], F32, kind="ExternalInput") if final else None
    xout = c.dram("xout", [TOWN, D], F32, kind="ExternalOutput")
    xmid = c.dram("xmid", [TOWN, D], F32)
    scr = moe_scratch(c, TOWN)
    k = load_consts(c, cd)
    modsb = c.sbuf([P, 6 * D], F32)
    stage_mods(c, k, mw["c_r"], mw["mod_w"], mw["mod_b"], mw["norm_g"], modsb)
    stage_attn(c, k, modsb, xin, aw, kf_d, v_d, qf_d, mask_d, xmid)
    stage_moe(c, k, modsb, xmid, ew, scr, xout, final_g=fg)
    c.finish([xout])
    c.close()
    return nc


_PROGS = {}


def _prog(name, fn, *a):
    if name not in _PROGS:
        _PROGS[name] = fn(*a)
    return _PROGS[name]


def _run(nc, in_maps):
    res = run_bass_kernel_spmd(nc, in_maps, core_ids=list(range(NCORES)))
    return res.results


def kernel(**inp):
    inp = {k_: np.asarray(v_) for k_, v_ in inp.items()}
    hc = host_consts()
    x = inp["x"]
    xs = [x[b] for b in range(2)]
    for l in range(2):
        maps = []
        for core in range(NCORES):
            b, r = divmod(core, 4)
            t0 = r * TOWN
            xin = np.zeros((P + TOWN, D), np.float32)
            xin[P:] = xs[b][t0:t0 + TOWN]
            if r > 0:
                xin[:P] = xs[b][t0 - P:t0]
            m = dict(hc)
            m.update(mod_inputs(inp, l, b)); m.update(conv_inputs(inp, l)); m.update(moe_inputs(inp, l))
            m["xin"] = xin
            m["flag"] = np.full((P, 1), 1.0 if r > 0 else 0.0, np.float32)
            maps.append(m)
        res = _run(_prog("conv", build_conv_layer), maps)
        xs = [np.concatenate([res[b * 4 + r]["xout"] for r in range(4)], axis=0) for b in range(2)]
    maps = []
    for core in range(NCORES):
        b, r = divmod(core, 4)
        m = dict(hc)
        m.update(dict(c_r=pcol(inp["c"][b], 8), mod_w=inp["kv_mod_w"], mod_b=inp["kv_mod_b"][None, :],
                      norm_g=inp["kv_norm_g"][None, :], w_kvf=inp["w_kvf"], b_f=inp["b_f"][None, :], k_g=inp["k_norm_g"][None, :]))
        m["xin"] = np.ascontiguousarray(xs[b][r * TOWN:(r + 1) * TOWN])
        maps.append(m)
    res = _run(_prog("kv", build_kv), maps)
    kT = [np.concatenate([res[b * 4 + r]["kT"] for r in range(4)], axis=2) for b in range(2)]
    vv = [np.concatenate([res[b * 4 + r]["v"] for r in range(4)], axis=0) for b in range(2)]
    lf = [np.concatenate([res[b * 4 + r]["logf"] for r in range(4)], axis=0) for b in range(2)]
    maps = []
    for core in range(NCORES):
        m = dict(hc)
        m["logf"] = np.ascontiguousarray(lf[core % 2])
        maps.append(m)
    res = _run(_prog("fcum", build_fcum), maps)
    ft = [res[b]["ft"] for b in range(2)]
    ftn = [res[b]["ftn"] for b in range(2)]
    one = np.ones((), np.float32).astype(ml_dtypes.bfloat16)
    kf, vh = [], []
    for b in range(2):
        a = np.empty((16, 70, SEQ), ml_dtypes.bfloat16)
        a[:, 0:64] = kT[b]
        a[:, 64:67] = one
        a[:, 67:70] = ftn[b]
        kf.append(a)
        vh.append(np.ascontiguousarray(vv[b].reshape(SEQ // P, P, 16, 72).transpose(2, 1, 0, 3)))
    ii = np.arange(P)
    masks = []
    for r in range(4):
        mk = np.zeros((P, 16, 512), np.float32)
        for tz in range(16):
            spos = (tz // 4) * 512 + (tz % 4) * P + ii[:, None]
            tpos = r * 512 + np.arange(512)[None, :]
            mk[:, tz, :] = np.where(spos <= tpos, 0.0, -30000.0)
        masks.append(mk.astype(ml_dtypes.bfloat16))
    def own_rows(r):
        return np.concatenate([np.arange((4 * j + r) * 512, (4 * j + r + 1) * 512) for j in range(NQB)])
    for li in range(2):
        l = 2 + li
        final = (li == 1)
        maps = []
        for core in range(NCORES):
            b, r = divmod(core, 4)
            rows = own_rows(r)
            m = dict(hc)
            m.update(mod_inputs(inp, l, b)); m.update(moe_inputs(inp, l))
            m.update(dict(w_qg=inp["attn_w_qg"][li], q_g=inp["q_norm_g"][li][None, :], w_o=inp["attn_w_o"][li]))
            m["xin"] = np.ascontiguousarray(xs[b][rows])
            m["kf"] = kf[b]
            m["vh"] = vh[b]
            qf = np.empty((16, 6, TOWN), ml_dtypes.bfloat16)
            qf[:, 0:3] = ft[b][:, :, rows]
            qf[:, 3:6] = one
            m["qf"] = qf
            m["maskadd"] = masks[r]
            if final:
                m["final_g"] = inp["final_norm_g"][None, :]
            maps.append(m)
        res = _run(_prog("attn%d" % final, build_attn_layer, final), maps)
        nx = [np.empty((SEQ, D), np.float32) for _ in range(2)]
        for core in range(NCORES):
            b, r = divmod(core, 4)
            nx[b][own_rows(r)] = res[core]["xout"]
        xs = nx
    return np.stack(xs, axis=0).astype(np.float32)
```

```python
import os
import numpy as np
import ml_dtypes
import concourse.bass as bass
import concourse.mybir as mybir
from concourse.bass_utils import run_bass_kernel_spmd
from contextlib import ExitStack, contextmanager

F32 = mybir.dt.float32
BF16 = mybir.dt.bfloat16
I32 = mybir.dt.int32
ALU = mybir.AluOpType
AF = mybir.ActivationFunctionType
IOA = bass.IndirectOffsetOnAxis

P = 128
D = 1024
NE = 32
EPS = 1e-6
NCORES = 8
SEQ = 16384
TOWN = 4096
BLK = 512
SAME_ENGINE_INORDER = tuple(os.environ.get("INORDER", "pe,sp").split(","))
CONV_CHAIN = os.environ.get("CONV_CHAIN", "1") == "1"


class Buf:
    def __init__(self, t, name, dma_sem_key=None):
        self.t = t
        self.name = name
        self.last_w = None
        self.reads = []
        self.dma_key = dma_sem_key
        self.dma_cnt = 0

    def __getitem__(self, idx):
        return self.t[idx]

    def ap(self):
        return self.t.ap()


class Eng:
    def __init__(self, name, h):
        self.name = name
        self.h = h
        self.key = "e_" + name
        self.cnt = 0
        self.waited = {}


class Ctx:
    def __init__(self, nc):
        self.nc = nc
        self.root = ExitStack()
        self.stack = [self.root]
        self.sems = {}
        self.engs = {}
        self.free_dma_sems = []
        self.live_dma = {}
        for name, h in (("pe", nc.tensor), ("act", nc.scalar), ("dve", nc.vector),
                        ("pool", nc.gpsimd), ("sp", nc.sync)):
            e = Eng(name, h)
            self.sems[e.key] = self.root.enter_context(nc.semaphore(e.key))
            self.engs[name] = e
        self.nbuf = 0
        self.n_inst = 0
        self.sem_cnt = {}

    def _dma_key(self):
        if self.free_dma_sems:
            return self.free_dma_sems.pop()
        key = f"d{len(self.sems)}"
        self.sems[key] = self.root.enter_context(self.nc.semaphore(key))
        self.sem_cnt[key] = 0
        return key

    def sbuf(self, shape, dtype=F32, dma=False, name=None):
        self.nbuf += 1
        name = name or f"sb{self.nbuf}"
        t = self.stack[-1].enter_context(self.nc.sbuf_tensor(name, list(shape), dtype))
        b = Buf(t, name)
        if dma:
            b.dma_key = self._dma_key()
            b.dma_cnt = self.sem_cnt[b.dma_key]
            self.scope_keys[-1].append(b.dma_key) if self.scope_keys else None
        return b

    def psum(self, shape, dtype=F32, name=None):
        self.nbuf += 1
        name = name or f"ps{self.nbuf}"
        t = self.stack[-1].enter_context(self.nc.psum_tensor(name, list(shape), dtype))
        return Buf(t, name)

    def dram(self, name, shape, dtype, kind="Internal"):
        t = self.nc.dram_tensor(name, list(shape), dtype, kind=kind)
        return Buf(t, name)

    scope_keys = []

    @contextmanager
    def scope(self):
        es = ExitStack()
        self.stack.append(es)
        self.scope_keys.append([])
        try:
            yield
        finally:
            self.barrier()
            keys = self.scope_keys.pop()
            self.free_dma_sems.extend(keys)
            self.stack.pop()
            es.close()

    def barrier(self):
        for e in self.engs.values():
            for o in self.engs.values():
                if o is not e and o.cnt > 0:
                    self._wait(e, (o.key, o.cnt))
            for key, cnt in self.sem_cnt.items():
                if cnt > 0:
                    self._wait(e, (key, cnt))

    def _wait(self, eng, tok):
        if tok is None:
            return
        key, val = tok
        if key == eng.key and eng.name in SAME_ENGINE_INORDER:
            return
        if eng.waited.get(key, 0) >= val:
            return
        eng.waited[key] = val
        eng.h.wait_ge(self.sems[key], val)

    def _deps(self, eng, reads, writes, nowaw=False):
        for r in reads:
            self._wait(eng, r.last_w)
        for w in writes:
            if not nowaw:
                self._wait(eng, w.last_w)
            for tok in w.reads:
                self._wait(eng, tok)

    def _commit(self, tok, reads, writes):
        for r in reads:
            r.reads.append(tok)
            if len(r.reads) > 48:
                best = {}
                for k, v in r.reads:
                    best[k] = max(best.get(k, 0), v)
                r.reads = list(best.items())
        for w in writes:
            w.last_w = tok
            w.reads = []

    def op(self, eng_name, fn, reads=(), writes=(), chain=False):
        eng = self.engs[eng_name]
        if chain:
            eng.waited[eng.key] = max(eng.waited.get(eng.key, 0), eng.cnt)
        self._deps(eng, reads, writes)
        inst = fn(eng.h)
        eng.cnt += 1
        inst.then_inc(self.sems[eng.key], 1)
        self._commit((eng.key, eng.cnt), reads, writes)
        self.n_inst += 1

    def dma(self, eng_name, fn, reads=(), writes=(), nowaw=False):
        eng = self.engs[eng_name]
        self._deps(eng, reads, writes, nowaw)
        sb = writes[0]
        if sb.dma_key is None:
            sb.dma_key = self._dma_key()
        inst = fn(eng.h)
        self.sem_cnt[sb.dma_key] += 16
        inst.then_inc(self.sems[sb.dma_key], 16)
        self._commit((sb.dma_key, self.sem_cnt[sb.dma_key]), reads, writes)
        self.n_inst += 1

    def finish(self, bufs):
        self.barrier()

    def close(self):
        self.root.close()


def rms_rstd(c, xt, junk, ss):
    c.op("act", lambda e: e.activation(out=junk[:, :], in_=xt[:, :], func=AF.Square, accum_out=ss[:, 0:1]),
         reads=[xt], writes=[junk, ss])
    c.op("dve", lambda e: e.tensor_scalar(out=ss[:, 1:2], in0=ss[:, 0:1], scalar1=1.0 / D, scalar2=EPS,
                                          op0=ALU.mult, op1=ALU.add), reads=[ss], writes=[ss])
    c.op("dve", lambda e: e.reciprocal(out=ss[:, 1:2], in_=ss[:, 1:2]), reads=[ss], writes=[ss])
    c.op("act", lambda e: e.activation(out=ss[:, 1:2], in_=ss[:, 1:2], func=AF.Sqrt), reads=[ss], writes=[ss])


def norm_mod(c, xt, h, ss, A, sh, modsb):
    rms_rstd(c, xt, h, ss)
    c.op("dve", lambda e: e.scalar_tensor_tensor(out=h[:, :], in0=xt[:, :], scalar=ss[:, 1:2],
                                                 in1=modsb[:, A:A + D], op0=ALU.mult, op1=ALU.mult),
         reads=[xt, ss, modsb], writes=[h])
    c.op("dve", lambda e: e.tensor_tensor(out=h[:, :], in0=h[:, :], in1=modsb[:, sh:sh + D], op=ALU.add),
         reads=[h, modsb], writes=[h])


class Consts:
    pass


def load_consts(c, d):
    k = Consts()
    k.identf = c.sbuf([P, P], F32, dma=True)
    k.identb = c.sbuf([P, P], BF16, dma=True)
    k.triu = c.sbuf([P, P], F32, dma=True)
    k.tril = c.sbuf([P, P], F32, dma=True)
    k.onesf = c.sbuf([P, P], F32)
    k.onesb = c.sbuf([P, P], BF16)
    c.dma("sp", lambda e: e.dma_start(out=k.identf[:, :], in_=d["identf"].ap()), writes=[k.identf])
    c.dma("sp", lambda e: e.dma_start(out=k.identb[:, :], in_=d["identb"].ap()), writes=[k.identb])
    c.dma("sp", lambda e: e.dma_start(out=k.triu[:, :], in_=d["triu"].ap()), writes=[k.triu])
    c.dma("sp", lambda e: e.dma_start(out=k.tril[:, :], in_=d["tril"].ap()), writes=[k.tril])
    c.op("dve", lambda e: e.memset(k.onesf[:, :], 1.0), writes=[k.onesf])
    c.op("dve", lambda e: e.memset(k.onesb[:, :], 1.0), writes=[k.onesb])
    return k


def host_consts():
    ii = np.arange(P)
    return dict(
        identf=np.eye(P, dtype=np.float32),
        identb=np.eye(P, dtype=np.float32).astype(ml_dtypes.bfloat16),
        triu=(ii[:, None] < ii[None, :]).astype(np.float32),
        tril=(ii[:, None] <= ii[None, :]).astype(np.float32),
    )


CONST_SPECS = dict(identf=([P, P], F32), identb=([P, P], BF16), triu=([P, P], F32), tril=([P, P], F32))


def stage_mods(c, k, cr_d, mw_d, mb_d, ng_d, modsb, ncols=6 * D, nnorm=2):
    with c.scope():
        cr = c.sbuf([P, 8], F32, dma=True)
        sg = c.sbuf([P, 8], F32)
        cb = c.sbuf([P, 8, P], F32)
        mbb = c.sbuf([P, ncols], F32, dma=True)
        nb = c.sbuf([P, nnorm, D], F32, dma=True)
        mwt = [c.sbuf([P, 8, 512], F32, dma=True) for _ in range(2)]
        ps = [c.psum([P, 512], F32) for _ in range(2)]
        c.dma("sp", lambda e: e.dma_start(out=cr[:, :], in_=cr_d.ap()), writes=[cr])
        c.dma("sp", lambda e: e.dma_start(out=mbb[:, :], in_=mb_d.ap().partition_broadcast(P)), writes=[mbb])
        for i in range(nnorm):
            c.dma("sp", lambda e: e.dma_start(out=nb[:, i, :], in_=ng_d.ap()[i:i + 1, :].partition_broadcast(P)),
                  writes=[nb])
        c.op("act", lambda e: e.activation(out=sg[:, :], in_=cr[:, :], func=AF.Sigmoid), reads=[cr], writes=[sg])
        c.op("dve", lambda e: e.tensor_tensor(out=sg[:, :], in0=sg[:, :], in1=cr[:, :], op=ALU.mult),
             reads=[sg, cr], writes=[sg])
        for kc in range(8):
            c.op("dve", lambda e: e.tensor_scalar(out=cb[:, kc, :], in0=k.onesf[:, :], scalar1=sg[:, kc:kc + 1],
                                                  scalar2=None, op0=ALU.mult), reads=[k.onesf, sg], writes=[cb])
        mwv = mw_d.ap().rearrange("(kc p) n -> p kc n", p=P)
        for j in range(ncols // 512):
            w = mwt[j % 2]
            pj = ps[j % 2]
            c.dma("sp", lambda e: e.dma_start(out=w[:, :, :], in_=mwv[:, :, j * 512:(j + 1) * 512]), writes=[w])
            for kc in range(8):
                c.op("pe", lambda e: e.matmul(pj[:, :], lhsT=cb[:, kc, :], rhs=w[:, kc, :], start=(kc == 0),
                                              stop=(kc == 7)), reads=[cb, w], writes=[pj])
            c.op("dve", lambda e: e.tensor_tensor(out=modsb[:, j * 512:(j + 1) * 512], in0=pj[:, :],
                                                  in1=mbb[:, j * 512:(j + 1) * 512], op=ALU.add),
                 reads=[pj, mbb], writes=[modsb])
        if nnorm == 2:
            slots = [(1, 0), (4, 1)]
        else:
            slots = [(1, 0)]
        for s, i in slots:
            c.op("dve", lambda e: e.scalar_tensor_tensor(out=modsb[:, s * D:(s + 1) * D], in0=modsb[:, s * D:(s + 1) * D],
                                                         scalar=1.0, in1=nb[:, i, :], op0=ALU.add, op1=ALU.mult),
                 reads=[modsb, nb], writes=[modsb])


def stage_conv(c, k, modsb, xin_d, flag_d, w, xmid_d, town=TOWN):
    CW = 31
    with c.scope():
        w1b = c.sbuf([P, 8, 2 * D], BF16, dma=True)
        w2b = c.sbuf([P, 8, D], BF16, dma=True)
        b1 = c.sbuf([P, 16], F32, dma=True)
        wdw = c.sbuf([P, 8, CW], F32, dma=True)
        bdw = c.sbuf([P, 8], F32, dma=True)
        lng = c.sbuf([P, 8], F32, dma=True)
        lnb = c.sbuf([P, 8], F32, dma=True)
        b2b = c.sbuf([1, D], BF16, dma=True)
        flag = c.sbuf([P, 1], F32, dma=True)
        w1v = w["pw1"].ap().rearrange("(kc p) n -> p kc n", p=P)
        w2v = w["pw2"].ap().rearrange("(kc p) n -> p kc n", p=P)
        for kc in range(8):
            c.dma("pool", lambda e: e.dma_start(out=w1b[:, kc, :], in_=w1v[:, kc, :]), writes=[w1b], nowaw=True)
            c.dma("pool", lambda e: e.dma_start(out=w2b[:, kc, :], in_=w2v[:, kc, :]), writes=[w2b], nowaw=True)
        c.dma("pool", lambda e: e.dma_start(out=b2b[:, :], in_=w["b_pw2"].ap()), writes=[b2b])
        for t, src in ((b1, "b_pw1"), (wdw, "w_dw"), (bdw, "b_dw"), (lng, "ln_g"), (lnb, "ln_b")):
            if t is wdw:
                c.dma("sp", lambda e: e.dma_start(out=t[:, :, :], in_=w[src].ap()), writes=[t])
            else:
                c.dma("sp", lambda e: e.dma_start(out=t[:, :], in_=w[src].ap()), writes=[t])
        c.dma("sp", lambda e: e.dma_start(out=flag[:, :], in_=flag_d.ap()), writes=[flag])

        xts = [c.sbuf([P, D], F32, dma=True) for _ in range(2)]
        xrs = [c.sbuf([P, D], F32, dma=True) for _ in range(2)]
        xos = [c.sbuf([P, D], F32) for _ in range(2)]
        h = c.sbuf([P, D], F32)
        ss = c.sbuf([P, 2], F32)
        hT = c.sbuf([P, 8, BLK], BF16)
        uT = c.sbuf([P, 8, 30 + BLK], BF16)
        acc = [c.sbuf([P, BLK], F32) for _ in range(8)]
        vb = c.sbuf([P, 8, BLK], BF16)
        v2 = c.sbuf([P, 8, BLK], BF16)
        sT = c.sbuf([P, 8, BLK], BF16)
        sgt = [c.sbuf([P, BLK], F32) for _ in range(2)]
        mean = c.sbuf([P, BLK], F32)
        msq = c.sbuf([P, BLK], F32)
        rstd = c.sbuf([P, BLK], F32)
        tmp = c.sbuf([P, 512], F32)
        pT = c.psum([P, D], F32)
        psA = [c.psum([P, BLK], F32) for _ in range(2)]
        psG = [c.psum([P, BLK], F32) for _ in range(2)]
        psO = [c.psum([P, 512], F32) for _ in range(2)]
        c.op("dve", lambda e: e.memset(uT[:, :, 0:30], 0.0), writes=[uT])

        blocks = [(0, P, True)] + [(P + i * BLK, BLK, False) for i in range(town // BLK)]
        nx = 0
        for (t0, n, is_halo) in blocks:
            nt = n // P
            for i in range(nt):
                xt = xts[nx % 2]
                nx += 1
                r0 = t0 + i * P
                c.dma("sp", lambda e: e.dma_start(out=xt[:, :], in_=xin_d.ap()[r0:r0 + P, :]), writes=[xt])
                norm_mod(c, xt, h, ss, 1 * D, 0 * D, modsb)
                for kc in range(8):
                    c.op("pe", lambda e: e.transpose(out=pT[:, kc * P:(kc + 1) * P], in_=h[:, kc * P:(kc + 1) * P],
                                                     identity=k.identf[:, :]), reads=[h, k.identf], writes=[pT])
                c.op("act", lambda e: e.activation(out=hT[:, :, i * P:(i + 1) * P],
                                                   in_=pT[:, :].rearrange("p (k t) -> p k t", k=8), func=AF.Copy),
                     reads=[pT], writes=[hT])
            for fc in range(8):
                pa, pg, sg = psA[fc % 2], psG[fc % 2], sgt[fc % 2]
                for kc in range(8):
                    c.op("pe", lambda e: e.matmul(pa[:, 0:n], lhsT=w1b[:, kc, fc * P:(fc + 1) * P], rhs=hT[:, kc, 0:n],
                                                  start=(kc == 0), stop=(kc == 7)), reads=[w1b, hT], writes=[pa])
                for kc in range(8):
                    c.op("pe", lambda e: e.matmul(pg[:, 0:n], lhsT=w1b[:, kc, D + fc * P:D + (fc + 1) * P],
                                                  rhs=hT[:, kc, 0:n], start=(kc == 0), stop=(kc == 7)),
                         reads=[w1b, hT], writes=[pg])
                c.op("act", lambda e: e.activation(out=sg[:, 0:n], in_=pg[:, 0:n], func=AF.Sigmoid,
                                                   bias=b1[:, 8 + fc:9 + fc]), reads=[pg, b1], writes=[sg])
                c.op("dve", lambda e: e.scalar_tensor_tensor(out=uT[:, fc, 30:30 + n], in0=pa[:, 0:n],
                                                             scalar=b1[:, fc:fc + 1], in1=sg[:, 0:n],
                                                             op0=ALU.add, op1=ALU.mult),
                     reads=[pa, b1, sg], writes=[uT])
            if is_halo:
                c.op("dve", lambda e: e.tensor_scalar(out=uT[:, :, 30:30 + n], in0=uT[:, :, 30:30 + n],
                                                      scalar1=flag[:, 0:1], scalar2=None, op0=ALU.mult),
                     reads=[uT, flag], writes=[uT])
            else:
                for cc in range(8):
                    en = "dve"
                    a = acc[cc]
                    c.op(en, lambda e: e.tensor_scalar(out=a[:, 0:n], in0=uT[:, cc, 0:n], scalar1=wdw[:, cc, 0:1],
                                                       scalar2=bdw[:, cc:cc + 1], op0=ALU.mult, op1=ALU.add),
                         reads=[uT, wdw, bdw], writes=[a])
                    for j in range(1, CW):
                        c.op(en, lambda e: e.scalar_tensor_tensor(out=a[:, 0:n], in0=uT[:, cc, j:j + n],
                                                                  scalar=wdw[:, cc, j:j + 1], in1=a[:, 0:n],
                                                                  op0=ALU.mult, op1=ALU.add),
                             reads=[uT, wdw, a], writes=[a], chain=CONV_CHAIN)
                for cc in range(8):
                    a = acc[cc]
                    c.op("act", lambda e: e.activation(out=vb[:, cc, 0:n], in_=a[:, 0:n], func=AF.Copy),
                         reads=[a], writes=[vb])
                    c.op("act", lambda e: e.activation(out=v2[:, cc, 0:n], in_=a[:, 0:n], func=AF.Square),
                         reads=[a], writes=[v2])
                s1, s2 = psA[0], psG[0]
                for cc in range(8):
                    c.op("pe", lambda e: e.matmul(s1[:, 0:n], lhsT=k.onesb[:, :], rhs=vb[:, cc, 0:n], start=(cc == 0),
                                                  stop=(cc == 7)), reads=[k.onesb, vb], writes=[s1])
                for cc in range(8):
                    c.op("pe", lambda e: e.matmul(s2[:, 0:n], lhsT=k.onesb[:, :], rhs=v2[:, cc, 0:n], start=(cc == 0),
                                                  stop=(cc == 7)), reads=[k.onesb, v2], writes=[s2])
                c.op("dve", lambda e: e.tensor_scalar(out=mean[:, 0:n], in0=s1[:, 0:n], scalar1=1.0 / D, scalar2=None,
                                                      op0=ALU.mult), reads=[s1], writes=[mean])
                c.op("dve", lambda e: e.tensor_tensor(out=msq[:, 0:n], in0=mean[:, 0:n], in1=mean[:, 0:n], op=ALU.mult),
                     reads=[mean], writes=[msq])
                c.op("dve", lambda e: e.scalar_tensor_tensor(out=rstd[:, 0:n], in0=s2[:, 0:n], scalar=1.0 / D,
                                                             in1=msq[:, 0:n], op0=ALU.mult, op1=ALU.subtract),
                     reads=[s2, msq], writes=[rstd])
                c.op("dve", lambda e: e.tensor_scalar(out=rstd[:, 0:n], in0=rstd[:, 0:n], scalar1=EPS, scalar2=None,
                                                      op0=ALU.add), reads=[rstd], writes=[rstd])
                c.op("dve", lambda e: e.reciprocal(out=rstd[:, 0:n], in_=rstd[:, 0:n]), reads=[rstd], writes=[rstd])
                c.op("act", lambda e: e.activation(out=rstd[:, 0:n], in_=rstd[:, 0:n], func=AF.Sqrt),
                     reads=[rstd], writes=[rstd])
                for cc in range(8):
                    a = acc[cc]
                    en = "dve" if cc < 5 else "pool"
                    c.op(en, lambda e: e.tensor_tensor(out=a[:, 0:n], in0=a[:, 0:n], in1=mean[:, 0:n], op=ALU.subtract),
                         reads=[a, mean], writes=[a])
                    c.op(en, lambda e: e.tensor_tensor(out=a[:, 0:n], in0=a[:, 0:n], in1=rstd[:, 0:n], op=ALU.mult),
                         reads=[a, rstd], writes=[a])
                    c.op("act", lambda e: e.activation(out=sT[:, cc, 0:n], in_=a[:, 0:n], func=AF.Silu,
                                                       scale=lng[:, cc:cc + 1], bias=lnb[:, cc:cc + 1]),
                         reads=[a, lng, lnb], writes=[sT])
                for i in range(nt):
                    r0 = t0 + i * P
                    xr = xrs[i % 2]
                    xo = xos[i % 2]
                    c.dma("sp", lambda e: e.dma_start(out=xr[:, :], in_=xin_d.ap()[r0:r0 + P, :]), writes=[xr])
                    for hf in range(2):
                        po = psO[hf]
                        for cc in range(8):
                            c.op("pe", lambda e: e.matmul(po[:, :], lhsT=sT[:, cc, i * P:(i + 1) * P],
                                                          rhs=w2b[:, cc, hf * 512:(hf + 1) * 512], start=(cc == 0),
                                                          stop=False), reads=[sT, w2b], writes=[po])
                        c.op("pe", lambda e: e.matmul(po[:, :], lhsT=k.onesb[0:1, :], rhs=b2b[0:1, hf * 512:(hf + 1) * 512],
                                                      start=False, stop=True), reads=[k.onesb, b2b], writes=[po])
                        c.op("dve", lambda e: e.tensor_tensor(out=tmp[:, :], in0=po[:, :],
                                                              in1=modsb[:, 2 * D + hf * 512:2 * D + (hf + 1) * 512],
                                                              op=ALU.mult), reads=[po, modsb], writes=[tmp])
                        c.op("dve", lambda e: e.tensor_tensor(out=xo[:, hf * 512:(hf + 1) * 512], in0=tmp[:, :],
                                                              in1=xr[:, hf * 512:(hf + 1) * 512], op=ALU.add),
                             reads=[tmp, xr], writes=[xo])
                    c.dma("sp", lambda e: e.dma_start(out=xmid_d.ap()[r0 - P:r0, :], in_=xo[:, :]),
                          reads=[xo], writes=[xmid_d], nowaw=True)
            c.op("dve", lambda e: e.tensor_copy(out=uT[:, :, 0:30], in_=uT[:, :, n:n + 30]), reads=[uT], writes=[uT])


def stage_moe(c, k, modsb, xmid_d, w, scr, xout_d, T=TOWN, final_g=None):
    NT = T // P
    NB = (T * 4) // BLK + NE
    KMAX = T // BLK
    hbuf_d, table_d, ysl_d = scr["hbuf"], scr["table"], scr["ysl"]
    with c.scope():
        maskall = c.sbuf([P, NT, NE], F32)
        gall = c.sbuf([P, NT, NE], F32)
        s4i = c.sbuf([P, NT, 4], I32)
        ebi = c.sbuf([P, NB], I32)
        widx = c.sbuf([P, NB, 8], I32)
        bidx = c.sbuf([P, NB], I32)
        with c.scope():
            rw = c.sbuf([P, 8, NE], F32, dma=True)
            rbb = c.sbuf([P, NE], F32, dma=True)
            c.dma("sp", lambda e: e.dma_start(out=rw[:, :, :], in_=w["router_w"].ap().rearrange("(kc p) n -> p kc n", p=P)),
                  writes=[rw])
            c.dma("sp", lambda e: e.dma_start(out=rbb[:, :], in_=w["router_b"].ap().partition_broadcast(P)), writes=[rbb])
            xts = [c.sbuf([P, D], F32, dma=True) for _ in range(2)]
            hq = c.sbuf([P, D], F32)
            hbs = [c.sbuf([P, D], BF16) for _ in range(2)]
            hT32 = c.sbuf([P, 8, P], F32)
            ss = c.sbuf([P, 2], F32)
            lg = c.sbuf([P, NE], F32)
            ex = c.sbuf([P, NE], F32)
            m8 = c.sbuf([P, 8], F32)
            sm = c.sbuf([P, 4], F32)
            pT = c.psum([P, D], F32)
            pl = c.psum([P, NE], F32)
            for i in range(NT):
                xt = xts[i % 2]
                hb = hbs[i % 2]
                c.dma("sp", lambda e: e.dma_start(out=xt[:, :], in_=xmid_d.ap()[i * P:(i + 1) * P, :]),
                      reads=[xmid_d], writes=[xt])
                norm_mod(c, xt, hq, ss, 4 * D, 3 * D, modsb)
                c.op("pool", lambda e: e.tensor_copy(out=hb[:, :], in_=hq[:, :]), reads=[hq], writes=[hb])
                c.dma("sp", lambda e: e.dma_start(out=hbuf_d.ap()[i * P:(i + 1) * P, :], in_=hb[:, :]),
                      reads=[hb], writes=[hbuf_d], nowaw=True)
                for kc in range(8):
                    c.op("pe", lambda e: e.transpose(out=pT[:, kc * P:(kc + 1) * P], in_=hq[:, kc * P:(kc + 1) * P],
                                                     identity=k.identf[:, :]), reads=[hq, k.identf], writes=[pT])
                c.op("act", lambda e: e.activation(out=hT32[:, :, :], in_=pT[:, :].rearrange("p (k t) -> p k t", k=8),
                                                   func=AF.Copy), reads=[pT], writes=[hT32])
                for kc in range(8):
                    c.op("pe", lambda e: e.matmul(pl[:, :], lhsT=hT32[:, kc, :], rhs=rw[:, kc, :], start=(kc == 0),
                                                  stop=(kc == 7)), reads=[hT32, rw], writes=[pl])
                c.op("dve", lambda e: e.tensor_tensor(out=lg[:, :], in0=pl[:, :], in1=rbb[:, :], op=ALU.add),
                     reads=[pl, rbb], writes=[lg])
                c.op("dve", lambda e: e.max(out=m8[:, :], in_=lg[:, :]), reads=[lg], writes=[m8])
                c.op("dve", lambda e: e.tensor_scalar(out=maskall[:, i, :], in0=lg[:, :], scalar1=m8[:, 3:4], scalar2=None,
                                                      op0=ALU.is_ge), reads=[lg, m8], writes=[maskall])
                c.op("dve", lambda e: e.tensor_scalar(out=sm[:, 0:1], in0=m8[:, 0:1], scalar1=-1.0, scalar2=None,
                                                      op0=ALU.mult), reads=[m8], writes=[sm])
                c.op("act", lambda e: e.activation(out=ex[:, :], in_=lg[:, :], func=AF.Exp, bias=sm[:, 0:1]),
                     reads=[lg, sm], writes=[ex])
                c.op("dve", lambda e: e.scalar_tensor_tensor(out=ex[:, :], in0=ex[:, :], scalar=1.0, in1=maskall[:, i, :],
                                                             op0=ALU.mult, op1=ALU.mult, accum_out=sm[:, 1:2]),
                     reads=[ex, maskall], writes=[ex, sm])
                c.op("dve", lambda e: e.reciprocal(out=sm[:, 2:3], in_=sm[:, 1:2]), reads=[sm], writes=[sm])
                c.op("dve", lambda e: e.tensor_scalar(out=gall[:, i, :], in0=ex[:, :], scalar1=sm[:, 2:3], scalar2=None,
                                                      op0=ALU.mult), reads=[ex, sm], writes=[gall])
        with c.scope():
            W = NT * NE
            pos = c.sbuf([P, NT, NE], F32)
            cs = c.sbuf([P, NT, NE], F32)
            base = c.sbuf([P, NT + 1, NE], F32)
            key = c.sbuf([P, NT, NE], F32)
            top = c.sbuf([P, NT, 8], F32)
            g4 = c.sbuf([P, NT, 4], F32)
            src = c.sbuf([P, NT, 4, 2], I32)
            tok = c.sbuf([P, NT], I32)
            cnt = c.sbuf([P, NE], F32)
            nbk = c.sbuf([P, NE], F32)
            tmpe = c.sbuf([P, NE], F32)
            pend = c.sbuf([P, NE], F32)
            pstart = c.sbuf([P, NE], F32)
            thr_i = c.sbuf([P, NB], I32)
            thr = c.sbuf([P, NB], F32)
            eb = c.sbuf([P, NB], F32)
            same = c.sbuf([P, NB], F32)
            ebs = c.sbuf([P, NB], F32)
            pk_i = c.sbuf([P, 8], I32)
            pk = c.sbuf([P, 8], F32)
            wf = c.sbuf([P, NB, 8], F32)
            s4f = c.sbuf([P, NT, 4], F32)
            zt = c.sbuf([P, NB * BLK * 2 // P], I32)
            pp = [c.psum([P, 512], F32) for _ in range(2)]
            mflat = maskall[:, :, :].rearrange("p t e -> p (t e)")
            posf = pos[:, :, :].rearrange("p t e -> p (t e)")
            csf = cs[:, :, :].rearrange("p t e -> p (t e)")
            c.op("pool", lambda e: e.memset(zt[:, :], 0), writes=[zt])
            c.dma("sp", lambda e: e.dma_start(out=table_d.ap().rearrange("(p r) w -> p (r w)", p=P), in_=zt[:, :]),
                  reads=[zt], writes=[table_d])
            for j0 in range(0, W, 512):
                n = min(512, W - j0)
                c.op("pe", lambda e: e.matmul(pp[0][:, 0:n], lhsT=k.triu[:, :], rhs=mflat[:, j0:j0 + n], start=True, stop=True),
                     reads=[k.triu, maskall], writes=[pp[0]])
                c.op("pe", lambda e: e.matmul(pp[1][:, 0:n], lhsT=k.onesf[:, :], rhs=mflat[:, j0:j0 + n], start=True, stop=True),
                     reads=[k.onesf, maskall], writes=[pp[1]])
                c.op("dve", lambda e: e.tensor_copy(out=posf[:, j0:j0 + n], in_=pp[0][:, 0:n]), reads=[pp[0]], writes=[pos])
                c.op("act", lambda e: e.activation(out=csf[:, j0:j0 + n], in_=pp[1][:, 0:n], func=AF.Copy),
                     reads=[pp[1]], writes=[cs])
            c.op("dve", lambda e: e.memset(base[:, 0, :], 0.0), writes=[base])
            for i in range(NT):
                c.op("dve", lambda e: e.tensor_tensor(out=base[:, i + 1, :], in0=base[:, i, :], in1=cs[:, i, :], op=ALU.add),
                     reads=[base, cs], writes=[base])
            c.op("dve", lambda e: e.tensor_copy(out=cnt[:, :], in_=base[:, NT, :]), reads=[base], writes=[cnt])
            c.op("dve", lambda e: e.tensor_scalar(out=nbk[:, :], in0=cnt[:, :], scalar1=0.0, scalar2=None, op0=ALU.is_gt),
                 reads=[cnt], writes=[nbk])
            for kk in range(1, KMAX + 1):
                c.op("dve", lambda e: e.tensor_scalar(out=tmpe[:, :], in0=cnt[:, :], scalar1=float(BLK * kk), scalar2=None,
                                                      op0=ALU.is_gt), reads=[cnt], writes=[tmpe])
                c.op("dve", lambda e: e.tensor_tensor(out=nbk[:, :], in0=nbk[:, :], in1=tmpe[:, :], op=ALU.add),
                     reads=[nbk, tmpe], writes=[nbk])
            c.op("dve", lambda e: e.tensor_scalar(out=nbk[:, :], in0=nbk[:, :], scalar1=float(BLK), scalar2=None, op0=ALU.mult),
                 reads=[nbk], writes=[nbk])
            c.op("dve", lambda e: e.tensor_copy(out=pend[:, 0:1], in_=nbk[:, 0:1]), reads=[nbk], writes=[pend])
            for e_ in range(1, NE):
                c.op("dve", lambda e: e.tensor_tensor(out=pend[:, e_:e_ + 1], in0=pend[:, e_ - 1:e_], in1=nbk[:, e_:e_ + 1],
                                                      op=ALU.add), reads=[pend, nbk], writes=[pend])
            c.op("dve", lambda e: e.tensor_tensor(out=pstart[:, :], in0=pend[:, :], in1=nbk[:, :], op=ALU.subtract),
                 reads=[pend, nbk], writes=[pstart])
            c.op("dve", lambda e: e.tensor_tensor(out=pos[:, :, :], in0=pos[:, :, :], in1=base[:, 0:NT, :], op=ALU.add),
                 reads=[pos, base], writes=[pos])
            for i in range(NT):
                c.op("dve", lambda e: e.tensor_tensor(out=pos[:, i, :], in0=pos[:, i, :], in1=pstart[:, :], op=ALU.add),
                     reads=[pos, pstart], writes=[pos])
            c.op("dve", lambda e: e.scalar_tensor_tensor(out=key[:, :, :], in0=pos[:, :, :], scalar=1.0, in1=maskall[:, :, :],
                                                         op0=ALU.add, op1=ALU.mult), reads=[pos, maskall], writes=[key])
            for i in range(NT):
                c.op("dve", lambda e: e.max(out=top[:, i, :], in_=key[:, i, :]), reads=[key], writes=[top])
            c.op("dve", lambda e: e.tensor_scalar(out=s4f[:, :, :], in0=top[:, :, 0:4], scalar1=-1.0, scalar2=None, op0=ALU.add),
                 reads=[top], writes=[s4f])
            c.op("dve", lambda e: e.tensor_copy(out=s4i[:, :, :], in_=s4f[:, :, :]), reads=[s4f], writes=[s4i])
            for i in range(NT):
                for kk in range(4):
                    c.op("dve", lambda e: e.scalar_tensor_tensor(out=tmpe[:, :], in0=key[:, i, :], scalar=top[:, i, kk:kk + 1],
                                                                 in1=gall[:, i, :], op0=ALU.is_equal, op1=ALU.mult,
                                                                 accum_out=g4[:, i, kk:kk + 1]),
                         reads=[key, top, gall], writes=[tmpe, g4])
            c.op("pool", lambda e: e.iota(tok[:, :], pattern=[[P, NT]], base=0, channel_multiplier=1), writes=[tok])
            for kk in range(4):
                c.op("dve", lambda e: e.tensor_copy(out=src[:, :, kk, 0], in_=tok[:, :]), reads=[tok], writes=[src])
            c.op("dve", lambda e: e.tensor_copy(out=src[:, :, :, 1].bitcast(F32), in_=g4[:, :, :]), reads=[g4], writes=[src])
            for i in range(NT):
                for kk in range(4):
                    c.dma("pool", lambda e: e.indirect_dma_start(out=table_d.ap(), out_offset=IOA(ap=s4i[:, i, kk:kk + 1], axis=0),
                                                                 in_=src[:, i, kk, :], in_offset=None),
                          reads=[s4i, src], writes=[table_d], nowaw=(i + kk > 0))
            c.op("pool", lambda e: e.iota(thr_i[:, :], pattern=[[BLK, NB]], base=0, channel_multiplier=0), writes=[thr_i])
            c.op("dve", lambda e: e.tensor_copy(out=thr[:, :], in_=thr_i[:, :]), reads=[thr_i], writes=[thr])
            for b in range(NB):
                c.op("dve", lambda e: e.tensor_scalar(out=tmpe[:, :], in0=pend[:, :], scalar1=thr[:, b:b + 1], scalar2=0.0,
                                                      op0=ALU.is_le, op1=ALU.add, accum_out=eb[:, b:b + 1]),
                     reads=[pend, thr], writes=[tmpe, eb])
            c.op("dve", lambda e: e.tensor_scalar(out=eb[:, :], in0=eb[:, :], scalar1=float(NE - 1), scalar2=None, op0=ALU.min),
                 reads=[eb], writes=[eb])
            c.op("dve", lambda e: e.memset(same[:, :], 0.0), writes=[same])
            c.op("dve", lambda e: e.tensor_tensor(out=same[:, 1:NB], in0=eb[:, 1:NB], in1=eb[:, 0:NB - 1], op=ALU.is_equal),
                 reads=[eb], writes=[same])
            c.op("dve", lambda e: e.memset(same[:, NB // 2:NB // 2 + 1], 0.0), writes=[same])
            c.op("dve", lambda e: e.scalar_tensor_tensor(out=ebs[:, :], in0=same[:, :], scalar=1.0e9, in1=eb[:, :], op0=ALU.mult, op1=ALU.add),
                 reads=[same, eb], writes=[ebs])
            c.op("dve", lambda e: e.tensor_copy(out=ebi[:, :], in_=ebs[:, :]), reads=[ebs], writes=[ebi])
            c.op("pool", lambda e: e.iota(pk_i[:, :], pattern=[[P, 8]], base=0, channel_multiplier=1), writes=[pk_i])
            c.op("dve", lambda e: e.tensor_copy(out=pk[:, :], in_=pk_i[:, :]), reads=[pk_i], writes=[pk])
            for kc in range(8):
                c.op("dve", lambda e: e.tensor_scalar(out=wf[:, :, kc], in0=eb[:, :], scalar1=float(D), scalar2=pk[:, kc:kc + 1],
                                                      op0=ALU.mult, op1=ALU.add), reads=[eb, pk], writes=[wf])
            for kc in range(8):
                c.op("dve", lambda e: e.scalar_tensor_tensor(out=wf[:, :, kc], in0=same[:, :], scalar=1.0e9, in1=wf[:, :, kc],
                                                             op0=ALU.mult, op1=ALU.add), reads=[same, wf], writes=[wf])
            c.op("dve", lambda e: e.tensor_copy(out=widx[:, :, :], in_=wf[:, :, :]), reads=[wf], writes=[widx])
            c.op("dve", lambda e: e.tensor_scalar(out=eb[:, :], in0=eb[:, :], scalar1=float(P), scalar2=pk[:, 0:1],
                                                  op0=ALU.mult, op1=ALU.add), reads=[eb, pk], writes=[eb])
            c.op("dve", lambda e: e.scalar_tensor_tensor(out=eb[:, :], in0=same[:, :], scalar=1.0e9, in1=eb[:, :], op0=ALU.mult, op1=ALU.add),
                 reads=[same, eb], writes=[eb])
            c.op("dve", lambda e: e.tensor_copy(out=bidx[:, :], in_=eb[:, :]), reads=[eb], writes=[bidx])
        with c.scope():
            wg = [c.sbuf([P, 8, 2 * D], BF16, dma=True) for _ in range(2)]
            wd = [c.sbuf([P, 8, D], BF16, dma=True) for _ in range(2)]
            bg = [c.sbuf([P, 16], F32, dma=True) for _ in range(2)]
            bd = [c.sbuf([2, D], BF16, dma=True) for _ in range(2)]
            tk = [c.sbuf([P, 8], I32, dma=True) for _ in range(2)]
            xg = [c.sbuf([P, 4, D], BF16, dma=True) for _ in range(2)]
            xgT = c.sbuf([P, 8, BLK], BF16)
            actT = c.sbuf([P, 8, BLK], BF16)
            yo = [c.sbuf([P, 4, D], BF16) for _ in range(2)]
            t1 = [c.sbuf([P, BLK], F32) for _ in range(2)]
            sg = [c.sbuf([P, BLK], F32) for _ in range(2)]
            xl = [c.sbuf([P, BLK], F32) for _ in range(2)]
            bgp = [c.sbuf([P, 8], F32) for _ in range(2)]
            ptr = [c.psum([P, 2 * BLK], BF16) for _ in range(2)]
            psg = [c.psum([P, BLK], F32) for _ in range(2)]
            psl = [c.psum([P, BLK], F32) for _ in range(2)]
            psd = [c.psum([P, 512], F32) for _ in range(2)]
            wguv = w["w_gu"].ap().rearrange("e k n -> (e k) n")
            wdnv = w["w_down"].ap().rearrange("e k n -> (e k) n")
            bguv = w["b_gu_r"].ap().rearrange("e p n -> (e p) n")

            rg_w = c.nc.gpsimd.to_reg(NE * D - 1)
            rg_b = c.nc.gpsimd.to_reg(NE * P - 1)
            rg_e = c.nc.gpsimd.to_reg(NE - 1)

            def blk(kpos):
                return (kpos // 2) + (NB // 2) * (kpos % 2)

            def prefetch(kpos):
                s = kpos % 2
                b = blk(kpos)
                c.dma("sp", lambda e: e.dma_start(out=tk[s][:, :],
                                                  in_=table_d.ap()[b * BLK:(b + 1) * BLK, :].rearrange("(p j) w -> p (j w)", j=4)),
                      reads=[table_d], writes=[tk[s]])
                for j in range(4):
                    c.dma("pool", lambda e: e.indirect_dma_start(out=xg[s][:, j, :], out_offset=None, in_=hbuf_d.ap(),
                                                                 in_offset=IOA(ap=tk[s][:, 2 * j:2 * j + 1], axis=0)),
                          reads=[tk[s], hbuf_d], writes=[xg[s]], nowaw=(j > 0))
                for kc in range(8):
                    c.dma("pool", lambda e: e.indirect_dma_start(out=wg[s][:, kc, :], out_offset=None, in_=wguv,
                                                                 in_offset=IOA(ap=widx[:, b, kc:kc + 1], axis=0),
                                                                 bounds_check=rg_w, oob_is_err=False),
                          reads=[widx], writes=[wg[s]], nowaw=(kc > 0))
                for kc in range(8):
                    c.dma("pool", lambda e: e.indirect_dma_start(out=wd[s][:, kc, :], out_offset=None, in_=wdnv,
                                                                 in_offset=IOA(ap=widx[:, b, kc:kc + 1], axis=0),
                                                                 bounds_check=rg_w, oob_is_err=False),
                          reads=[widx], writes=[wd[s]], nowaw=(kc > 0))
                c.dma("pool", lambda e: e.indirect_dma_start(out=bg[s][:, :], out_offset=None, in_=bguv,
                                                             in_offset=IOA(ap=bidx[:, b:b + 1], axis=0),
                                                             bounds_check=rg_b, oob_is_err=False),
                      reads=[bidx], writes=[bg[s]])
                c.dma("pool", lambda e: e.indirect_dma_start(out=bd[s][0:2, :], out_offset=None, in_=w["b_down"].ap(),
                                                             in_offset=IOA(ap=ebi[0:2, b:b + 1], axis=0),
                                                             bounds_check=rg_e, oob_is_err=False),
                      reads=[ebi], writes=[bd[s]])

            prefetch(0)
            for kpos in range(NB):
                s = kpos % 2
                b = blk(kpos)
                if kpos + 1 < NB:
                    prefetch(kpos + 1)
                for kc in range(8):
                    pt = ptr[(kc // 2) % 2]
                    o = (kc % 2) * BLK
                    for j in range(4):
                        c.op("pe", lambda e: e.transpose(out=pt[:, o + j * P:o + (j + 1) * P], in_=xg[s][:, j, kc * P:(kc + 1) * P],
                                                         identity=k.identb[:, :]), reads=[xg[s], k.identb], writes=[pt])
                    if kc % 2 == 1:
                        c.op("act", lambda e: e.activation(out=xgT[:, kc - 1:kc + 1, :],
                                                           in_=pt[:, :].rearrange("p (k t) -> p k t", k=2), func=AF.Copy),
                             reads=[pt], writes=[xgT])
                c.op("dve", lambda e: e.tensor_scalar(out=bgp[s][:, :], in0=bg[s][:, 8:16], scalar1=1.0, scalar2=None, op0=ALU.add),
                     reads=[bg[s]], writes=[bgp[s]])
                for fc in range(8):
                    q = fc % 2
                    for kc in range(8):
                        c.op("pe", lambda e: e.matmul(psg[q][:, :], lhsT=wg[s][:, kc, fc * P:(fc + 1) * P], rhs=xgT[:, kc, :],
                                                      start=(kc == 0), stop=(kc == 7)), reads=[wg[s], xgT], writes=[psg[q]])
                    for kc in range(8):
                        c.op("pe", lambda e: e.matmul(psl[q][:, :], lhsT=wg[s][:, kc, D + fc * P:D + (fc + 1) * P], rhs=xgT[:, kc, :],
                                                      start=(kc == 0), stop=(kc == 7)), reads=[wg[s], xgT], writes=[psl[q]])
                    c.op("dve", lambda e: e.tensor_scalar(out=t1[q][:, :], in0=psg[q][:, :], scalar1=bg[s][:, fc:fc + 1], scalar2=7.0,
                                                          op0=ALU.add, op1=ALU.min), reads=[psg[q], bg[s]], writes=[t1[q]])
                    c.op("act", lambda e: e.activation(out=sg[q][:, :], in_=t1[q][:, :], func=AF.Sigmoid, scale=1.702),
                         reads=[t1[q]], writes=[sg[q]])
                    c.op("dve", lambda e: e.tensor_scalar(out=xl[q][:, :], in0=psl[q][:, :], scalar1=bgp[s][:, fc:fc + 1], scalar2=-6.0,
                                                          op0=ALU.add, op1=ALU.max), reads=[psl[q], bgp[s]], writes=[xl[q]])
                    c.op("dve", lambda e: e.tensor_tensor(out=t1[q][:, :], in0=t1[q][:, :], in1=sg[q][:, :], op=ALU.mult),
                         reads=[t1[q], sg[q]], writes=[t1[q]])
                    c.op("dve", lambda e: e.scalar_tensor_tensor(out=actT[:, fc, :], in0=xl[q][:, :], scalar=8.0, in1=t1[q][:, :],
                                                                  op0=ALU.min, op1=ALU.mult), reads=[xl[q], t1[q]], writes=[actT])
                for j in range(4):
                    for hf in range(2):
                        pd = psd[hf]
                        for fc in range(8):
                            c.op("pe", lambda e: e.matmul(pd[:, :], lhsT=actT[:, fc, j * P:(j + 1) * P],
                                                          rhs=wd[s][:, fc, hf * 512:(hf + 1) * 512], start=(fc == 0), stop=False),
                                 reads=[actT, wd[s]], writes=[pd])
                        c.op("pe", lambda e: e.matmul(pd[:, :], lhsT=k.onesb[0:1, :], rhs=bd[s][0:1, hf * 512:(hf + 1) * 512],
                                                      start=False, stop=True), reads=[k.onesb, bd[s]], writes=[pd])
                        c.op("act", lambda e: e.activation(out=yo[s][:, j, hf * 512:(hf + 1) * 512], in_=pd[:, :], func=AF.Copy,
                                                           scale=tk[s][:, 2 * j + 1:2 * j + 2].bitcast(F32)),
                             reads=[pd, tk[s]], writes=[yo[s]])
                c.dma("sp", lambda e: e.dma_start(out=ysl_d.ap()[b * BLK:(b + 1) * BLK, :].rearrange("(p j) d -> p j d", j=4),
                                                  in_=yo[s][:, :, :]), reads=[yo[s]], writes=[ysl_d], nowaw=True)
        with c.scope():
            yg = [c.sbuf([P, 4, D], BF16, dma=True) for _ in range(2)]
            xts = [c.sbuf([P, D], F32, dma=True) for _ in range(2)]
            a0 = c.sbuf([P, D], F32)
            a1 = c.sbuf([P, D], F32)
            xo = [c.sbuf([P, D], F32) for _ in range(2)]
            ss = c.sbuf([P, 2], F32)
            if final_g is not None:
                fg = c.sbuf([P, D], F32, dma=True)
                c.dma("sp", lambda e: e.dma_start(out=fg[:, :], in_=final_g.ap().partition_broadcast(P)), writes=[fg])
            for i in range(NT):
                s = i % 2
                for kk in range(4):
                    c.dma("pool", lambda e: e.indirect_dma_start(out=yg[s][:, kk, :], out_offset=None, in_=ysl_d.ap(),
                                                                 in_offset=IOA(ap=s4i[:, i, kk:kk + 1], axis=0)),
                          reads=[s4i, ysl_d], writes=[yg[s]], nowaw=(kk > 0))
                c.dma("sp", lambda e: e.dma_start(out=xts[s][:, :], in_=xmid_d.ap()[i * P:(i + 1) * P, :]),
                      reads=[xmid_d], writes=[xts[s]])
                c.op("dve", lambda e: e.tensor_tensor(out=a0[:, :], in0=yg[s][:, 0, :], in1=yg[s][:, 1, :], op=ALU.add),
                     reads=[yg[s]], writes=[a0])
                c.op("pool", lambda e: e.tensor_tensor(out=a1[:, :], in0=yg[s][:, 2, :], in1=yg[s][:, 3, :], op=ALU.add),
                     reads=[yg[s]], writes=[a1])
                c.op("dve", lambda e: e.tensor_tensor(out=a0[:, :], in0=a0[:, :], in1=a1[:, :], op=ALU.add),
                     reads=[a0, a1], writes=[a0])
                c.op("dve", lambda e: e.tensor_tensor(out=a0[:, :], in0=a0[:, :], in1=modsb[:, 5 * D:6 * D], op=ALU.mult),
                     reads=[a0, modsb], writes=[a0])
                c.op("dve", lambda e: e.tensor_tensor(out=xo[s][:, :], in0=a0[:, :], in1=xts[s][:, :], op=ALU.add),
                     reads=[a0, xts[s]], writes=[xo[s]])
                if final_g is not None:
                    rms_rstd(c, xo[s], a1, ss)
                    c.op("dve", lambda e: e.scalar_tensor_tensor(out=xo[s][:, :], in0=xo[s][:, :], scalar=ss[:, 1:2], in1=fg[:, :],
                                                                 op0=ALU.mult, op1=ALU.mult), reads=[xo[s], ss, fg], writes=[xo[s]])
                c.dma("sp", lambda e: e.dma_start(out=xout_d.ap()[i * P:(i + 1) * P, :], in_=xo[s][:, :]),
                      reads=[xo[s]], writes=[xout_d], nowaw=True)


def moe_scratch(c, T, tag=""):
    NB = (T * 4) // BLK + NE
    return dict(hbuf=c.dram("hbuf" + tag, [T, D], BF16), table=c.dram("table" + tag, [NB * BLK, 2], I32),
                ysl=c.dram("ysl" + tag, [NB * BLK, D], BF16))


MOE_W = dict(router_w=([D, NE], F32), router_b=([1, NE], F32), w_gu=([NE, D, 2 * D], F32), b_gu_r=([NE, P, 16], F32),
             w_down=([NE, D, D], F32), b_down=([NE, D], F32))
CONV_W = dict(pw1=([D, 2 * D], F32), b_pw1=([P, 16], F32), w_dw=([P, 8, 31], F32), b_dw=([P, 8], F32), ln_g=([P, 8], F32),
              ln_b=([P, 8], F32), pw2=([D, D], F32), b_pw2=([1, D], F32))
MOD_W = dict(c_r=([P, 8], F32), mod_w=([D, 6 * D], F32), mod_b=([1, 6 * D], F32), norm_g=([2, D], F32))


def declare(c, specs, kind="ExternalInput", prefix=""):
    return {n: c.dram(prefix + n, shp, dt, kind=kind) for n, (shp, dt) in specs.items()}


def build_conv_layer():
    nc = bass.Bass("TRN2", target_bir_lowering=False)
    c = Ctx(nc)
    cd = declare(c, CONST_SPECS)
    mw = declare(c, MOD_W)
    cw = declare(c, CONV_W)
    ew = declare(c, MOE_W)
    xin = c.dram("xin", [P + TOWN, D], F32, kind="ExternalInput")
    flag = c.dram("flag", [P, 1], F32, kind="ExternalInput")
    xout = c.dram("xout", [TOWN, D], F32, kind="ExternalOutput")
    xmid = c.dram("xmid", [TOWN, D], F32)
    scr = moe_scratch(c, TOWN)
    k = load_consts(c, cd)
    modsb = c.sbuf([P, 6 * D], F32)
    stage_mods(c, k, mw["c_r"], mw["mod_w"], mw["mod_b"], mw["norm_g"], modsb)
    stage_conv(c, k, modsb, xin, flag, cw, xmid)
    stage_moe(c, k, modsb, xmid, ew, scr, xout)
    c.finish([xout])
    c.close()
    return nc


def pcol(v, n):
    return np.ascontiguousarray(np.asarray(v).reshape(n, P).T)


def mod_inputs(inp, l, b):
    return dict(c_r=pcol(inp["c"][b], 8), mod_w=inp["mod_w"][l], mod_b=inp["mod_b"][l][None, :],
                norm_g=np.stack([inp["norm1_g"][l], inp["norm2_g"][l]]))


def moe_inputs(inp, l):
    bgu = inp["moe_b_gu"][l]
    return dict(router_w=inp["moe_router_w"][l], router_b=inp["moe_router_b"][l][None, :], w_gu=inp["moe_w_gu"][l],
                b_gu_r=np.ascontiguousarray(bgu.reshape(NE, 16, P).transpose(0, 2, 1)), w_down=inp["moe_w_down"][l],
                b_down=inp["moe_b_down"][l])


def conv_inputs(inp, l):
    return dict(pw1=inp["conv_w_pw1"][l], b_pw1=pcol(inp["conv_b_pw1"][l], 16),
                w_dw=np.ascontiguousarray(inp["conv_w_dw"][l].reshape(31, 8, P).transpose(2, 1, 0)),
                b_dw=pcol(inp["conv_b_dw"][l], 8), ln_g=pcol(inp["conv_ln_g"][l], 8), ln_b=pcol(inp["conv_ln_b"][l], 8),
                pw2=inp["conv_w_pw2"][l], b_pw2=inp["conv_b_pw2"][l][None, :])


def head_rms(c, src, dst, sq, ssq, gbc, scale):
    for hh in range(16):
        c.op("dve", lambda e: e.scalar_tensor_tensor(out=sq[:, hh * 64:(hh + 1) * 64], in0=src[:, hh * 64:(hh + 1) * 64], scalar=1.0,
                                                     in1=src[:, hh * 64:(hh + 1) * 64], op0=ALU.mult, op1=ALU.mult,
                                                     accum_out=ssq[:, hh:hh + 1]), reads=[src], writes=[sq, ssq])
    c.op("dve", lambda e: e.tensor_scalar(out=ssq[:, 0:16], in0=ssq[:, 0:16], scalar1=1.0 / 64, scalar2=EPS, op0=ALU.mult,
                                          op1=ALU.add), reads=[ssq], writes=[ssq])
    c.op("dve", lambda e: e.reciprocal(out=ssq[:, 0:16], in_=ssq[:, 0:16]), reads=[ssq], writes=[ssq])
    c.op("act", lambda e: e.activation(out=ssq[:, 0:16], in_=ssq[:, 0:16], func=AF.Sqrt, scale=scale * scale),
         reads=[ssq], writes=[ssq])
    for hh in range(16):
        c.op("dve", lambda e: e.scalar_tensor_tensor(out=dst[:, hh * 64:(hh + 1) * 64], in0=src[:, hh * 64:(hh + 1) * 64],
                                                     scalar=ssq[:, hh:hh + 1], in1=gbc[:, 0:64], op0=ALU.mult, op1=ALU.mult),
             reads=[src, ssq, gbc], writes=[dst])


def pair_transpose_store(c, k, srcb, ptb, stg, dst_d, tok0, rows=64):
    for kc in range(8):
        c.op("pe", lambda e: e.transpose(out=ptb[:, kc * P:(kc + 1) * P], in_=srcb[:, kc * P:(kc + 1) * P], identity=k.identb[:, :]),
             reads=[srcb, k.identb], writes=[ptb])
    c.op("act", lambda e: e.activation(out=stg[:, :, :], in_=ptb[:, :].rearrange("p (k t) -> p k t", k=8), func=AF.Copy),
         reads=[ptb], writes=[stg])
    for kc in range(8):
        for half in range(2):
            c.dma("sp", lambda e: e.dma_start(out=dst_d.ap()[2 * kc + half, 0:64, tok0:tok0 + P], in_=stg[half * 64:(half + 1) * 64, kc, :]),
                  reads=[stg], writes=[dst_d], nowaw=True)


KV_W = dict(c_r=([P, 8], F32), mod_w=([D, 2 * D], F32), mod_b=([1, 2 * D], F32), norm_g=([1, D], F32),
            w_kvf=([D, 2 * D + 16], F32), b_f=([1, 16], F32), k_g=([1, 64], F32))


def build_kv():
    nc = bass.Bass("TRN2", target_bir_lowering=False)
    c = Ctx(nc)
    cd = declare(c, CONST_SPECS)
    w = declare(c, KV_W)
    xin = c.dram("xin", [TOWN, D], F32, kind="ExternalInput")
    kT_d = c.dram("kT", [16, 64, TOWN], BF16, kind="ExternalOutput")
    v_d = c.dram("v", [TOWN, 16, 72], BF16, kind="ExternalOutput")
    lf_d = c.dram("logf", [TOWN, 16], F32, kind="ExternalOutput")
    k = load_consts(c, cd)
    modsb = c.sbuf([P, 2 * D], F32)
    stage_mods(c, k, w["c_r"], w["mod_w"], w["mod_b"], w["norm_g"], modsb, ncols=2 * D, nnorm=1)
    with c.scope():
        wkv = c.sbuf([P, 8, 2 * D], BF16, dma=True)
        wf = c.sbuf([P, 8, 16], F32, dma=True)
        bfb = c.sbuf([P, 16], F32, dma=True)
        kgb = c.sbuf([P, 64], F32, dma=True)
        wv = w["w_kvf"].ap().rearrange("(kc p) n -> p kc n", p=P)
        for kc in range(8):
            c.dma("pool", lambda e: e.dma_start(out=wkv[:, kc, :], in_=wv[:, kc, 0:2 * D]), writes=[wkv], nowaw=True)
        c.dma("sp", lambda e: e.dma_start(out=wf[:, :, :], in_=wv[:, :, 2 * D:2 * D + 16]), writes=[wf])
        c.dma("sp", lambda e: e.dma_start(out=bfb[:, :], in_=w["b_f"].ap().partition_broadcast(P)), writes=[bfb])
        c.dma("sp", lambda e: e.dma_start(out=kgb[:, :], in_=w["k_g"].ap().partition_broadcast(P)), writes=[kgb])
        xts = [c.sbuf([P, D], F32, dma=True) for _ in range(2)]
        h = c.sbuf([P, D], F32)
        ss = c.sbuf([P, 2], F32)
        hT32 = c.sbuf([P, 8, P], F32)
        hTb = c.sbuf([P, 8, P], BF16)
        ksb = c.sbuf([P, D], F32)
        sq = c.sbuf([P, D], F32)
        ssq = c.sbuf([P, 16], F32)
        kn = c.sbuf([P, D], BF16)
        stg = [c.sbuf([P, 8, P], BF16) for _ in range(2)]
        vt = [c.sbuf([P, 16, 72], BF16) for _ in range(2)]
        fz = [c.sbuf([P, 16], F32) for _ in range(2)]
        pT = c.psum([P, D], F32)
        ptb = c.psum([P, D], BF16)
        pk = [c.psum([P, 512], F32) for _ in range(2)]
        pf = c.psum([P, 16], F32)
        for t in vt:
            c.op("dve", lambda e: e.memset(t[:, :, :], 1.0), writes=[t])
        LV = int(os.environ.get("KVSTOP", "9"))
        for i in range(int(os.environ.get("KVTILES", TOWN // P)) if LV > 2 else 0):
            xt = xts[i % 2]
            c.dma("sp", lambda e: e.dma_start(out=xt[:, :], in_=xin.ap()[i * P:(i + 1) * P, :]), writes=[xt])
            norm_mod(c, xt, h, ss, 1 * D, 0, modsb)
            if LV == 3:
                continue
            for kc in range(8):
                c.op("pe", lambda e: e.transpose(out=pT[:, kc * P:(kc + 1) * P], in_=h[:, kc * P:(kc + 1) * P], identity=k.identf[:, :]),
                     reads=[h, k.identf], writes=[pT])
            c.op("act", lambda e: e.activation(out=hT32[:, :, :], in_=pT[:, :].rearrange("p (k t) -> p k t", k=8), func=AF.Copy),
                 reads=[pT], writes=[hT32])
            c.op("dve", lambda e: e.tensor_copy(out=hTb[:, :, :], in_=hT32[:, :, :]), reads=[hT32], writes=[hTb])
            if LV == 4:
                continue
            for hf in range(2):
                for kc in range(8):
                    c.op("pe", lambda e: e.matmul(pk[hf][:, :], lhsT=hTb[:, kc, :], rhs=wkv[:, kc, hf * 512:(hf + 1) * 512],
                                                  start=(kc == 0), stop=(kc == 7)), reads=[hTb, wkv], writes=[pk[hf]])
                c.op("act", lambda e: e.activation(out=ksb[:, hf * 512:(hf + 1) * 512], in_=pk[hf][:, :], func=AF.Copy),
                     reads=[pk[hf]], writes=[ksb])
            if LV == 5:
                continue
            SK = os.environ.get("KVSKIP", "")
            if "k" not in SK:
                head_rms(c, ksb, kn, sq, ssq, kgb, 1.0)
            if "t" not in SK:
                pair_transpose_store(c, k, kn, ptb, stg[i % 2], kT_d, i * P)
            v = vt[i % 2]
            for hf in range(2 if "v" not in SK else 0):
                for kc in range(8):
                    c.op("pe", lambda e: e.matmul(pk[hf][:, :], lhsT=hTb[:, kc, :], rhs=wkv[:, kc, D + hf * 512:D + (hf + 1) * 512],
                                                  start=(kc == 0), stop=(kc == 7)), reads=[hTb, wkv], writes=[pk[hf]])
                c.op("act", lambda e: e.activation(out=v[:, hf * 8:(hf + 1) * 8, 0:64],
                                                   in_=pk[hf][:, :].rearrange("p (h d) -> p h d", d=64), func=AF.Copy),
                     reads=[pk[hf]], writes=[v])
            c.dma("sp", lambda e: e.dma_start(out=v_d.ap()[i * P:(i + 1) * P, :, :], in_=v[:, :, :]), reads=[v], writes=[v_d], nowaw=True)
            if "f" in SK:
                continue
            f = fz[i % 2]
            for kc in range(8):
                c.op("pe", lambda e: e.matmul(pf[:, :], lhsT=hT32[:, kc, :], rhs=wf[:, kc, :], start=(kc == 0), stop=(kc == 7)),
                     reads=[hT32, wf], writes=[pf])
            c.op("dve", lambda e: e.tensor_tensor(out=f[:, :], in0=pf[:, :], in1=bfb[:, :], op=ALU.add), reads=[pf, bfb], writes=[f])
            c.op("act", lambda e: e.activation(out=f[:, :], in_=f[:, :], func=AF.Exp, scale=-1.0), reads=[f], writes=[f])
            c.op("act", lambda e: e.activation(out=f[:, :], in_=f[:, :], func=AF.Ln, bias=1.0), reads=[f], writes=[f])
            c.op("dve", lambda e: e.tensor_scalar(out=f[:, :], in0=f[:, :], scalar1=-1.0, scalar2=None, op0=ALU.mult), reads=[f], writes=[f])
            c.dma("sp", lambda e: e.dma_start(out=lf_d.ap()[i * P:(i + 1) * P, :], in_=f[:, :]), reads=[f], writes=[lf_d], nowaw=True)
    c.finish([])
    c.close()
    return nc


def build_fcum():
    NTL = SEQ // P
    nc = bass.Bass("TRN2", target_bir_lowering=False)
    c = Ctx(nc)
    cd = declare(c, CONST_SPECS)
    lf_d = c.dram("logf", [SEQ, 16], F32, kind="ExternalInput")
    ft_d = c.dram("ft", [16, 3, SEQ], BF16, kind="ExternalOutput")
    fn_d = c.dram("ftn", [16, 3, SEQ], BF16, kind="ExternalOutput")
    k = load_consts(c, cd)
    with c.scope():
        lf = c.sbuf([P, NTL, 16], F32, dma=True)
        F = c.sbuf([16, SEQ], F32)
        R = c.sbuf([16, 4096], F32)
        base = c.sbuf([16, NTL + 1], F32)
        sp3s = [c.sbuf([16, 3, 4096], BF16) for _ in range(2)]
        ps = [c.psum([16, 512], F32) for _ in range(2)]
        c.dma("sp", lambda e: e.dma_start(out=lf[:, :, :], in_=lf_d.ap().rearrange("(n p) h -> p n h", p=P)), writes=[lf])
        for g in range(NTL // 4):
            pg = ps[g % 2]
            for q in range(4):
                n = g * 4 + q
                c.op("pe", lambda e: e.matmul(pg[:, q * P:(q + 1) * P], lhsT=lf[:, n, :], rhs=k.tril[:, :], start=True, stop=True),
                     reads=[lf, k.tril], writes=[pg])
            c.op("act", lambda e: e.activation(out=F[:, g * 512:(g + 1) * 512], in_=pg[:, :], func=AF.Copy), reads=[pg], writes=[F])
        c.op("dve", lambda e: e.memset(base[:, 0:1], 0.0), writes=[base])
        for n in range(NTL):
            c.op("dve", lambda e: e.tensor_tensor(out=base[:, n + 1:n + 2], in0=base[:, n:n + 1], in1=F[:, n * P + P - 1:n * P + P],
                                                  op=ALU.add), reads=[base, F], writes=[base])
        for n in range(1, NTL):
            c.op("dve", lambda e: e.tensor_scalar(out=F[:, n * P:(n + 1) * P], in0=F[:, n * P:(n + 1) * P], scalar1=base[:, n:n + 1],
                                                  scalar2=None, op0=ALU.add), reads=[F, base], writes=[F])
        CH = 4096
        for ci in range(SEQ // CH):
            Fc = F[:, ci * CH:(ci + 1) * CH]
            sp = sp3s[ci % 2]
            c.op("dve", lambda e: e.tensor_copy(out=sp[:, 0, :], in_=Fc), reads=[F], writes=[sp])
            c.op("dve", lambda e: e.tensor_tensor(out=R[:, :], in0=Fc, in1=sp[:, 0, :], op=ALU.subtract), reads=[F, sp], writes=[R])
            c.op("dve", lambda e: e.tensor_copy(out=sp[:, 1, :], in_=R[:, :]), reads=[R], writes=[sp])
            c.op("dve", lambda e: e.tensor_tensor(out=R[:, :], in0=R[:, :], in1=sp[:, 1, :], op=ALU.subtract), reads=[R, sp], writes=[R])
            c.op("dve", lambda e: e.tensor_copy(out=sp[:, 2, :], in_=R[:, :]), reads=[R], writes=[sp])
            c.dma("sp", lambda e: e.dma_start(out=ft_d.ap()[:, :, ci * CH:(ci + 1) * CH], in_=sp[:, :, :]), reads=[sp], writes=[ft_d], nowaw=True)
            for q in range(3):
                c.op("dve", lambda e: e.tensor_scalar(out=sp[:, q, :], in0=sp[:, q, :], scalar1=-1.0, scalar2=None, op0=ALU.mult),
                     reads=[sp], writes=[sp])
            c.dma("sp", lambda e: e.dma_start(out=fn_d.ap()[:, :, ci * CH:(ci + 1) * CH], in_=sp[:, :, :]), reads=[sp], writes=[fn_d], nowaw=True)
    c.finish([])
    c.close()
    return nc


ATT_W = dict(w_qg=([D, 2 * D], F32), q_g=([1, 64], F32), w_o=([D, D], F32))
NQB = TOWN // 512


def stage_attn(c, k, modsb, xin_d, w, kf_d, v_d, qf_d, mask_d, xmid_d):
    qT_d = c.dram("qT_s", [16, 70, TOWN], BF16)
    sg_d = c.dram("sgT_s", [16, 64, TOWN], BF16)
    og_d = c.dram("ogT_s", [16, 64, TOWN], BF16)
    c.dma("sp", lambda e: e.dma_start(out=qT_d.ap()[:, 64:70, :], in_=qf_d.ap()), writes=[qT_d])
    with c.scope():
        wq = c.sbuf([P, 8, 2 * D], BF16, dma=True)
        qgb = c.sbuf([P, 64], F32, dma=True)
        wv = w["w_qg"].ap().rearrange("(kc p) n -> p kc n", p=P)
        for kc in range(8):
            c.dma("pool", lambda e: e.dma_start(out=wq[:, kc, :], in_=wv[:, kc, :]), writes=[wq], nowaw=True)
        c.dma("sp", lambda e: e.dma_start(out=qgb[:, :], in_=w["q_g"].ap().partition_broadcast(P)), writes=[qgb])
        xts = [c.sbuf([P, D], F32, dma=True) for _ in range(2)]
        h = c.sbuf([P, D], F32)
        ss = c.sbuf([P, 2], F32)
        hTb = c.sbuf([P, 8, P], BF16)
        qsb = c.sbuf([P, D], F32)
        sq = c.sbuf([P, D], F32)
        ssq = c.sbuf([P, 16], F32)
        qn = c.sbuf([P, D], BF16)
        sgn = c.sbuf([P, D], BF16)
        stg = [c.sbuf([P, 8, P], BF16) for _ in range(4)]
        pT = c.psum([P, D], F32)
        ptb = c.psum([P, D], BF16)
        pk = [c.psum([P, 512], F32) for _ in range(2)]
        for i in range(TOWN // P):
            xt = xts[i % 2]
            c.dma("sp", lambda e: e.dma_start(out=xt[:, :], in_=xin_d.ap()[i * P:(i + 1) * P, :]), writes=[xt])
            norm_mod(c, xt, h, ss, 1 * D, 0, modsb)
            for kc in range(8):
                c.op("pe", lambda e: e.transpose(out=pT[:, kc * P:(kc + 1) * P], in_=h[:, kc * P:(kc + 1) * P], identity=k.identf[:, :]),
                     reads=[h, k.identf], writes=[pT])
            c.op("act", lambda e: e.activation(out=hTb[:, :, :], in_=pT[:, :].rearrange("p (k t) -> p k t", k=8), func=AF.Copy),
                 reads=[pT], writes=[hTb])
            for hf in range(2):
                for kc in range(8):
                    c.op("pe", lambda e: e.matmul(pk[hf][:, :], lhsT=hTb[:, kc, :], rhs=wq[:, kc, hf * 512:(hf + 1) * 512],
                                                  start=(kc == 0), stop=(kc == 7)), reads=[hTb, wq], writes=[pk[hf]])
                c.op("act", lambda e: e.activation(out=qsb[:, hf * 512:(hf + 1) * 512], in_=pk[hf][:, :], func=AF.Copy),
                     reads=[pk[hf]], writes=[qsb])
            head_rms(c, qsb, qn, sq, ssq, qgb, 0.125)
            pair_transpose_store(c, k, qn, ptb, stg[(2 * i) % 4], qT_d, i * P)
            for hf in range(2):
                for kc in range(8):
                    c.op("pe", lambda e: e.matmul(pk[hf][:, :], lhsT=hTb[:, kc, :], rhs=wq[:, kc, D + hf * 512:D + (hf + 1) * 512],
                                                  start=(kc == 0), stop=(kc == 7)), reads=[hTb, wq], writes=[pk[hf]])
                c.op("act", lambda e: e.activation(out=sgn[:, hf * 512:(hf + 1) * 512], in_=pk[hf][:, :], func=AF.Sigmoid),
                     reads=[pk[hf]], writes=[sgn])
            pair_transpose_store(c, k, sgn, ptb, stg[(2 * i + 1) % 4], sg_d, i * P)
    with c.scope():
        NKT = SEQ // P
        kf = [c.sbuf([70, SEQ], BF16, dma=True) for _ in range(2)]
        vv = [c.sbuf([P, NKT, 72], BF16, dma=True) for _ in range(2)]
        qq = [c.sbuf([70, TOWN], BF16, dma=True) for _ in range(2)]
        sgh = [c.sbuf([64, TOWN], BF16, dma=True) for _ in range(2)]
        mk = c.sbuf([P, 16, 512], BF16, dma=True)
        ssb = [c.sbuf([P, 512], F32) for _ in range(3)]
        pTb = [c.sbuf([P, 512], BF16) for _ in range(4)]
        rinv = c.sbuf([P, 512], F32)
        bcs = c.sbuf([64, 512], F32)
        on = c.sbuf([64, 512], F32)
        og = [c.sbuf([64, 512], BF16) for _ in range(2)]
        pS = [c.psum([P, 512], F32) for _ in range(4)]
        pO = [c.psum([P, 512], F32) for _ in range(2)]
        pB = c.psum([64, 512], F32)
        c.dma("sp", lambda e: e.dma_start(out=mk[:, :, :], in_=mask_d.ap()), writes=[mk])

        def load_head(hh):
            s = hh % 2
            c.dma("sp", lambda e: e.dma_start(out=kf[s][:, :], in_=kf_d.ap()[hh]), writes=[kf[s]])
            c.dma("sp", lambda e: e.dma_start(out=vv[s][:, :, :], in_=v_d.ap()[hh]), writes=[vv[s]])
            c.dma("sp", lambda e: e.dma_start(out=qq[s][:, :], in_=qT_d.ap()[hh]), reads=[qT_d], writes=[qq[s]])
            c.dma("sp", lambda e: e.dma_start(out=sgh[s][:, :], in_=sg_d.ap()[hh]), reads=[sg_d], writes=[sgh[s]])

        load_head(0)
        NH_RUN = int(os.environ.get("ATT_HEADS", "16"))
        NS, LA = 4, 2
        units = []
        nq = 0
        for hh in range(NH_RUN):
            for j in range(NQB):
                nkt = 16 * (j + 1)
                for kt in range(nkt):
                    units.append((hh, j, kt, nkt, nq))
                nq += 1
        epi = {}

        def emit_qk(i):
            hh, j, kt, nkt, nq_ = units[i]
            s = hh % 2
            ps_ = pS[i % NS]
            c.op("pe", lambda e: e.matmul(ps_[:, :], lhsT=kf[s][:, kt * P:(kt + 1) * P], rhs=qq[s][:, j * 512:(j + 1) * 512],
                                          start=True, stop=True), reads=[kf[s], qq[s]], writes=[ps_])
            if kt >= 16 * j:
                sb_ = ssb[i % 3]
                c.op("dve", lambda e: e.tensor_tensor(out=sb_[:, :], in0=ps_[:, :], in1=mk[:, kt - 16 * j, :], op=ALU.add),
                     reads=[ps_, mk], writes=[sb_])

        def emit_rest(i):
            hh, j, kt, nkt, nq_ = units[i]
            s = hh % 2
            if j == 0 and kt == 6 and hh + 1 < NH_RUN:
                load_head(hh + 1)
            ps_ = pS[i % NS]
            pt_ = pTb[i % NS]
            po = pO[nq_ % 2]
            if kt >= 16 * j:
                sb_ = ssb[i % 3]
                c.op("act", lambda e: e.activation(out=pt_[:, :], in_=sb_[:, :], func=AF.Exp), reads=[sb_], writes=[pt_])
            else:
                c.op("act", lambda e: e.activation(out=pt_[:, :], in_=ps_[:, :], func=AF.Exp), reads=[ps_], writes=[pt_])
            c.op("pe", lambda e: e.matmul(po[0:65, :], lhsT=vv[s][:, kt, 0:65], rhs=pt_[:, :], start=(kt == 0), stop=(kt == nkt - 1)),
                 reads=[vv[s], pt_], writes=[po])
            if kt == nkt - 1:
                c.op("dve", lambda e: e.reciprocal(out=rinv[64:65, :], in_=po[64:65, :]), reads=[po], writes=[rinv])
                epi[i + 2] = (hh, j, nq_)

        def emit_epi(hh, j, nq_):
            s = hh % 2
            po = pO[nq_ % 2]
            o_ = og[nq_ % 2]
            c.op("pe", lambda e: e.matmul(pB[:, :], lhsT=k.onesf[64:65, 0:64], rhs=rinv[64:65, :], start=True, stop=True),
                 reads=[k.onesf, rinv], writes=[pB])
            c.op("act", lambda e: e.activation(out=bcs[:, :], in_=pB[:, :], func=AF.Copy), reads=[pB], writes=[bcs])
            c.op("dve", lambda e: e.tensor_tensor(out=on[:, :], in0=po[0:64, :], in1=bcs[:, :], op=ALU.mult), reads=[po, bcs], writes=[on])
            c.op("pool", lambda e: e.tensor_tensor(out=o_[:, :], in0=on[:, :], in1=sgh[s][:, j * 512:(j + 1) * 512], op=ALU.mult),
                 reads=[on, sgh[s]], writes=[o_])
            c.dma("sp", lambda e: e.dma_start(out=og_d.ap()[hh, :, j * 512:(j + 1) * 512], in_=o_[:, :]), reads=[o_], writes=[og_d], nowaw=True)

        NU = len(units)
        for idx in range(NU + LA + 3):
            if idx < NU:
                emit_qk(idx)
            if 0 <= idx - LA < NU:
                emit_rest(idx - LA)
            if (idx - LA) in epi:
                emit_epi(*epi.pop(idx - LA))
        assert not epi
    with c.scope():
        wo = c.sbuf([64, 16, D], BF16, dma=True)
        c.dma("pool", lambda e: e.dma_start(out=wo[:, :, :], in_=w["w_o"].ap().rearrange("(h d) n -> d h n", d=64)), writes=[wo])
        ogt = [c.sbuf([64, 16, P], BF16, dma=True) for _ in range(2)]
        xrs = [c.sbuf([P, D], F32, dma=True) for _ in range(2)]
        xos = [c.sbuf([P, D], F32) for _ in range(2)]
        tmp = c.sbuf([P, 512], F32)
        py = [c.psum([P, 512], F32) for _ in range(2)]
        for i in range(TOWN // P):
            o_ = ogt[i % 2]
            xr = xrs[i % 2]
            xo = xos[i % 2]
            c.dma("sp", lambda e: e.dma_start(out=o_[:, :, :], in_=og_d.ap()[:, :, i * P:(i + 1) * P].rearrange("h d t -> d h t")),
                  reads=[og_d], writes=[o_])
            c.dma("sp", lambda e: e.dma_start(out=xr[:, :], in_=xin_d.ap()[i * P:(i + 1) * P, :]), writes=[xr])
            for hf in range(2):
                for hh in range(16):
                    c.op("pe", lambda e: e.matmul(py[hf][:, :], lhsT=o_[:, hh, :], rhs=wo[:, hh, hf * 512:(hf + 1) * 512],
                                                  start=(hh == 0), stop=(hh == 15)), reads=[o_, wo], writes=[py[hf]])
                c.op("dve", lambda e: e.tensor_tensor(out=tmp[:, :], in0=py[hf][:, :], in1=modsb[:, 2 * D + hf * 512:2 * D + (hf + 1) * 512],
                                                      op=ALU.mult), reads=[py[hf], modsb], writes=[tmp])
                c.op("dve", lambda e: e.tensor_tensor(out=xo[:, hf * 512:(hf + 1) * 512], in0=tmp[:, :], in1=xr[:, hf * 512:(hf + 1) * 512],
                                                      op=ALU.add), reads=[tmp, xr], writes=[xo])
            c.dma("sp", lambda e: e.dma_start(out=xmid_d.ap()[i * P:(i + 1) * P, :], in_=xo[:, :]), reads=[xo], writes=[xmid_d], nowaw=True)


def build_attn_layer(final):
    nc = bass.Bass("TRN2", target_bir_lowering=False)
    c = Ctx(nc)
    cd = declare(c, CONST_SPECS)
    mw = declare(c, MOD_W)
    aw = declare(c, ATT_W)
    ew = declare(c, MOE_W)
    xin = c.dram("xin", [TOWN, D], F32, kind="ExternalInput")
    kf_d = c.dram("kf", [16, 70, SEQ], BF16, kind="ExternalInput")
    v_d = c.dram("vh", [16, P, SEQ // P, 72], BF16, kind="ExternalInput")
    qf_d = c.dram("qf", [16, 6, TOWN], BF16, kind="ExternalInput")
    mask_d = c.dram("maskadd", [P, 16, 512], BF16, kind="ExternalInput")
    fg = c.dram("final_g", [1, D], F32, kind="ExternalInput") if final else None
    xout = c.dram("xout", [TOWN, D], F32, kind="ExternalOutput")
    xmid = c.dram("xmid", [TOWN, D], F32)
    scr = moe_scratch(c, TOWN)
    k = load_consts(c, cd)
    modsb = c.sbuf([P, 6 * D], F32)
    stage_mods(c, k, mw["c_r"], mw["mod_w"], mw["mod_b"], mw["norm_g"], modsb)
    stage_attn(c, k, modsb, xin, aw, kf_d, v_d, qf_d, mask_d, xmid)
    stage_moe(c, k, modsb, xmid, ew, scr, xout, final_g=fg)
    c.finish([xout])
    c.close()
    return nc


_PROGS = {}


def _prog(name, fn, *a):
    if name not in _PROGS:
        _PROGS[name] = fn(*a)
    return _PROGS[name]


def _run(nc, in_maps):
    res = run_bass_kernel_spmd(nc, in_maps, core_ids=list(range(NCORES)))
    return res.results


def kernel(**inp):
    inp = {k_: np.asarray(v_) for k_, v_ in inp.items()}
    hc = host_consts()
    x = inp["x"]
    xs = [x[b] for b in range(2)]
    for l in range(2):
        maps = []
        for core in range(NCORES):
            b, r = divmod(core, 4)
            t0 = r * TOWN
            xin = np.zeros((P + TOWN, D), np.float32)
            xin[P:] = xs[b][t0:t0 + TOWN]
            if r > 0:
                xin[:P] = xs[b][t0 - P:t0]
            m = dict(hc)
            m.update(mod_inputs(inp, l, b)); m.update(conv_inputs(inp, l)); m.update(moe_inputs(inp, l))
            m["xin"] = xin
            m["flag"] = np.full((P, 1), 1.0 if r > 0 else 0.0, np.float32)
            maps.append(m)
        res = _run(_prog("conv", build_conv_layer), maps)
        xs = [np.concatenate([res[b * 4 + r]["xout"] for r in range(4)], axis=0) for b in range(2)]
    maps = []
    for core in range(NCORES):
        b, r = divmod(core, 4)
        m = dict(hc)
        m.update(dict(c_r=pcol(inp["c"][b], 8), mod_w=inp["kv_mod_w"], mod_b=inp["kv_mod_b"][None, :],
                      norm_g=inp["kv_norm_g"][None, :], w_kvf=inp["w_kvf"], b_f=inp["b_f"][None, :], k_g=inp["k_norm_g"][None, :]))
        m["xin"] = np.ascontiguousarray(xs[b][r * TOWN:(r + 1) * TOWN])
        maps.append(m)
    res = _run(_prog("kv", build_kv), maps)
    kT = [np.concatenate([res[b * 4 + r]["kT"] for r in range(4)], axis=2) for b in range(2)]
    vv = [np.concatenate([res[b * 4 + r]["v"] for r in range(4)], axis=0) for b in range(2)]
    lf = [np.concatenate([res[b * 4 + r]["logf"] for r in range(4)], axis=0) for b in range(2)]
    maps = []
    for core in range(NCORES):
        m = dict(hc)
        m["logf"] = np.ascontiguousarray(lf[core % 2])
        maps.append(m)
    res = _run(_prog("fcum", build_fcum), maps)
    ft = [res[b]["ft"] for b in range(2)]
    ftn = [res[b]["ftn"] for b in range(2)]
    one = np.ones((), np.float32).astype(ml_dtypes.bfloat16)
    kf, vh = [], []
    for b in range(2):
        a = np.empty((16, 70, SEQ), ml_dtypes.bfloat16)
        a[:, 0:64] = kT[b]
        a[:, 64:67] = one
        a[:, 67:70] = ftn[b]
        kf.append(a)
        vh.append(np.ascontiguousarray(vv[b].reshape(SEQ // P, P, 16, 72).transpose(2, 1, 0, 3)))
    ii = np.arange(P)
    masks = []
    for r in range(4):
        mk = np.zeros((P, 16, 512), np.float32)
        for tz in range(16):
            spos = (tz // 4) * 512 + (tz % 4) * P + ii[:, None]
            tpos = r * 512 + np.arange(512)[None, :]
            mk[:, tz, :] = np.where(spos <= tpos, 0.0, -30000.0)
        masks.append(mk.astype(ml_dtypes.bfloat16))
    def own_rows(r):
        return np.concatenate([np.arange((4 * j + r) * 512, (4 * j + r + 1) * 512) for j in range(NQB)])
    for li in range(2):
        l = 2 + li
        final = (li == 1)
        maps = []
        for core in range(NCORES):
            b, r = divmod(core, 4)
            rows = own_rows(r)
            m = dict(hc)
            m.update(mod_inputs(inp, l, b)); m.update(moe_inputs(inp, l))
            m.update(dict(w_qg=inp["attn_w_qg"][li], q_g=inp["q_norm_g"][li][None, :], w_o=inp["attn_w_o"][li]))
            m["xin"] = np.ascontiguousarray(xs[b][rows])
            m["kf"] = kf[b]
            m["vh"] = vh[b]
            qf = np.empty((16, 6, TOWN), ml_dtypes.bfloat16)
            qf[:, 0:3] = ft[b][:, :, rows]
            qf[:, 3:6] = one
            m["qf"] = qf
            m["maskadd"] = masks[r]
            if final:
                m["final_g"] = inp["final_norm_g"][None, :]
            maps.append(m)
        res = _run(_prog("attn%d" % final, build_attn_layer, final), maps)
        nx = [np.empty((SEQ, D), np.float32) for _ in range(2)]
        for core in range(NCORES):
            b, r = divmod(core, 4)
            nx[b][own_rows(r)] = res[core]["xout"]
        xs = nx
    return np.stack(xs, axis=0).astype(np.float32)
```

```python
import os
import numpy as np
import ml_dtypes
import concourse.bass as bass
import concourse.mybir as mybir
from concourse.bass_utils import run_bass_kernel_spmd
from contextlib import ExitStack, contextmanager

F32 = mybir.dt.float32
BF16 = mybir.dt.bfloat16
I32 = mybir.dt.int32
ALU = mybir.AluOpType
AF = mybir.ActivationFunctionType
IOA = bass.IndirectOffsetOnAxis

P = 128
D = 1024
NE = 32
EPS = 1e-6
NCORES = 8
SEQ = 16384
TOWN = 4096
BLK = 512
SAME_ENGINE_INORDER = tuple(os.environ.get("INORDER", "pe,sp").split(","))
CONV_CHAIN = os.environ.get("CONV_CHAIN", "1") == "1"


class Buf:
    def __init__(self, t, name, dma_sem_key=None):
        self.t = t
        self.name = name
        self.last_w = None
        self.reads = []
        self.dma_key = dma_sem_key
        self.dma_cnt = 0

    def __getitem__(self, idx):
        return self.t[idx]

    def ap(self):
        return self.t.ap()


class Eng:
    def __init__(self, name, h):
        self.name = name
        self.h = h
        self.key = "e_" + name
        self.cnt = 0
        self.waited = {}


class Ctx:
    def __init__(self, nc):
        self.nc = nc
        self.root = ExitStack()
        self.stack = [self.root]
        self.sems = {}
        self.engs = {}
        self.free_dma_sems = []
        self.live_dma = {}
        for name, h in (("pe", nc.tensor), ("act", nc.scalar), ("dve", nc.vector),
                        ("pool", nc.gpsimd), ("sp", nc.sync)):
            e = Eng(name, h)
            self.sems[e.key] = self.root.enter_context(nc.semaphore(e.key))
            self.engs[name] = e
        self.nbuf = 0
        self.n_inst = 0
        self.sem_cnt = {}

    def _dma_key(self):
        if self.free_dma_sems:
            return self.free_dma_sems.pop()
        key = f"d{len(self.sems)}"
        self.sems[key] = self.root.enter_context(self.nc.semaphore(key))
        self.sem_cnt[key] = 0
        return key

    def sbuf(self, shape, dtype=F32, dma=False, name=None):
        self.nbuf += 1
        name = name or f"sb{self.nbuf}"
        t = self.stack[-1].enter_context(self.nc.sbuf_tensor(name, list(shape), dtype))
        b = Buf(t, name)
        if dma:
            b.dma_key = self._dma_key()
            b.dma_cnt = self.sem_cnt[b.dma_key]
            self.scope_keys[-1].append(b.dma_key) if self.scope_keys else None
        return b

    def psum(self, shape, dtype=F32, name=None):
        self.nbuf += 1
        name = name or f"ps{self.nbuf}"
        t = self.stack[-1].enter_context(self.nc.psum_tensor(name, list(shape), dtype))
        return Buf(t, name)

    def dram(self, name, shape, dtype, kind="Internal"):
        t = self.nc.dram_tensor(name, list(shape), dtype, kind=kind)
        return Buf(t, name)

    scope_keys = []

    @contextmanager
    def scope(self):
        es = ExitStack()
        self.stack.append(es)
        self.scope_keys.append([])
        try:
            yield
        finally:
            self.barrier()
            keys = self.scope_keys.pop()
            self.free_dma_sems.extend(keys)
            self.stack.pop()
            es.close()

    def barrier(self):
        for e in self.engs.values():
            for o in self.engs.values():
                if o is not e and o.cnt > 0:
                    self._wait(e, (o.key, o.cnt))
            for key, cnt in self.sem_cnt.items():
                if cnt > 0:
                    self._wait(e, (key, cnt))

    def _wait(self, eng, tok):
        if tok is None:
            return
        key, val = tok
        if key == eng.key and eng.name in SAME_ENGINE_INORDER:
            return
        if eng.waited.get(key, 0) >= val:
            return
        eng.waited[key] = val
        eng.h.wait_ge(self.sems[key], val)

    def _deps(self, eng, reads, writes, nowaw=False):
        for r in reads:
            self._wait(eng, r.last_w)
        for w in writes:
            if not nowaw:
                self._wait(eng, w.last_w)
            for tok in w.reads:
                self._wait(eng, tok)

    def _commit(self, tok, reads, writes):
        for r in reads:
            r.reads.append(tok)
            if len(r.reads) > 48:
                best = {}
                for k, v in r.reads:
                    best[k] = max(best.get(k, 0), v)
                r.reads = list(best.items())
        for w in writes:
            w.last_w = tok
            w.reads = []

    def op(self, eng_name, fn, reads=(), writes=(), chain=False):
        eng = self.engs[eng_name]
        if chain:
            eng.waited[eng.key] = max(eng.waited.get(eng.key, 0), eng.cnt)
        self._deps(eng, reads, writes)
        inst = fn(eng.h)
        eng.cnt += 1
        inst.then_inc(self.sems[eng.key], 1)
        self._commit((eng.key, eng.cnt), reads, writes)
        self.n_inst += 1

    def dma(self, eng_name, fn, reads=(), writes=(), nowaw=False):
        eng = self.engs[eng_name]
        self._deps(eng, reads, writes, nowaw)
        sb = writes[0]
        if sb.dma_key is None:
            sb.dma_key = self._dma_key()
        inst = fn(eng.h)
        self.sem_cnt[sb.dma_key] += 16
        inst.then_inc(self.sems[sb.dma_key], 16)
        self._commit((sb.dma_key, self.sem_cnt[sb.dma_key]), reads, writes)
        self.n_inst += 1

    def finish(self, bufs):
        self.barrier()

    def close(self):
        self.root.close()


def rms_rstd(c, xt, junk, ss):
    c.op("act", lambda e: e.activation(out=junk[:, :], in_=xt[:, :], func=AF.Square, accum_out=ss[:, 0:1]),
         reads=[xt], writes=[junk, ss])
    c.op("dve", lambda e: e.tensor_scalar(out=ss[:, 1:2], in0=ss[:, 0:1], scalar1=1.0 / D, scalar2=EPS,
                                          op0=ALU.mult, op1=ALU.add), reads=[ss], writes=[ss])
    c.op("dve", lambda e: e.reciprocal(out=ss[:, 1:2], in_=ss[:, 1:2]), reads=[ss], writes=[ss])
    c.op("act", lambda e: e.activation(out=ss[:, 1:2], in_=ss[:, 1:2], func=AF.Sqrt), reads=[ss], writes=[ss])


def norm_mod(c, xt, h, ss, A, sh, modsb):
    rms_rstd(c, xt, h, ss)
    c.op("dve", lambda e: e.scalar_tensor_tensor(out=h[:, :], in0=xt[:, :], scalar=ss[:, 1:2],
                                                 in1=modsb[:, A:A + D], op0=ALU.mult, op1=ALU.mult),
         reads=[xt, ss, modsb], writes=[h])
    c.op("dve", lambda e: e.tensor_tensor(out=h[:, :], in0=h[:, :], in1=modsb[:, sh:sh + D], op=ALU.add),
         reads=[h, modsb], writes=[h])


class Consts:
    pass


def load_consts(c, d):
    k = Consts()
    k.identf = c.sbuf([P, P], F32, dma=True)
    k.identb = c.sbuf([P, P], BF16, dma=True)
    k.triu = c.sbuf([P, P], F32, dma=True)
    k.tril = c.sbuf([P, P], F32, dma=True)
    k.onesf = c.sbuf([P, P], F32)
    k.onesb = c.sbuf([P, P], BF16)
    c.dma("sp", lambda e: e.dma_start(out=k.identf[:, :], in_=d["identf"].ap()), writes=[k.identf])
    c.dma("sp", lambda e: e.dma_start(out=k.identb[:, :], in_=d["identb"].ap()), writes=[k.identb])
    c.dma("sp", lambda e: e.dma_start(out=k.triu[:, :], in_=d["triu"].ap()), writes=[k.triu])
    c.dma("sp", lambda e: e.dma_start(out=k.tril[:, :], in_=d["tril"].ap()), writes=[k.tril])
    c.op("dve", lambda e: e.memset(k.onesf[:, :], 1.0), writes=[k.onesf])
    c.op("dve", lambda e: e.memset(k.onesb[:, :], 1.0), writes=[k.onesb])
    return k


def host_consts():
    ii = np.arange(P)
    return dict(
        identf=np.eye(P, dtype=np.float32),
        identb=np.eye(P, dtype=np.float32).astype(ml_dtypes.bfloat16),
        triu=(ii[:, None] < ii[None, :]).astype(np.float32),
        tril=(ii[:, None] <= ii[None, :]).astype(np.float32),
    )


CONST_SPECS = dict(identf=([P, P], F32), identb=([P, P], BF16), triu=([P, P], F32), tril=([P, P], F32))


def stage_mods(c, k, cr_d, mw_d, mb_d, ng_d, modsb, ncols=6 * D, nnorm=2):
    with c.scope():
        cr = c.sbuf([P, 8], F32, dma=True)
        sg = c.sbuf([P, 8], F32)
        cb = c.sbuf([P, 8, P], F32)
        mbb = c.sbuf([P, ncols], F32, dma=True)
        nb = c.sbuf([P, nnorm, D], F32, dma=True)
        mwt = [c.sbuf([P, 8, 512], F32, dma=True) for _ in range(2)]
        ps = [c.psum([P, 512], F32) for _ in range(2)]
        c.dma("sp", lambda e: e.dma_start(out=cr[:, :], in_=cr_d.ap()), writes=[cr])
        c.dma("sp", lambda e: e.dma_start(out=mbb[:, :], in_=mb_d.ap().partition_broadcast(P)), writes=[mbb])
        for i in range(nnorm):
            c.dma("sp", lambda e: e.dma_start(out=nb[:, i, :], in_=ng_d.ap()[i:i + 1, :].partition_broadcast(P)),
                  writes=[nb])
        c.op("act", lambda e: e.activation(out=sg[:, :], in_=cr[:, :], func=AF.Sigmoid), reads=[cr], writes=[sg])
        c.op("dve", lambda e: e.tensor_tensor(out=sg[:, :], in0=sg[:, :], in1=cr[:, :], op=ALU.mult),
             reads=[sg, cr], writes=[sg])
        for kc in range(8):
            c.op("dve", lambda e: e.tensor_scalar(out=cb[:, kc, :], in0=k.onesf[:, :], scalar1=sg[:, kc:kc + 1],
                                                  scalar2=None, op0=ALU.mult), reads=[k.onesf, sg], writes=[cb])
        mwv = mw_d.ap().rearrange("(kc p) n -> p kc n", p=P)
        for j in range(ncols // 512):
            w = mwt[j % 2]
            pj = ps[j % 2]
            c.dma("sp", lambda e: e.dma_start(out=w[:, :, :], in_=mwv[:, :, j * 512:(j + 1) * 512]), writes=[w])
            for kc in range(8):
                c.op("pe", lambda e: e.matmul(pj[:, :], lhsT=cb[:, kc, :], rhs=w[:, kc, :], start=(kc == 0),
                                              stop=(kc == 7)), reads=[cb, w], writes=[pj])
            c.op("dve", lambda e: e.tensor_tensor(out=modsb[:, j * 512:(j + 1) * 512], in0=pj[:, :],
                                                  in1=mbb[:, j * 512:(j + 1) * 512], op=ALU.add),
                 reads=[pj, mbb], writes=[modsb])
        if nnorm == 2:
            slots = [(1, 0), (4, 1)]
        else:
            slots = [(1, 0)]
        for s, i in slots:
            c.op("dve", lambda e: e.scalar_tensor_tensor(out=modsb[:, s * D:(s + 1) * D], in0=modsb[:, s * D:(s + 1) * D],
                                                         scalar=1.0, in1=nb[:, i, :], op0=ALU.add, op1=ALU.mult),
                 reads=[modsb, nb], writes=[modsb])


def stage_conv(c, k, modsb, xin_d, flag_d, w, xmid_d, town=TOWN):
    CW = 31
    with c.scope():
        w1b = c.sbuf([P, 8, 2 * D], BF16, dma=True)
        w2b = c.sbuf([P, 8, D], BF16, dma=True)
        b1 = c.sbuf([P, 16], F32, dma=True)
        wdw = c.sbuf([P, 8, CW], F32, dma=True)
        bdw = c.sbuf([P, 8], F32, dma=True)
        lng = c.sbuf([P, 8], F32, dma=True)
        lnb = c.sbuf([P, 8], F32, dma=True)
        b2b = c.sbuf([1, D], BF16, dma=True)
        flag = c.sbuf([P, 1], F32, dma=True)
        w1v = w["pw1"].ap().rearrange("(kc p) n -> p kc n", p=P)
        w2v = w["pw2"].ap().rearrange("(kc p) n -> p kc n", p=P)
        for kc in range(8):
            c.dma("pool", lambda e: e.dma_start(out=w1b[:, kc, :], in_=w1v[:, kc, :]), writes=[w1b], nowaw=True)
            c.dma("pool", lambda e: e.dma_start(out=w2b[:, kc, :], in_=w2v[:, kc, :]), writes=[w2b], nowaw=True)
        c.dma("pool", lambda e: e.dma_start(out=b2b[:, :], in_=w["b_pw2"].ap()), writes=[b2b])
        for t, src in ((b1, "b_pw1"), (wdw, "w_dw"), (bdw, "b_dw"), (lng, "ln_g"), (lnb, "ln_b")):
            if t is wdw:
                c.dma("sp", lambda e: e.dma_start(out=t[:, :, :], in_=w[src].ap()), writes=[t])
            else:
                c.dma("sp", lambda e: e.dma_start(out=t[:, :], in_=w[src].ap()), writes=[t])
        c.dma("sp", lambda e: e.dma_start(out=flag[:, :], in_=flag_d.ap()), writes=[flag])

        xts = [c.sbuf([P, D], F32, dma=True) for _ in range(2)]
        xrs = [c.sbuf([P, D], F32, dma=True) for _ in range(2)]
        xos = [c.sbuf([P, D], F32) for _ in range(2)]
        h = c.sbuf([P, D], F32)
        ss = c.sbuf([P, 2], F32)
        hT = c.sbuf([P, 8, BLK], BF16)
        uT = c.sbuf([P, 8, 30 + BLK], BF16)
        acc = [c.sbuf([P, BLK], F32) for _ in range(8)]
        vb = c.sbuf([P, 8, BLK], BF16)
        v2 = c.sbuf([P, 8, BLK], BF16)
        sT = c.sbuf([P, 8, BLK], BF16)
        sgt = [c.sbuf([P, BLK], F32) for _ in range(2)]
        mean = c.sbuf([P, BLK], F32)
        msq = c.sbuf([P, BLK], F32)
        rstd = c.sbuf([P, BLK], F32)
        tmp = c.sbuf([P, 512], F32)
        pT = c.psum([P, D], F32)
        psA = [c.psum([P, BLK], F32) for _ in range(2)]
        psG = [c.psum([P, BLK], F32) for _ in range(2)]
        psO = [c.psum([P, 512], F32) for _ in range(2)]
        c.op("dve", lambda e: e.memset(uT[:, :, 0:30], 0.0), writes=[uT])

        blocks = [(0, P, True)] + [(P + i * BLK, BLK, False) for i in range(town // BLK)]
        nx = 0
        for (t0, n, is_halo) in blocks:
            nt = n // P
            for i in range(nt):
                xt = xts[nx % 2]
                nx += 1
                r0 = t0 + i * P
                c.dma("sp", lambda e: e.dma_start(out=xt[:, :], in_=xin_d.ap()[r0:r0 + P, :]), writes=[xt])
                norm_mod(c, xt, h, ss, 1 * D, 0 * D, modsb)
                for kc in range(8):
                    c.op("pe", lambda e: e.transpose(out=pT[:, kc * P:(kc + 1) * P], in_=h[:, kc * P:(kc + 1) * P],
                                                     identity=k.identf[:, :]), reads=[h, k.identf], writes=[pT])
                c.op("act", lambda e: e.activation(out=hT[:, :, i * P:(i + 1) * P],
                                                   in_=pT[:, :].rearrange("p (k t) -> p k t", k=8), func=AF.Copy),
                     reads=[pT], writes=[hT])
            for fc in range(8):
                pa, pg, sg = psA[fc % 2], psG[fc % 2], sgt[fc % 2]
                for kc in range(8):
                    c.op("pe", lambda e: e.matmul(pa[:, 0:n], lhsT=w1b[:, kc, fc * P:(fc + 1) * P], rhs=hT[:, kc, 0:n],
                                                  start=(kc == 0), stop=(kc == 7)), reads=[w1b, hT], writes=[pa])
                for kc in range(8):
                    c.op("pe", lambda e: e.matmul(pg[:, 0:n], lhsT=w1b[:, kc, D + fc * P:D + (fc + 1) * P],
                                                  rhs=hT[:, kc, 0:n], start=(kc == 0), stop=(kc == 7)),
                         reads=[w1b, hT], writes=[pg])
                c.op("act", lambda e: e.activation(out=sg[:, 0:n], in_=pg[:, 0:n], func=AF.Sigmoid,
                                                   bias=b1[:, 8 + fc:9 + fc]), reads=[pg, b1], writes=[sg])
                c.op("dve", lambda e: e.scalar_tensor_tensor(out=uT[:, fc, 30:30 + n], in0=pa[:, 0:n],
                                                             scalar=b1[:, fc:fc + 1], in1=sg[:, 0:n],
                                                             op0=ALU.add, op1=ALU.mult),
                     reads=[pa, b1, sg], writes=[uT])
            if is_halo:
                c.op("dve", lambda e: e.tensor_scalar(out=uT[:, :, 30:30 + n], in0=uT[:, :, 30:30 + n],
                                                      scalar1=flag[:, 0:1], scalar2=None, op0=ALU.mult),
                     reads=[uT, flag], writes=[uT])
            else:
                for cc in range(8):
                    en = "dve"
                    a = acc[cc]
                    c.op(en, lambda e: e.tensor_scalar(out=a[:, 0:n], in0=uT[:, cc, 0:n], scalar1=wdw[:, cc, 0:1],
                                                       scalar2=bdw[:, cc:cc + 1], op0=ALU.mult, op1=ALU.add),
                         reads=[uT, wdw, bdw], writes=[a])
                    for j in range(1, CW):
                        c.op(en, lambda e: e.scalar_tensor_tensor(out=a[:, 0:n], in0=uT[:, cc, j:j + n],
                                                                  scalar=wdw[:, cc, j:j + 1], in1=a[:, 0:n],
                                                                  op0=ALU.mult, op1=ALU.add),
                             reads=[uT, wdw, a], writes=[a], chain=CONV_CHAIN)
                for cc in range(8):
                    a = acc[cc]
                    c.op("act", lambda e: e.activation(out=vb[:, cc, 0:n], in_=a[:, 0:n], func=AF.Copy),
                         reads=[a], writes=[vb])
                    c.op("act", lambda e: e.activation(out=v2[:, cc, 0:n], in_=a[:, 0:n], func=AF.Square),
                         reads=[a], writes=[v2])
                s1, s2 = psA[0], psG[0]
                for cc in range(8):
                    c.op("pe", lambda e: e.matmul(s1[:, 0:n], lhsT=k.onesb[:, :], rhs=vb[:, cc, 0:n], start=(cc == 0),
                                                  stop=(cc == 7)), reads=[k.onesb, vb], writes=[s1])
                for cc in range(8):
                    c.op("pe", lambda e: e.matmul(s2[:, 0:n], lhsT=k.onesb[:, :], rhs=v2[:, cc, 0:n], start=(cc == 0),
                                                  stop=(cc == 7)), reads=[k.onesb, v2], writes=[s2])
                c.op("dve", lambda e: e.tensor_scalar(out=mean[:, 0:n], in0=s1[:, 0:n], scalar1=1.0 / D, scalar2=None,
                                                      op0=ALU.mult), reads=[s1], writes=[mean])
                c.op("dve", lambda e: e.tensor_tensor(out=msq[:, 0:n], in0=mean[:, 0:n], in1=mean[:, 0:n], op=ALU.mult),
                     reads=[mean], writes=[msq])
                c.op("dve", lambda e: e.scalar_tensor_tensor(out=rstd[:, 0:n], in0=s2[:, 0:n], scalar=1.0 / D,
                                                             in1=msq[:, 0:n], op0=ALU.mult, op1=ALU.subtract),
                     reads=[s2, msq], writes=[rstd])
                c.op("dve", lambda e: e.tensor_scalar(out=rstd[:, 0:n], in0=rstd[:, 0:n], scalar1=EPS, scalar2=None,
                                                      op0=ALU.add), reads=[rstd], writes=[rstd])
                c.op("dve", lambda e: e.reciprocal(out=rstd[:, 0:n], in_=rstd[:, 0:n]), reads=[rstd], writes=[rstd])
                c.op("act", lambda e: e.activation(out=rstd[:, 0:n], in_=rstd[:, 0:n], func=AF.Sqrt),
                     reads=[rstd], writes=[rstd])
                for cc in range(8):
                    a = acc[cc]
                    en = "dve" if cc < 5 else "pool"
                    c.op(en, lambda e: e.tensor_tensor(out=a[:, 0:n], in0=a[:, 0:n], in1=mean[:, 0:n], op=ALU.subtract),
                         reads=[a, mean], writes=[a])
                    c.op(en, lambda e: e.tensor_tensor(out=a[:, 0:n], in0=a[:, 0:n], in1=rstd[:, 0:n], op=ALU.mult),
                         reads=[a, rstd], writes=[a])
                    c.op("act", lambda e: e.activation(out=sT[:, cc, 0:n], in_=a[:, 0:n], func=AF.Silu,
                                                       scale=lng[:, cc:cc + 1], bias=lnb[:, cc:cc + 1]),
                         reads=[a, lng, lnb], writes=[sT])
                for i in range(nt):
                    r0 = t0 + i * P
                    xr = xrs[i % 2]
                    xo = xos[i % 2]
                    c.dma("sp", lambda e: e.dma_start(out=xr[:, :], in_=xin_d.ap()[r0:r0 + P, :]), writes=[xr])
                    for hf in range(2):
                        po = psO[hf]
                        for cc in range(8):
                            c.op("pe", lambda e: e.matmul(po[:, :], lhsT=sT[:, cc, i * P:(i + 1) * P],
                                                          rhs=w2b[:, cc, hf * 512:(hf + 1) * 512], start=(cc == 0),
                                                          stop=False), reads=[sT, w2b], writes=[po])
                        c.op("pe", lambda e: e.matmul(po[:, :], lhsT=k.onesb[0:1, :], rhs=b2b[0:1, hf * 512:(hf + 1) * 512],
                                                      start=False, stop=True), reads=[k.onesb, b2b], writes=[po])
                        c.op("dve", lambda e: e.tensor_tensor(out=tmp[:, :], in0=po[:, :],
                                                              in1=modsb[:, 2 * D + hf * 512:2 * D + (hf + 1) * 512],
                                                              op=ALU.mult), reads=[po, modsb], writes=[tmp])
                        c.op("dve", lambda e: e.tensor_tensor(out=xo[:, hf * 512:(hf + 1) * 512], in0=tmp[:, :],
                                                              in1=xr[:, hf * 512:(hf + 1) * 512], op=ALU.add),
                             reads=[tmp, xr], writes=[xo])
                    c.dma("sp", lambda e: e.dma_start(out=xmid_d.ap()[r0 - P:r0, :], in_=xo[:, :]),
                          reads=[xo], writes=[xmid_d], nowaw=True)
            c.op("dve", lambda e: e.tensor_copy(out=uT[:, :, 0:30], in_=uT[:, :, n:n + 30]), reads=[uT], writes=[uT])


def stage_moe(c, k, modsb, xmid_d, w, scr, xout_d, T=TOWN, final_g=None):
    NT = T // P
    NB = (T * 4) // BLK + NE
    KMAX = T // BLK
    hbuf_d, table_d, ysl_d = scr["hbuf"], scr["table"], scr["ysl"]
    with c.scope():
        maskall = c.sbuf([P, NT, NE], F32)
        gall = c.sbuf([P, NT, NE], F32)
        s4i = c.sbuf([P, NT, 4], I32)
        ebi = c.sbuf([P, NB], I32)
        widx = c.sbuf([P, NB, 8], I32)
        bidx = c.sbuf([P, NB], I32)
        with c.scope():
            rw = c.sbuf([P, 8, NE], F32, dma=True)
            rbb = c.sbuf([P, NE], F32, dma=True)
            c.dma("sp", lambda e: e.dma_start(out=rw[:, :, :], in_=w["router_w"].ap().rearrange("(kc p) n -> p kc n", p=P)),
                  writes=[rw])
            c.dma("sp", lambda e: e.dma_start(out=rbb[:, :], in_=w["router_b"].ap().partition_broadcast(P)), writes=[rbb])
            xts = [c.sbuf([P, D], F32, dma=True) for _ in range(2)]
            hq = c.sbuf([P, D], F32)
            hbs = [c.sbuf([P, D], BF16) for _ in range(2)]
            hT32 = c.sbuf([P, 8, P], F32)
            ss = c.sbuf([P, 2], F32)
            lg = c.sbuf([P, NE], F32)
            ex = c.sbuf([P, NE], F32)
            m8 = c.sbuf([P, 8], F32)
            sm = c.sbuf([P, 4], F32)
            pT = c.psum([P, D], F32)
            pl = c.psum([P, NE], F32)
            for i in range(NT):
                xt = xts[i % 2]
                hb = hbs[i % 2]
                c.dma("sp", lambda e: e.dma_start(out=xt[:, :], in_=xmid_d.ap()[i * P:(i + 1) * P, :]),
                      reads=[xmid_d], writes=[xt])
                norm_mod(c, xt, hq, ss, 4 * D, 3 * D, modsb)
                c.op("pool", lambda e: e.tensor_copy(out=hb[:, :], in_=hq[:, :]), reads=[hq], writes=[hb])
                c.dma("sp", lambda e: e.dma_start(out=hbuf_d.ap()[i * P:(i + 1) * P, :], in_=hb[:, :]),
                      reads=[hb], writes=[hbuf_d], nowaw=True)
                for kc in range(8):
                    c.op("pe", lambda e: e.transpose(out=pT[:, kc * P:(kc + 1) * P], in_=hq[:, kc * P:(kc + 1) * P],
                                                     identity=k.identf[:, :]), reads=[hq, k.identf], writes=[pT])
                c.op("act", lambda e: e.activation(out=hT32[:, :, :], in_=pT[:, :].rearrange("p (k t) -> p k t", k=8),
                                                   func=AF.Copy), reads=[pT], writes=[hT32])
                for kc in range(8):
                    c.op("pe", lambda e: e.matmul(pl[:, :], lhsT=hT32[:, kc, :], rhs=rw[:, kc, :], start=(kc == 0),
                                                  stop=(kc == 7)), reads=[hT32, rw], writes=[pl])
                c.op("dve", lambda e: e.tensor_tensor(out=lg[:, :], in0=pl[:, :], in1=rbb[:, :], op=ALU.add),
                     reads=[pl, rbb], writes=[lg])
                c.op("dve", lambda e: e.max(out=m8[:, :], in_=lg[:, :]), reads=[lg], writes=[m8])
                c.op("dve", lambda e: e.tensor_scalar(out=maskall[:, i, :], in0=lg[:, :], scalar1=m8[:, 3:4], scalar2=None,
                                                      op0=ALU.is_ge), reads=[lg, m8], writes=[maskall])
                c.op("dve", lambda e: e.tensor_scalar(out=sm[:, 0:1], in0=m8[:, 0:1], scalar1=-1.0, scalar2=None,
                                                      op0=ALU.mult), reads=[m8], writes=[sm])
                c.op("act", lambda e: e.activation(out=ex[:, :], in_=lg[:, :], func=AF.Exp, bias=sm[:, 0:1]),
                     reads=[lg, sm], writes=[ex])
                c.op("dve", lambda e: e.scalar_tensor_tensor(out=ex[:, :], in0=ex[:, :], scalar=1.0, in1=maskall[:, i, :],
                                                             op0=ALU.mult, op1=ALU.mult, accum_out=sm[:, 1:2]),
                     reads=[ex, maskall], writes=[ex, sm])
                c.op("dve", lambda e: e.reciprocal(out=sm[:, 2:3], in_=sm[:, 1:2]), reads=[sm], writes=[sm])
                c.op("dve", lambda e: e.tensor_scalar(out=gall[:, i, :], in0=ex[:, :], scalar1=sm[:, 2:3], scalar2=None,
                                                      op0=ALU.mult), reads=[ex, sm], writes=[gall])
        with c.scope():
            W = NT * NE
            pos = c.sbuf([P, NT, NE], F32)
            cs = c.sbuf([P, NT, NE], F32)
            base = c.sbuf([P, NT + 1, NE], F32)
            key = c.sbuf([P, NT, NE], F32)
            top = c.sbuf([P, NT, 8], F32)
            g4 = c.sbuf([P, NT, 4], F32)
            src = c.sbuf([P, NT, 4, 2], I32)
            tok = c.sbuf([P, NT], I32)
            cnt = c.sbuf([P, NE], F32)
            nbk = c.sbuf([P, NE], F32)
            tmpe = c.sbuf([P, NE], F32)
            pend = c.sbuf([P, NE], F32)
            pstart = c.sbuf([P, NE], F32)
            thr_i = c.sbuf([P, NB], I32)
            thr = c.sbuf([P, NB], F32)
            eb = c.sbuf([P, NB], F32)
            same = c.sbuf([P, NB], F32)
            ebs = c.sbuf([P, NB], F32)
            pk_i = c.sbuf([P, 8], I32)
            pk = c.sbuf([P, 8], F32)
            wf = c.sbuf([P, NB, 8], F32)
            s4f = c.sbuf([P, NT, 4], F32)
            zt = c.sbuf([P, NB * BLK * 2 // P], I32)
            pp = [c.psum([P, 512], F32) for _ in range(2)]
            mflat = maskall[:, :, :].rearrange("p t e -> p (t e)")
            posf = pos[:, :, :].rearrange("p t e -> p (t e)")
            csf = cs[:, :, :].rearrange("p t e -> p (t e)")
            c.op("pool", lambda e: e.memset(zt[:, :], 0), writes=[zt])
            c.dma("sp", lambda e: e.dma_start(out=table_d.ap().rearrange("(p r) w -> p (r w)", p=P), in_=zt[:, :]),
                  reads=[zt], writes=[table_d])
            for j0 in range(0, W, 512):
                n = min(512, W - j0)
                c.op("pe", lambda e: e.matmul(pp[0][:, 0:n], lhsT=k.triu[:, :], rhs=mflat[:, j0:j0 + n], start=True, stop=True),
                     reads=[k.triu, maskall], writes=[pp[0]])
                c.op("pe", lambda e: e.matmul(pp[1][:, 0:n], lhsT=k.onesf[:, :], rhs=mflat[:, j0:j0 + n], start=True, stop=True),
                     reads=[k.onesf, maskall], writes=[pp[1]])
                c.op("dve", lambda e: e.tensor_copy(out=posf[:, j0:j0 + n], in_=pp[0][:, 0:n]), reads=[pp[0]], writes=[pos])
                c.op("act", lambda e: e.activation(out=csf[:, j0:j0 + n], in_=pp[1][:, 0:n], func=AF.Copy),
                     reads=[pp[1]], writes=[cs])
            c.op("dve", lambda e: e.memset(base[:, 0, :], 0.0), writes=[base])
            for i in range(NT):
                c.op("dve", lambda e: e.tensor_tensor(out=base[:, i + 1, :], in0=base[:, i, :], in1=cs[:, i, :], op=ALU.add),
                     reads=[base, cs], writes=[base])
            c.op("dve", lambda e: e.tensor_copy(out=cnt[:, :], in_=base[:, NT, :]), reads=[base], writes=[cnt])
            c.op("dve", lambda e: e.tensor_scalar(out=nbk[:, :], in0=cnt[:, :], scalar1=0.0, scalar2=None, op0=ALU.is_gt),
                 reads=[cnt], writes=[nbk])
            for kk in range(1, KMAX + 1):
                c.op("dve", lambda e: e.tensor_scalar(out=tmpe[:, :], in0=cnt[:, :], scalar1=float(BLK * kk), scalar2=None,
                                                      op0=ALU.is_gt), reads=[cnt], writes=[tmpe])
                c.op("dve", lambda e: e.tensor_tensor(out=nbk[:, :], in0=nbk[:, :], in1=tmpe[:, :], op=ALU.add),
                     reads=[nbk, tmpe], writes=[nbk])
            c.op("dve", lambda e: e.tensor_scalar(out=nbk[:, :], in0=nbk[:, :], scalar1=float(BLK), scalar2=None, op0=ALU.mult),
                 reads=[nbk], writes=[nbk])
            c.op("dve", lambda e: e.tensor_copy(out=pend[:, 0:1], in_=nbk[:, 0:1]), reads=[nbk], writes=[pend])
            for e_ in range(1, NE):
                c.op("dve", lambda e: e.tensor_tensor(out=pend[:, e_:e_ + 1], in0=pend[:, e_ - 1:e_], in1=nbk[:, e_:e_ + 1],
                                                      op=ALU.add), reads=[pend, nbk], writes=[pend])
            c.op("dve", lambda e: e.tensor_tensor(out=pstart[:, :], in0=pend[:, :], in1=nbk[:, :], op=ALU.subtract),
                 reads=[pend, nbk], writes=[pstart])
            c.op("dve", lambda e: e.tensor_tensor(out=pos[:, :, :], in0=pos[:, :, :], in1=base[:, 0:NT, :], op=ALU.add),
                 reads=[pos, base], writes=[pos])
            for i in range(NT):
                c.op("dve", lambda e: e.tensor_tensor(out=pos[:, i, :], in0=pos[:, i, :], in1=pstart[:, :], op=ALU.add),
                     reads=[pos, pstart], writes=[pos])
            c.op("dve", lambda e: e.scalar_tensor_tensor(out=key[:, :, :], in0=pos[:, :, :], scalar=1.0, in1=maskall[:, :, :],
                                                         op0=ALU.add, op1=ALU.mult), reads=[pos, maskall], writes=[key])
            for i in range(NT):
                c.op("dve", lambda e: e.max(out=top[:, i, :], in_=key[:, i, :]), reads=[key], writes=[top])
            c.op("dve", lambda e: e.tensor_scalar(out=s4f[:, :, :], in0=top[:, :, 0:4], scalar1=-1.0, scalar2=None, op0=ALU.add),
                 reads=[top], writes=[s4f])
            c.op("dve", lambda e: e.tensor_copy(out=s4i[:, :, :], in_=s4f[:, :, :]), reads=[s4f], writes=[s4i])
            for i in range(NT):
                for kk in range(4):
                    c.op("dve", lambda e: e.scalar_tensor_tensor(out=tmpe[:, :], in0=key[:, i, :], scalar=top[:, i, kk:kk + 1],
                                                                 in1=gall[:, i, :], op0=ALU.is_equal, op1=ALU.mult,
                                                                 accum_out=g4[:, i, kk:kk + 1]),
                         reads=[key, top, gall], writes=[tmpe, g4])
            c.op("pool", lambda e: e.iota(tok[:, :], pattern=[[P, NT]], base=0, channel_multiplier=1), writes=[tok])
            for kk in range(4):
                c.op("dve", lambda e: e.tensor_copy(out=src[:, :, kk, 0], in_=tok[:, :]), reads=[tok], writes=[src])
            c.op("dve", lambda e: e.tensor_copy(out=src[:, :, :, 1].bitcast(F32), in_=g4[:, :, :]), reads=[g4], writes=[src])
            for i in range(NT):
                for kk in range(4):
                    c.dma("pool", lambda e: e.indirect_dma_start(out=table_d.ap(), out_offset=IOA(ap=s4i[:, i, kk:kk + 1], axis=0),
                                                                 in_=src[:, i, kk, :], in_offset=None),
                          reads=[s4i, src], writes=[table_d], nowaw=(i + kk > 0))
            c.op("pool", lambda e: e.iota(thr_i[:, :], pattern=[[BLK, NB]], base=0, channel_multiplier=0), writes=[thr_i])
            c.op("dve", lambda e: e.tensor_copy(out=thr[:, :], in_=thr_i[:, :]), reads=[thr_i], writes=[thr])
            for b in range(NB):
                c.op("dve", lambda e: e.tensor_scalar(out=tmpe[:, :], in0=pend[:, :], scalar1=thr[:, b:b + 1], scalar2=0.0,
                                                      op0=ALU.is_le, op1=ALU.add, accum_out=eb[:, b:b + 1]),
                     reads=[pend, thr], writes=[tmpe, eb])
            c.op("dve", lambda e: e.tensor_scalar(out=eb[:, :], in0=eb[:, :], scalar1=float(NE - 1), scalar2=None, op0=ALU.min),
                 reads=[eb], writes=[eb])
            c.op("dve", lambda e: e.memset(same[:, :], 0.0), writes=[same])
            c.op("dve", lambda e: e.tensor_tensor(out=same[:, 1:NB], in0=eb[:, 1:NB], in1=eb[:, 0:NB - 1], op=ALU.is_equal),
                 reads=[eb], writes=[same])
            c.op("dve", lambda e: e.memset(same[:, NB // 2:NB // 2 + 1], 0.0), writes=[same])
            c.op("dve", lambda e: e.scalar_tensor_tensor(out=ebs[:, :], in0=same[:, :], scalar=1.0e9, in1=eb[:, :], op0=ALU.mult, op1=ALU.add),
                 reads=[same, eb], writes=[ebs])
            c.op("dve", lambda e: e.tensor_copy(out=ebi[:, :], in_=ebs[:, :]), reads=[ebs], writes=[ebi])
            c.op("pool", lambda e: e.iota(pk_i[:, :], pattern=[[P, 8]], base=0, channel_multiplier=1), writes=[pk_i])
            c.op("dve", lambda e: e.tensor_copy(out=pk[:, :], in_=pk_i[:, :]), reads=[pk_i], writes=[pk])
            for kc in range(8):
                c.op("dve", lambda e: e.tensor_scalar(out=wf[:, :, kc], in0=eb[:, :], scalar1=float(D), scalar2=pk[:, kc:kc + 1],
                                                      op0=ALU.mult, op1=ALU.add), reads=[eb, pk], writes=[wf])
            for kc in range(8):
                c.op("dve", lambda e: e.scalar_tensor_tensor(out=wf[:, :, kc], in0=same[:, :], scalar=1.0e9, in1=wf[:, :, kc],
                                                             op0=ALU.mult, op1=ALU.add), reads=[same, wf], writes=[wf])
            c.op("dve", lambda e: e.tensor_copy(out=widx[:, :, :], in_=wf[:, :, :]), reads=[wf], writes=[widx])
            c.op("dve", lambda e: e.tensor_scalar(out=eb[:, :], in0=eb[:, :], scalar1=float(P), scalar2=pk[:, 0:1],
                                                  op0=ALU.mult, op1=ALU.add), reads=[eb, pk], writes=[eb])
            c.op("dve", lambda e: e.scalar_tensor_tensor(out=eb[:, :], in0=same[:, :], scalar=1.0e9, in1=eb[:, :], op0=ALU.mult, op1=ALU.add),
                 reads=[same, eb], writes=[eb])
            c.op("dve", lambda e: e.tensor_copy(out=bidx[:, :], in_=eb[:, :]), reads=[eb], writes=[bidx])
        with c.scope():
            wg = [c.sbuf([P, 8, 2 * D], BF16, dma=True) for _ in range(2)]
            wd = [c.sbuf([P, 8, D], BF16, dma=True) for _ in range(2)]
            bg = [c.sbuf([P, 16], F32, dma=True) for _ in range(2)]
            bd = [c.sbuf([2, D], BF16, dma=True) for _ in range(2)]
            tk = [c.sbuf([P, 8], I32, dma=True) for _ in range(2)]
            xg = [c.sbuf([P, 4, D], BF16, dma=True) for _ in range(2)]
            xgT = c.sbuf([P, 8, BLK], BF16)
            actT = c.sbuf([P, 8, BLK], BF16)
            yo = [c.sbuf([P, 4, D], BF16) for _ in range(2)]
            t1 = [c.sbuf([P, BLK], F32) for _ in range(2)]
            sg = [c.sbuf([P, BLK], F32) for _ in range(2)]
            xl = [c.sbuf([P, BLK], F32) for _ in range(2)]
            bgp = [c.sbuf([P, 8], F32) for _ in range(2)]
            ptr = [c.psum([P, 2 * BLK], BF16) for _ in range(2)]
            psg = [c.psum([P, BLK], F32) for _ in range(2)]
            psl = [c.psum([P, BLK], F32) for _ in range(2)]
            psd = [c.psum([P, 512], F32) for _ in range(2)]
            wguv = w["w_gu"].ap().rearrange("e k n -> (e k) n")
            wdnv = w["w_down"].ap().rearrange("e k n -> (e k) n")
            bguv = w["b_gu_r"].ap().rearrange("e p n -> (e p) n")

            rg_w = c.nc.gpsimd.to_reg(NE * D - 1)
            rg_b = c.nc.gpsimd.to_reg(NE * P - 1)
            rg_e = c.nc.gpsimd.to_reg(NE - 1)

            def blk(kpos):
                return (kpos // 2) + (NB // 2) * (kpos % 2)

            def prefetch(kpos):
                s = kpos % 2
                b = blk(kpos)
                c.dma("sp", lambda e: e.dma_start(out=tk[s][:, :],
                                                  in_=table_d.ap()[b * BLK:(b + 1) * BLK, :].rearrange("(p j) w -> p (j w)", j=4)),
                      reads=[table_d], writes=[tk[s]])
                for j in range(4):
                    c.dma("pool", lambda e: e.indirect_dma_start(out=xg[s][:, j, :], out_offset=None, in_=hbuf_d.ap(),
                                                                 in_offset=IOA(ap=tk[s][:, 2 * j:2 * j + 1], axis=0)),
                          reads=[tk[s], hbuf_d], writes=[xg[s]], nowaw=(j > 0))
                for kc in range(8):
                    c.dma("pool", lambda e: e.indirect_dma_start(out=wg[s][:, kc, :], out_offset=None, in_=wguv,
                                                                 in_offset=IOA(ap=widx[:, b, kc:kc + 1], axis=0),
                                                                 bounds_check=rg_w, oob_is_err=False),
                          reads=[widx], writes=[wg[s]], nowaw=(kc > 0))
                for kc in range(8):
                    c.dma("pool", lambda e: e.indirect_dma_start(out=wd[s][:, kc, :], out_offset=None, in_=wdnv,
                                                                 in_offset=IOA(ap=widx[:, b, kc:kc + 1], axis=0),
                                                                 bounds_check=rg_w, oob_is_err=False),
                          reads=[widx], writes=[wd[s]], nowaw=(kc > 0))
                c.dma("pool", lambda e: e.indirect_dma_start(out=bg[s][:, :], out_offset=None, in_=bguv,
                                                             in_offset=IOA(ap=bidx[:, b:b + 1], axis=0),
                                                             bounds_check=rg_b, oob_is_err=False),
                      reads=[bidx], writes=[bg[s]])
                c.dma("pool", lambda e: e.indirect_dma_start(out=bd[s][0:2, :], out_offset=None, in_=w["b_down"].ap(),
                                                             in_offset=IOA(ap=ebi[0:2, b:b + 1], axis=0),
                                                             bounds_check=rg_e, oob_is_err=False),
                      reads=[ebi], writes=[bd[s]])

            prefetch(0)
            for kpos in range(NB):
                s = kpos % 2
                b = blk(kpos)
                if kpos + 1 < NB:
                    prefetch(kpos + 1)
                for kc in range(8):
                    pt = ptr[(kc // 2) % 2]
                    o = (kc % 2) * BLK
                    for j in range(4):
                        c.op("pe", lambda e: e.transpose(out=pt[:, o + j * P:o + (j + 1) * P], in_=xg[s][:, j, kc * P:(kc + 1) * P],
                                                         identity=k.identb[:, :]), reads=[xg[s], k.identb], writes=[pt])
                    if kc % 2 == 1:
                        c.op("act", lambda e: e.activation(out=xgT[:, kc - 1:kc + 1, :],
                                                           in_=pt[:, :].rearrange("p (k t) -> p k t", k=2), func=AF.Copy),
                             reads=[pt], writes=[xgT])
                c.op("dve", lambda e: e.tensor_scalar(out=bgp[s][:, :], in0=bg[s][:, 8:16], scalar1=1.0, scalar2=None, op0=ALU.add),
                     reads=[bg[s]], writes=[bgp[s]])
                for fc in range(8):
                    q = fc % 2
                    for kc in range(8):
                        c.op("pe", lambda e: e.matmul(psg[q][:, :], lhsT=wg[s][:, kc, fc * P:(fc + 1) * P], rhs=xgT[:, kc, :],
                                                      start=(kc == 0), stop=(kc == 7)), reads=[wg[s], xgT], writes=[psg[q]])
                    for kc in range(8):
                        c.op("pe", lambda e: e.matmul(psl[q][:, :], lhsT=wg[s][:, kc, D + fc * P:D + (fc + 1) * P], rhs=xgT[:, kc, :],
                                                      start=(kc == 0), stop=(kc == 7)), reads=[wg[s], xgT], writes=[psl[q]])
                    c.op("dve", lambda e: e.tensor_scalar(out=t1[q][:, :], in0=psg[q][:, :], scalar1=bg[s][:, fc:fc + 1], scalar2=7.0,
                                                          op0=ALU.add, op1=ALU.min), reads=[psg[q], bg[s]], writes=[t1[q]])
                    c.op("act", lambda e: e.activation(out=sg[q][:, :], in_=t1[q][:, :], func=AF.Sigmoid, scale=1.702),
                         reads=[t1[q]], writes=[sg[q]])
                    c.op("dve", lambda e: e.tensor_scalar(out=xl[q][:, :], in0=psl[q][:, :], scalar1=bgp[s][:, fc:fc + 1], scalar2=-6.0,
                                                          op0=ALU.add, op1=ALU.max), reads=[psl[q], bgp[s]], writes=[xl[q]])
                    c.op("dve", lambda e: e.tensor_tensor(out=t1[q][:, :], in0=t1[q][:, :], in1=sg[q][:, :], op=ALU.mult),
                         reads=[t1[q], sg[q]], writes=[t1[q]])
                    c.op("dve", lambda e: e.scalar_tensor_tensor(out=actT[:, fc, :], in0=xl[q][:, :], scalar=8.0, in1=t1[q][:, :],
                                                                  op0=ALU.min, op1=ALU.mult), reads=[xl[q], t1[q]], writes=[actT])
                for j in range(4):
                    for hf in range(2):
                        pd = psd[hf]
                        for fc in range(8):
                            c.op("pe", lambda e: e.matmul(pd[:, :], lhsT=actT[:, fc, j * P:(j + 1) * P],
                                                          rhs=wd[s][:, fc, hf * 512:(hf + 1) * 512], start=(fc == 0), stop=False),
                                 reads=[actT, wd[s]], writes=[pd])
                        c.op("pe", lambda e: e.matmul(pd[:, :], lhsT=k.onesb[0:1, :], rhs=bd[s][0:1, hf * 512:(hf + 1) * 512],
                                                      start=False, stop=True), reads=[k.onesb, bd[s]], writes=[pd])
                        c.op("act", lambda e: e.activation(out=yo[s][:, j, hf * 512:(hf + 1) * 512], in_=pd[:, :], func=AF.Copy,
                                                           scale=tk[s][:, 2 * j + 1:2 * j + 2].bitcast(F32)),
                             reads=[pd, tk[s]], writes=[yo[s]])
                c.dma("sp", lambda e: e.dma_start(out=ysl_d.ap()[b * BLK:(b + 1) * BLK, :].rearrange("(p j) d -> p j d", j=4),
                                                  in_=yo[s][:, :, :]), reads=[yo[s]], writes=[ysl_d], nowaw=True)
        with c.scope():
            yg = [c.sbuf([P, 4, D], BF16, dma=True) for _ in range(2)]
            xts = [c.sbuf([P, D], F32, dma=True) for _ in range(2)]
            a0 = c.sbuf([P, D], F32)
            a1 = c.sbuf([P, D], F32)
            xo = [c.sbuf([P, D], F32) for _ in range(2)]
            ss = c.sbuf([P, 2], F32)
            if final_g is not None:
                fg = c.sbuf([P, D], F32, dma=True)
                c.dma("sp", lambda e: e.dma_start(out=fg[:, :], in_=final_g.ap().partition_broadcast(P)), writes=[fg])
            for i in range(NT):
                s = i % 2
                for kk in range(4):
                    c.dma("pool", lambda e: e.indirect_dma_start(out=yg[s][:, kk, :], out_offset=None, in_=ysl_d.ap(),
                                                                 in_offset=IOA(ap=s4i[:, i, kk:kk + 1], axis=0)),
                          reads=[s4i, ysl_d], writes=[yg[s]], nowaw=(kk > 0))
                c.dma("sp", lambda e: e.dma_start(out=xts[s][:, :], in_=xmid_d.ap()[i * P:(i + 1) * P, :]),
                      reads=[xmid_d], writes=[xts[s]])
                c.op("dve", lambda e: e.tensor_tensor(out=a0[:, :], in0=yg[s][:, 0, :], in1=yg[s][:, 1, :], op=ALU.add),
                     reads=[yg[s]], writes=[a0])
                c.op("pool", lambda e: e.tensor_tensor(out=a1[:, :], in0=yg[s][:, 2, :], in1=yg[s][:, 3, :], op=ALU.add),
                     reads=[yg[s]], writes=[a1])
                c.op("dve", lambda e: e.tensor_tensor(out=a0[:, :], in0=a0[:, :], in1=a1[:, :], op=ALU.add),
                     reads=[a0, a1], writes=[a0])
                c.op("dve", lambda e: e.tensor_tensor(out=a0[:, :], in0=a0[:, :], in1=modsb[:, 5 * D:6 * D], op=ALU.mult),
                     reads=[a0, modsb], writes=[a0])
                c.op("dve", lambda e: e.tensor_tensor(out=xo[s][:, :], in0=a0[:, :], in1=xts[s][:, :], op=ALU.add),
                     reads=[a0, xts[s]], writes=[xo[s]])
                if final_g is not None:
                    rms_rstd(c, xo[s], a1, ss)
                    c.op("dve", lambda e: e.scalar_tensor_tensor(out=xo[s][:, :], in0=xo[s][:, :], scalar=ss[:, 1:2], in1=fg[:, :],
                                                                 op0=ALU.mult, op1=ALU.mult), reads=[xo[s], ss, fg], writes=[xo[s]])
                c.dma("sp", lambda e: e.dma_start(out=xout_d.ap()[i * P:(i + 1) * P, :], in_=xo[s][:, :]),
                      reads=[xo[s]], writes=[xout_d], nowaw=True)


def moe_scratch(c, T, tag=""):
    NB = (T * 4) // BLK + NE
    return dict(hbuf=c.dram("hbuf" + tag, [T, D], BF16), table=c.dram("table" + tag, [NB * BLK, 2], I32),
                ysl=c.dram("ysl" + tag, [NB * BLK, D], BF16))


MOE_W = dict(router_w=([D, NE], F32), router_b=([1, NE], F32), w_gu=([NE, D, 2 * D], F32), b_gu_r=([NE, P, 16], F32),
             w_down=([NE, D, D], F32), b_down=([NE, D], F32))
CONV_W = dict(pw1=([D, 2 * D], F32), b_pw1=([P, 16], F32), w_dw=([P, 8, 31], F32), b_dw=([P, 8], F32), ln_g=([P, 8], F32),
              ln_b=([P, 8], F32), pw2=([D, D], F32), b_pw2=([1, D], F32))
MOD_W = dict(c_r=([P, 8], F32), mod_w=([D, 6 * D], F32), mod_b=([1, 6 * D], F32), norm_g=([2, D], F32))


def declare(c, specs, kind="ExternalInput", prefix=""):
    return {n: c.dram(prefix + n, shp, dt, kind=kind) for n, (shp, dt) in specs.items()}


def build_conv_layer():
    nc = bass.Bass("TRN2", target_bir_lowering=False)
    c = Ctx(nc)
    cd = declare(c, CONST_SPECS)
    mw = declare(c, MOD_W)
    cw = declare(c, CONV_W)
    ew = declare(c, MOE_W)
    xin = c.dram("xin", [P + TOWN, D], F32, kind="ExternalInput")
    flag = c.dram("flag", [P, 1], F32, kind="ExternalInput")
    xout = c.dram("xout", [TOWN, D], F32, kind="ExternalOutput")
    xmid = c.dram("xmid", [TOWN, D], F32)
    scr = moe_scratch(c, TOWN)
    k = load_consts(c, cd)
    modsb = c.sbuf([P, 6 * D], F32)
    stage_mods(c, k, mw["c_r"], mw["mod_w"], mw["mod_b"], mw["norm_g"], modsb)
    stage_conv(c, k, modsb, xin, flag, cw, xmid)
    stage_moe(c, k, modsb, xmid, ew, scr, xout)
    c.finish([xout])
    c.close()
    return nc


def pcol(v, n):
    return np.ascontiguousarray(np.asarray(v).reshape(n, P).T)


def mod_inputs(inp, l, b):
    return dict(c_r=pcol(inp["c"][b], 8), mod_w=inp["mod_w"][l], mod_b=inp["mod_b"][l][None, :],
                norm_g=np.stack([inp["norm1_g"][l], inp["norm2_g"][l]]))


def moe_inputs(inp, l):
    bgu = inp["moe_b_gu"][l]
    return dict(router_w=inp["moe_router_w"][l], router_b=inp["moe_router_b"][l][None, :], w_gu=inp["moe_w_gu"][l],
                b_gu_r=np.ascontiguousarray(bgu.reshape(NE, 16, P).transpose(0, 2, 1)), w_down=inp["moe_w_down"][l],
                b_down=inp["moe_b_down"][l])


def conv_inputs(inp, l):
    return dict(pw1=inp["conv_w_pw1"][l], b_pw1=pcol(inp["conv_b_pw1"][l], 16),
                w_dw=np.ascontiguousarray(inp["conv_w_dw"][l].reshape(31, 8, P).transpose(2, 1, 0)),
                b_dw=pcol(inp["conv_b_dw"][l], 8), ln_g=pcol(inp["conv_ln_g"][l], 8), ln_b=pcol(inp["conv_ln_b"][l], 8),
                pw2=inp["conv_w_pw2"][l], b_pw2=inp["conv_b_pw2"][l][None, :])


def head_rms(c, src, dst, sq, ssq, gbc, scale):
    for hh in range(16):
        c.op("dve", lambda e: e.scalar_tensor_tensor(out=sq[:, hh * 64:(hh + 1) * 64], in0=src[:, hh * 64:(hh + 1) * 64], scalar=1.0,
                                                     in1=src[:, hh * 64:(hh + 1) * 64], op0=ALU.mult, op1=ALU.mult,
                                                     accum_out=ssq[:, hh:hh + 1]), reads=[src], writes=[sq, ssq])
    c.op("dve", lambda e: e.tensor_scalar(out=ssq[:, 0:16], in0=ssq[:, 0:16], scalar1=1.0 / 64, scalar2=EPS, op0=ALU.mult,
                                          op1=ALU.add), reads=[ssq], writes=[ssq])
    c.op("dve", lambda e: e.reciprocal(out=ssq[:, 0:16], in_=ssq[:, 0:16]), reads=[ssq], writes=[ssq])
    c.op("act", lambda e: e.activation(out=ssq[:, 0:16], in_=ssq[:, 0:16], func=AF.Sqrt, scale=scale * scale),
         reads=[ssq], writes=[ssq])
    for hh in range(16):
        c.op("dve", lambda e: e.scalar_tensor_tensor(out=dst[:, hh * 64:(hh + 1) * 64], in0=src[:, hh * 64:(hh + 1) * 64],
                                                     scalar=ssq[:, hh:hh + 1], in1=gbc[:, 0:64], op0=ALU.mult, op1=ALU.mult),
             reads=[src, ssq, gbc], writes=[dst])


def pair_transpose_store(c, k, srcb, ptb, stg, dst_d, tok0, rows=64):
    for kc in range(8):
        c.op("pe", lambda e: e.transpose(out=ptb[:, kc * P:(kc + 1) * P], in_=srcb[:, kc * P:(kc + 1) * P], identity=k.identb[:, :]),
             reads=[srcb, k.identb], writes=[ptb])
    c.op("act", lambda e: e.activation(out=stg[:, :, :], in_=ptb[:, :].rearrange("p (k t) -> p k t", k=8), func=AF.Copy),
         reads=[ptb], writes=[stg])
    dv = dst_d.ap().rearrange("(hp two) d t -> two d hp t", two=2)
    for half in range(2):
        c.dma("sp", lambda e: e.dma_start(out=dv[half, 0:64, :, tok0:tok0 + P], in_=stg[half * 64:(half + 1) * 64, :, :]),
              reads=[stg], writes=[dst_d], nowaw=True)


KV_W = dict(c_r=([P, 8], F32), mod_w=([D, 2 * D], F32), mod_b=([1, 2 * D], F32), norm_g=([1, D], F32),
            w_kvf=([D, 2 * D + 16], F32), b_f=([1, 16], F32), k_g=([1, 64], F32))


def build_kv():
    nc = bass.Bass("TRN2", target_bir_lowering=False)
    c = Ctx(nc)
    cd = declare(c, CONST_SPECS)
    w = declare(c, KV_W)
    xin = c.dram("xin", [TOWN, D], F32, kind="ExternalInput")
    kT_d = c.dram("kT", [16, 64, TOWN], BF16, kind="ExternalOutput")
    v_d = c.dram("v", [TOWN, 16, 72], BF16, kind="ExternalOutput")
    lf_d = c.dram("logf", [TOWN, 16], F32, kind="ExternalOutput")
    k = load_consts(c, cd)
    modsb = c.sbuf([P, 2 * D], F32)
    stage_mods(c, k, w["c_r"], w["mod_w"], w["mod_b"], w["norm_g"], modsb, ncols=2 * D, nnorm=1)
    with c.scope():
        wkv = c.sbuf([P, 8, 2 * D], BF16, dma=True)
        wf = c.sbuf([P, 8, 16], F32, dma=True)
        bfb = c.sbuf([P, 16], F32, dma=True)
        kgb = c.sbuf([P, 64], F32, dma=True)
        wv = w["w_kvf"].ap().rearrange("(kc p) n -> p kc n", p=P)
        for kc in range(8):
            c.dma("pool", lambda e: e.dma_start(out=wkv[:, kc, :], in_=wv[:, kc, 0:2 * D]), writes=[wkv], nowaw=True)
        c.dma("sp", lambda e: e.dma_start(out=wf[:, :, :], in_=wv[:, :, 2 * D:2 * D + 16]), writes=[wf])
        c.dma("sp", lambda e: e.dma_start(out=bfb[:, :], in_=w["b_f"].ap().partition_broadcast(P)), writes=[bfb])
        c.dma("sp", lambda e: e.dma_start(out=kgb[:, :], in_=w["k_g"].ap().partition_broadcast(P)), writes=[kgb])
        xts = [c.sbuf([P, D], F32, dma=True) for _ in range(2)]
        h = c.sbuf([P, D], F32)
        ss = c.sbuf([P, 2], F32)
        hT32 = c.sbuf([P, 8, P], F32)
        hTb = c.sbuf([P, 8, P], BF16)
        ksb = c.sbuf([P, D], F32)
        sq = c.sbuf([P, D], F32)
        ssq = c.sbuf([P, 16], F32)
        kn = c.sbuf([P, D], BF16)
        stg = [c.sbuf([P, 8, P], BF16) for _ in range(2)]
        vt = [c.sbuf([P, 16, 72], BF16) for _ in range(2)]
        fz = [c.sbuf([P, 16], F32) for _ in range(2)]
        pT = c.psum([P, D], F32)
        ptb = c.psum([P, D], BF16)
        pk = [c.psum([P, 512], F32) for _ in range(2)]
        pf = c.psum([P, 16], F32)
        for t in vt:
            c.op("dve", lambda e: e.memset(t[:, :, :], 1.0), writes=[t])
        LV = int(os.environ.get("KVSTOP", "9"))
        for i in range(int(os.environ.get("KVTILES", TOWN // P)) if LV > 2 else 0):
            xt = xts[i % 2]
            c.dma("sp", lambda e: e.dma_start(out=xt[:, :], in_=xin.ap()[i * P:(i + 1) * P, :]), writes=[xt])
            norm_mod(c, xt, h, ss, 1 * D, 0, modsb)
            if LV == 3:
                continue
            for kc in range(8):
                c.op("pe", lambda e: e.transpose(out=pT[:, kc * P:(kc + 1) * P], in_=h[:, kc * P:(kc + 1) * P], identity=k.identf[:, :]),
                     reads=[h, k.identf], writes=[pT])
            c.op("act", lambda e: e.activation(out=hT32[:, :, :], in_=pT[:, :].rearrange("p (k t) -> p k t", k=8), func=AF.Copy),
                 reads=[pT], writes=[hT32])
            c.op("dve", lambda e: e.tensor_copy(out=hTb[:, :, :], in_=hT32[:, :, :]), reads=[hT32], writes=[hTb])
            if LV == 4:
                continue
            for hf in range(2):
                for kc in range(8):
                    c.op("pe", lambda e: e.matmul(pk[hf][:, :], lhsT=hTb[:, kc, :], rhs=wkv[:, kc, hf * 512:(hf + 1) * 512],
                                                  start=(kc == 0), stop=(kc == 7)), reads=[hTb, wkv], writes=[pk[hf]])
                c.op("act", lambda e: e.activation(out=ksb[:, hf * 512:(hf + 1) * 512], in_=pk[hf][:, :], func=AF.Copy),
                     reads=[pk[hf]], writes=[ksb])
            if LV == 5:
                continue
            SK = os.environ.get("KVSKIP", "")
            if "k" not in SK:
                head_rms(c, ksb, kn, sq, ssq, kgb, 1.0)
            if "t" not in SK:
                pair_transpose_store(c, k, kn, ptb, stg[i % 2], kT_d, i * P)
            v = vt[i % 2]
            for hf in range(2 if "v" not in SK else 0):
                for kc in range(8):
                    c.op("pe", lambda e: e.matmul(pk[hf][:, :], lhsT=hTb[:, kc, :], rhs=wkv[:, kc, D + hf * 512:D + (hf + 1) * 512],
                                                  start=(kc == 0), stop=(kc == 7)), reads=[hTb, wkv], writes=[pk[hf]])
                c.op("act", lambda e: e.activation(out=v[:, hf * 8:(hf + 1) * 8, 0:64],
                                                   in_=pk[hf][:, :].rearrange("p (h d) -> p h d", d=64), func=AF.Copy),
                     reads=[pk[hf]], writes=[v])
            c.dma("sp", lambda e: e.dma_start(out=v_d.ap()[i * P:(i + 1) * P, :, :], in_=v[:, :, :]), reads=[v], writes=[v_d], nowaw=True)
            if "f" in SK:
                continue
            f = fz[i % 2]
            for kc in range(8):
                c.op("pe", lambda e: e.matmul(pf[:, :], lhsT=hT32[:, kc, :], rhs=wf[:, kc, :], start=(kc == 0), stop=(kc == 7)),
                     reads=[hT32, wf], writes=[pf])
            c.op("dve", lambda e: e.tensor_tensor(out=f[:, :], in0=pf[:, :], in1=bfb[:, :], op=ALU.add), reads=[pf, bfb], writes=[f])
            c.op("act", lambda e: e.activation(out=f[:, :], in_=f[:, :], func=AF.Exp, scale=-1.0), reads=[f], writes=[f])
            c.op("act", lambda e: e.activation(out=f[:, :], in_=f[:, :], func=AF.Ln, bias=1.0), reads=[f], writes=[f])
            c.op("dve", lambda e: e.tensor_scalar(out=f[:, :], in0=f[:, :], scalar1=-1.0, scalar2=None, op0=ALU.mult), reads=[f], writes=[f])
            c.dma("sp", lambda e: e.dma_start(out=lf_d.ap()[i * P:(i + 1) * P, :], in_=f[:, :]), reads=[f], writes=[lf_d], nowaw=True)
    c.finish([])
    c.close()
    return nc


def build_fcum():
    NTL = SEQ // P
    nc = bass.Bass("TRN2", target_bir_lowering=False)
    c = Ctx(nc)
    cd = declare(c, CONST_SPECS)
    lf_d = c.dram("logf", [SEQ, 16], F32, kind="ExternalInput")
    ft_d = c.dram("ft", [16, 3, SEQ], BF16, kind="ExternalOutput")
    fn_d = c.dram("ftn", [16, 3, SEQ], BF16, kind="ExternalOutput")
    k = load_consts(c, cd)
    with c.scope():
        lf = c.sbuf([P, NTL, 16], F32, dma=True)
        F = c.sbuf([16, SEQ], F32)
        R = c.sbuf([16, 4096], F32)
        base = c.sbuf([16, NTL + 1], F32)
        sp3s = [c.sbuf([16, 3, 4096], BF16) for _ in range(2)]
        ps = [c.psum([16, 512], F32) for _ in range(2)]
        c.dma("sp", lambda e: e.dma_start(out=lf[:, :, :], in_=lf_d.ap().rearrange("(n p) h -> p n h", p=P)), writes=[lf])
        for g in range(NTL // 4):
            pg = ps[g % 2]
            for q in range(4):
                n = g * 4 + q
                c.op("pe", lambda e: e.matmul(pg[:, q * P:(q + 1) * P], lhsT=lf[:, n, :], rhs=k.tril[:, :], start=True, stop=True),
                     reads=[lf, k.tril], writes=[pg])
            c.op("act", lambda e: e.activation(out=F[:, g * 512:(g + 1) * 512], in_=pg[:, :], func=AF.Copy), reads=[pg], writes=[F])
        c.op("dve", lambda e: e.memset(base[:, 0:1], 0.0), writes=[base])
        for n in range(NTL):
            c.op("dve", lambda e: e.tensor_tensor(out=base[:, n + 1:n + 2], in0=base[:, n:n + 1], in1=F[:, n * P + P - 1:n * P + P],
                                                  op=ALU.add), reads=[base, F], writes=[base])
        for n in range(1, NTL):
            c.op("dve", lambda e: e.tensor_scalar(out=F[:, n * P:(n + 1) * P], in0=F[:, n * P:(n + 1) * P], scalar1=base[:, n:n + 1],
                                                  scalar2=None, op0=ALU.add), reads=[F, base], writes=[F])
        CH = 4096
        for ci in range(SEQ // CH):
            Fc = F[:, ci * CH:(ci + 1) * CH]
            sp = sp3s[ci % 2]
            c.op("dve", lambda e: e.tensor_copy(out=sp[:, 0, :], in_=Fc), reads=[F], writes=[sp])
            c.op("dve", lambda e: e.tensor_tensor(out=R[:, :], in0=Fc, in1=sp[:, 0, :], op=ALU.subtract), reads=[F, sp], writes=[R])
            c.op("dve", lambda e: e.tensor_copy(out=sp[:, 1, :], in_=R[:, :]), reads=[R], writes=[sp])
            c.op("dve", lambda e: e.tensor_tensor(out=R[:, :], in0=R[:, :], in1=sp[:, 1, :], op=ALU.subtract), reads=[R, sp], writes=[R])
            c.op("dve", lambda e: e.tensor_copy(out=sp[:, 2, :], in_=R[:, :]), reads=[R], writes=[sp])
            c.dma("sp", lambda e: e.dma_start(out=ft_d.ap()[:, :, ci * CH:(ci + 1) * CH], in_=sp[:, :, :]), reads=[sp], writes=[ft_d], nowaw=True)
            for q in range(3):
                c.op("dve", lambda e: e.tensor_scalar(out=sp[:, q, :], in0=sp[:, q, :], scalar1=-1.0, scalar2=None, op0=ALU.mult),
                     reads=[sp], writes=[sp])
            c.dma("sp", lambda e: e.dma_start(out=fn_d.ap()[:, :, ci * CH:(ci + 1) * CH], in_=sp[:, :, :]), reads=[sp], writes=[fn_d], nowaw=True)
    c.finish([])
    c.close()
    return nc


ATT_W = dict(w_qg=([D, 2 * D], F32), q_g=([1, 64], F32), w_o=([D, D], F32))
NQB = TOWN // 512


def stage_attn(c, k, modsb, xin_d, w, kf_d, v_d, qf_d, mask_d, xmid_d):
    qT_d = c.dram("qT_s", [16, 70, TOWN], BF16)
    sg_d = c.dram("sgT_s", [16, 64, TOWN], BF16)
    og_d = c.dram("ogT_s", [16, 64, TOWN], BF16)
    c.dma("sp", lambda e: e.dma_start(out=qT_d.ap()[:, 64:70, :], in_=qf_d.ap()), writes=[qT_d])
    with c.scope():
        wq = c.sbuf([P, 8, 2 * D], BF16, dma=True)
        qgb = c.sbuf([P, 64], F32, dma=True)
        wv = w["w_qg"].ap().rearrange("(kc p) n -> p kc n", p=P)
        for kc in range(8):
            c.dma("pool", lambda e: e.dma_start(out=wq[:, kc, :], in_=wv[:, kc, :]), writes=[wq], nowaw=True)
        c.dma("sp", lambda e: e.dma_start(out=qgb[:, :], in_=w["q_g"].ap().partition_broadcast(P)), writes=[qgb])
        xts = [c.sbuf([P, D], F32, dma=True) for _ in range(2)]
        h = c.sbuf([P, D], F32)
        ss = c.sbuf([P, 2], F32)
        hTb = c.sbuf([P, 8, P], BF16)
        qsb = c.sbuf([P, D], F32)
        sq = c.sbuf([P, D], F32)
        ssq = c.sbuf([P, 16], F32)
        qn = c.sbuf([P, D], BF16)
        sgn = c.sbuf([P, D], BF16)
        stg = [c.sbuf([P, 8, P], BF16) for _ in range(4)]
        pT = c.psum([P, D], F32)
        ptb = c.psum([P, D], BF16)
        pk = [c.psum([P, 512], F32) for _ in range(2)]
        for i in range(TOWN // P):
            xt = xts[i % 2]
            c.dma("sp", lambda e: e.dma_start(out=xt[:, :], in_=xin_d.ap()[i * P:(i + 1) * P, :]), writes=[xt])
            norm_mod(c, xt, h, ss, 1 * D, 0, modsb)
            for kc in range(8):
                c.op("pe", lambda e: e.transpose(out=pT[:, kc * P:(kc + 1) * P], in_=h[:, kc * P:(kc + 1) * P], identity=k.identf[:, :]),
                     reads=[h, k.identf], writes=[pT])
            c.op("act", lambda e: e.activation(out=hTb[:, :, :], in_=pT[:, :].rearrange("p (k t) -> p k t", k=8), func=AF.Copy),
                 reads=[pT], writes=[hTb])
            for hf in range(2):
                for kc in range(8):
                    c.op("pe", lambda e: e.matmul(pk[hf][:, :], lhsT=hTb[:, kc, :], rhs=wq[:, kc, hf * 512:(hf + 1) * 512],
                                                  start=(kc == 0), stop=(kc == 7)), reads=[hTb, wq], writes=[pk[hf]])
                c.op("act", lambda e: e.activation(out=qsb[:, hf * 512:(hf + 1) * 512], in_=pk[hf][:, :], func=AF.Copy),
                     reads=[pk[hf]], writes=[qsb])
            head_rms(c, qsb, qn, sq, ssq, qgb, 0.125)
            pair_transpose_store(c, k, qn, ptb, stg[(2 * i) % 4], qT_d, i * P)
            for hf in range(2):
                for kc in range(8):
                    c.op("pe", lambda e: e.matmul(pk[hf][:, :], lhsT=hTb[:, kc, :], rhs=wq[:, kc, D + hf * 512:D + (hf + 1) * 512],
                                                  start=(kc == 0), stop=(kc == 7)), reads=[hTb, wq], writes=[pk[hf]])
                c.op("act", lambda e: e.activation(out=sgn[:, hf * 512:(hf + 1) * 512], in_=pk[hf][:, :], func=AF.Sigmoid),
                     reads=[pk[hf]], writes=[sgn])
            pair_transpose_store(c, k, sgn, ptb, stg[(2 * i + 1) % 4], sg_d, i * P)
    with c.scope():
        NKT = SEQ // P
        kf = [c.sbuf([70, SEQ], BF16, dma=True) for _ in range(2)]
        vv = [c.sbuf([P, NKT, 72], BF16, dma=True) for _ in range(2)]
        qq = [c.sbuf([70, TOWN], BF16, dma=True) for _ in range(2)]
        sgh = [c.sbuf([64, TOWN], BF16, dma=True) for _ in range(2)]
        mk = c.sbuf([P, 16, 512], BF16, dma=True)
        ssb = [c.sbuf([P, 512], F32) for _ in range(3)]
        pTb = [c.sbuf([P, 512], BF16) for _ in range(4)]
        rinv = c.sbuf([P, 512], F32)
        bcs = c.sbuf([64, 512], F32)
        on = c.sbuf([64, 512], F32)
        og = [c.sbuf([64, 512], BF16) for _ in range(2)]
        pS = [c.psum([P, 512], F32) for _ in range(4)]
        pO = [c.psum([P, 512], F32) for _ in range(2)]
        pB = c.psum([64, 512], F32)
        c.dma("sp", lambda e: e.dma_start(out=mk[:, :, :], in_=mask_d.ap()), writes=[mk])

        def load_head(hh):
            s = hh % 2
            c.dma("sp", lambda e: e.dma_start(out=kf[s][:, :], in_=kf_d.ap()[hh]), writes=[kf[s]])
            c.dma("sp", lambda e: e.dma_start(out=vv[s][:, :, :], in_=v_d.ap()[hh]), writes=[vv[s]])
            c.dma("sp", lambda e: e.dma_start(out=qq[s][:, :], in_=qT_d.ap()[hh]), reads=[qT_d], writes=[qq[s]])
            c.dma("sp", lambda e: e.dma_start(out=sgh[s][:, :], in_=sg_d.ap()[hh]), reads=[sg_d], writes=[sgh[s]])

        load_head(0)
        NH_RUN = int(os.environ.get("ATT_HEADS", "16"))
        NS, LA = 4, 2
        units = []
        nq = 0
        for hh in range(NH_RUN):
            for j in range(NQB):
                nkt = 16 * (j + 1)
                for kt in range(nkt):
                    units.append((hh, j, kt, nkt, nq))
                nq += 1
        epi = {}

        def emit_qk(i):
            hh, j, kt, nkt, nq_ = units[i]
            s = hh % 2
            ps_ = pS[i % NS]
            c.op("pe", lambda e: e.matmul(ps_[:, :], lhsT=kf[s][:, kt * P:(kt + 1) * P], rhs=qq[s][:, j * 512:(j + 1) * 512],
                                          start=True, stop=True), reads=[kf[s], qq[s]], writes=[ps_])
            if kt >= 16 * j:
                sb_ = ssb[i % 3]
                c.op("dve", lambda e: e.tensor_tensor(out=sb_[:, :], in0=ps_[:, :], in1=mk[:, kt - 16 * j, :], op=ALU.add),
                     reads=[ps_, mk], writes=[sb_])

        def emit_rest(i):
            hh, j, kt, nkt, nq_ = units[i]
            s = hh % 2
            if j == 0 and kt == 6 and hh + 1 < NH_RUN:
                load_head(hh + 1)
            ps_ = pS[i % NS]
            pt_ = pTb[i % NS]
            po = pO[nq_ % 2]
            if kt >= 16 * j:
                sb_ = ssb[i % 3]
                c.op("act", lambda e: e.activation(out=pt_[:, :], in_=sb_[:, :], func=AF.Exp), reads=[sb_], writes=[pt_])
            else:
                c.op("act", lambda e: e.activation(out=pt_[:, :], in_=ps_[:, :], func=AF.Exp), reads=[ps_], writes=[pt_])
            c.op("pe", lambda e: e.matmul(po[0:65, :], lhsT=vv[s][:, kt, 0:65], rhs=pt_[:, :], start=(kt == 0), stop=(kt == nkt - 1)),
                 reads=[vv[s], pt_], writes=[po])
            if kt == nkt - 1:
                c.op("dve", lambda e: e.reciprocal(out=rinv[64:65, :], in_=po[64:65, :]), reads=[po], writes=[rinv])
                epi[i + 2] = (hh, j, nq_)

        def emit_epi(hh, j, nq_):
            s = hh % 2
            po = pO[nq_ % 2]
            o_ = og[nq_ % 2]
            c.op("pe", lambda e: e.matmul(pB[:, :], lhsT=k.onesf[64:65, 0:64], rhs=rinv[64:65, :], start=True, stop=True),
                 reads=[k.onesf, rinv], writes=[pB])
            c.op("act", lambda e: e.activation(out=bcs[:, :], in_=pB[:, :], func=AF.Copy), reads=[pB], writes=[bcs])
            c.op("dve", lambda e: e.tensor_tensor(out=on[:, :], in0=po[0:64, :], in1=bcs[:, :], op=ALU.mult), reads=[po, bcs], writes=[on])
            c.op("pool", lambda e: e.tensor_tensor(out=o_[:, :], in0=on[:, :], in1=sgh[s][:, j * 512:(j + 1) * 512], op=ALU.mult),
                 reads=[on, sgh[s]], writes=[o_])
            c.dma("sp", lambda e: e.dma_start(out=og_d.ap()[hh, :, j * 512:(j + 1) * 512], in_=o_[:, :]), reads=[o_], writes=[og_d], nowaw=True)

        NU = len(units)
        for idx in range(NU + LA + 3):
            if idx < NU:
                emit_qk(idx)
            if 0 <= idx - LA < NU:
                emit_rest(idx - LA)
            if (idx - LA) in epi:
                emit_epi(*epi.pop(idx - LA))
        assert not epi
    with c.scope():
        wo = c.sbuf([64, 16, D], BF16, dma=True)
        c.dma("pool", lambda e: e.dma_start(out=wo[:, :, :], in_=w["w_o"].ap().rearrange("(h d) n -> d h n", d=64)), writes=[wo])
        ogt = [c.sbuf([64, 16, P], BF16, dma=True) for _ in range(2)]
        xrs = [c.sbuf([P, D], F32, dma=True) for _ in range(2)]
        xos = [c.sbuf([P, D], F32) for _ in range(2)]
        tmp = c.sbuf([P, 512], F32)
        py = [c.psum([P, 512], F32) for _ in range(2)]
        for i in range(TOWN // P):
            o_ = ogt[i % 2]
            xr = xrs[i % 2]
            xo = xos[i % 2]
            c.dma("sp", lambda e: e.dma_start(out=o_[:, :, :], in_=og_d.ap()[:, :, i * P:(i + 1) * P].rearrange("h d t -> d h t")),
                  reads=[og_d], writes=[o_])
            c.dma("sp", lambda e: e.dma_start(out=xr[:, :], in_=xin_d.ap()[i * P:(i + 1) * P, :]), writes=[xr])
            for hf in range(2):
                for hh in range(16):
                    c.op("pe", lambda e: e.matmul(py[hf][:, :], lhsT=o_[:, hh, :], rhs=wo[:, hh, hf * 512:(hf + 1) * 512],
                                                  start=(hh == 0), stop=(hh == 15)), reads=[o_, wo], writes=[py[hf]])
                c.op("dve", lambda e: e.tensor_tensor(out=tmp[:, :], in0=py[hf][:, :], in1=modsb[:, 2 * D + hf * 512:2 * D + (hf + 1) * 512],
                                                      op=ALU.mult), reads=[py[hf], modsb], writes=[tmp])
                c.op("dve", lambda e: e.tensor_tensor(out=xo[:, hf * 512:(hf + 1) * 512], in0=tmp[:, :], in1=xr[:, hf * 512:(hf + 1) * 512],
                                                      op=ALU.add), reads=[tmp, xr], writes=[xo])
            c.dma("sp", lambda e: e.dma_start(out=xmid_d.ap()[i * P:(i + 1) * P, :], in_=xo[:, :]), reads=[xo], writes=[xmid_d], nowaw=True)


def build_attn_layer(final):
    nc = bass.Bass("TRN2", target_bir_lowering=False)
    c = Ctx(nc)
    cd = declare(c, CONST_SPECS)
    mw = declare(c, MOD_W)
    aw = declare(c, ATT_W)
    ew = declare(c, MOE_W)
    xin = c.dram("xin", [TOWN, D], F32, kind="ExternalInput")
    kf_d = c.dram("kf", [16, 70, SEQ], BF16, kind="ExternalInput")
    v_d = c.dram("vh", [16, P, SEQ // P, 72], BF16, kind="ExternalInput")
    qf_d = c.dram("qf", [16, 6, TOWN], BF16, kind="ExternalInput")
    mask_d = c.dram("maskadd", [P, 16, 512], BF16, kind="ExternalInput")
    fg = c.dram("final_g", [1, D], F32, kind="ExternalInput") if final else None
    xout = c.dram("xout", [TOWN, D], F32, kind="ExternalOutput")
    xmid = c.dram("xmid", [TOWN, D], F32)
    scr = moe_scratch(c, TOWN)
    k = load_consts(c, cd)
    modsb = c.sbuf([P, 6 * D], F32)
    stage_mods(c, k, mw["c_r"], mw["mod_w"], mw["mod_b"], mw["norm_g"], modsb)
    stage_attn(c, k, modsb, xin, aw, kf_d, v_d, qf_d, mask_d, xmid)
    stage_moe(c, k, modsb, xmid, ew, scr, xout, final_g=fg)
    c.finish([xout])
    c.close()
    return nc


_PROGS = {}


def _prog(name, fn, *a):
    if name not in _PROGS:
        _PROGS[name] = fn(*a)
    return _PROGS[name]


def _run(nc, in_maps):
    res = run_bass_kernel_spmd(nc, in_maps, core_ids=list(range(NCORES)))
    return res.results


def kernel(**inp):
    inp = {k_: np.asarray(v_) for k_, v_ in inp.items()}
    hc = host_consts()
    x = inp["x"]
    xs = [x[b] for b in range(2)]
    for l in range(2):
        maps = []
        for core in range(NCORES):
            b, r = divmod(core, 4)
            t0 = r * TOWN
            xin = np.zeros((P + TOWN, D), np.float32)
            xin[P:] = xs[b][t0:t0 + TOWN]
            if r > 0:
                xin[:P] = xs[b][t0 - P:t0]
            m = dict(hc)
            m.update(mod_inputs(inp, l, b)); m.update(conv_inputs(inp, l)); m.update(moe_inputs(inp, l))
            m["xin"] = xin
            m["flag"] = np.full((P, 1), 1.0 if r > 0 else 0.0, np.float32)
            maps.append(m)
        res = _run(_prog("conv", build_conv_layer), maps)
        xs = [np.concatenate([res[b * 4 + r]["xout"] for r in range(4)], axis=0) for b in range(2)]
    maps = []
    for core in range(NCORES):
        b, r = divmod(core, 4)
        m = dict(hc)
        m.update(dict(c_r=pcol(inp["c"][b], 8), mod_w=inp["kv_mod_w"], mod_b=inp["kv_mod_b"][None, :],
                      norm_g=inp["kv_norm_g"][None, :], w_kvf=inp["w_kvf"], b_f=inp["b_f"][None, :], k_g=inp["k_norm_g"][None, :]))
        m["xin"] = np.ascontiguousarray(xs[b][r * TOWN:(r + 1) * TOWN])
        maps.append(m)
    res = _run(_prog("kv", build_kv), maps)
    kT = [np.concatenate([res[b * 4 + r]["kT"] for r in range(4)], axis=2) for b in range(2)]
    vv = [np.concatenate([res[b * 4 + r]["v"] for r in range(4)], axis=0) for b in range(2)]
    lf = [np.concatenate([res[b * 4 + r]["logf"] for r in range(4)], axis=0) for b in range(2)]
    maps = []
    for core in range(NCORES):
        m = dict(hc)
        m["logf"] = np.ascontiguousarray(lf[core % 2])
        maps.append(m)
    res = _run(_prog("fcum", build_fcum), maps)
    ft = [res[b]["ft"] for b in range(2)]
    ftn = [res[b]["ftn"] for b in range(2)]
    one = np.ones((), np.float32).astype(ml_dtypes.bfloat16)
    kf, vh = [], []
    for b in range(2):
        a = np.empty((16, 70, SEQ), ml_dtypes.bfloat16)
        a[:, 0:64] = kT[b]
        a[:, 64:67] = one
        a[:, 67:70] = ftn[b]
        kf.append(a)
        vh.append(np.ascontiguousarray(vv[b].reshape(SEQ // P, P, 16, 72).transpose(2, 1, 0, 3)))
    ii = np.arange(P)
    masks = []
    for r in range(4):
        mk = np.zeros((P, 16, 512), np.float32)
        for tz in range(16):
            spos = (tz // 4) * 512 + (tz % 4) * P + ii[:, None]
            tpos = r * 512 + np.arange(512)[None, :]
            mk[:, tz, :] = np.where(spos <= tpos, 0.0, -30000.0)
        masks.append(mk.astype(ml_dtypes.bfloat16))
    def own_rows(r):
        return np.concatenate([np.arange((4 * j + r) * 512, (4 * j + r + 1) * 512) for j in range(NQB)])
    for li in range(2):
        l = 2 + li
        final = (li == 1)
        maps = []
        for core in range(NCORES):
            b, r = divmod(core, 4)
            rows = own_rows(r)
            m = dict(hc)
            m.update(mod_inputs(inp, l, b)); m.update(moe_inputs(inp, l))
            m.update(dict(w_qg=inp["attn_w_qg"][li], q_g=inp["q_norm_g"][li][None, :], w_o=inp["attn_w_o"][li]))
            m["xin"] = np.ascontiguousarray(xs[b][rows])
            m["kf"] = kf[b]
            m["vh"] = vh[b]
            qf = np.empty((16, 6, TOWN), ml_dtypes.bfloat16)
            qf[:, 0:3] = ft[b][:, :, rows]
            qf[:, 3:6] = one
            m["qf"] = qf
            m["maskadd"] = masks[r]
            if final:
                m["final_g"] = inp["final_norm_g"][None, :]
            maps.append(m)
        res = _run(_prog("attn%d" % final, build_attn_layer, final), maps)
        nx = [np.empty((SEQ, D), np.float32) for _ in range(2)]
        for core in range(NCORES):
            b, r = divmod(core, 4)
            nx[b][own_rows(r)] = res[core]["xout"]
        xs = nx
    return np.stack(xs, axis=0).astype(np.float32)
```
